# Optimizing a Trainium2 kernel written in Bass

```python
import math
import jax, jax.numpy as jnp
from jax import lax
import numpy as np

D_MODEL = 1024
BATCH = 2
SEQ = 8192
DEPTH = 1

CONF_DIM = D_MODEL
CONF_KERNEL = 31
SSM_EXPAND = 2
SSM_INNER = SSM_EXPAND * D_MODEL
SSM_HEAD_DIM = 64
SSM_HEADS = SSM_INNER // SSM_HEAD_DIM
SSM_GROUPS = 4
SSM_STATE = 128
SSM_CONV = 5
SSM_CHUNK = 128
SSM_CONV_CH = SSM_INNER + 2 * SSM_GROUPS * SSM_STATE
IN_SPLITS = (2 * CONF_DIM, SSM_INNER, SSM_INNER, SSM_GROUPS * SSM_STATE,
             SSM_GROUPS * SSM_STATE, SSM_HEADS, SSM_HEADS, D_MODEL, D_MODEL)
IN_COLS = sum(IN_SPLITS)
N_EXPERTS = 32
TOP_K = 4
D_FF = D_MODEL
SWIGLU_ALPHA = 1.702
SWIGLU_LIMIT = 7.0
MOE_BLOCK = 128
EPS = 1e-6

kernel_name = "hybrid_conformer_ssd_moe_encoder"


def rms_norm(x, g):
    xf = x.astype(jnp.float32)
    y = xf * lax.rsqrt(jnp.mean(xf * xf, axis=-1, keepdims=True) + EPS)
    return (y * g).astype(x.dtype)


def layer_norm(x, g, b):
    xf = x.astype(jnp.float32)
    mu = jnp.mean(xf, axis=-1, keepdims=True)
    var = jnp.mean(jnp.square(xf - mu), axis=-1, keepdims=True)
    return ((xf - mu) * lax.rsqrt(var + EPS) * g + b).astype(x.dtype)


def modulate(x, g, shift, scale):
    return rms_norm(x, g) * (1.0 + scale[:, None, :]) + shift[:, None, :]


def depthwise_conv(x, w, b):
    k, ch = w.shape
    y = lax.conv_general_dilated(x, w[:, None, :], window_strides=(1,),
                                 padding=[(k // 2, k // 2)],
                                 dimension_numbers=("NWC", "WIO", "NWC"),
                                 feature_group_count=ch)
    return y + b


def ssd_scan(xh, dt, a_coef, bm, cm):
    bsz, seqlen, nh, hp = xh.shape
    ng, ns = bm.shape[2], bm.shape[3]
    r = nh // ng
    nc = seqlen // SSM_CHUNK
    q = SSM_CHUNK
    xdt = (xh * dt[..., None]).reshape(bsz, nc, q, ng, r, hp)
    a = (dt * a_coef).reshape(bsz, nc, q, ng, r)
    a_cs = jnp.cumsum(a, axis=2)
    bc = bm.reshape(bsz, nc, q, ng, ns)
    cc = cm.reshape(bsz, nc, q, ng, ns)
    acs_t = jnp.moveaxis(a_cs, 2, -1)
    seg = acs_t[..., :, None] - acs_t[..., None, :]
    lower = jnp.tril(jnp.ones((q, q), dtype=bool))
    lmat = jnp.exp(jnp.where(lower, seg, -jnp.inf))
    cb = jnp.einsum("bcign,bcjgn->bcgij", cc, bc)
    y_diag = jnp.einsum("bcgij,bcgrij,bcjgrp->bcigrp", cb, lmat, xdt)
    decay_states = jnp.exp(a_cs[:, :, -1:] - a_cs)
    states = jnp.einsum("bcjgn,bcjgr,bcjgrp->bcgrpn", bc, decay_states, xdt)
    chunk_decay = jnp.exp(a_cs[:, :, -1])

    def step(h, inp):
        s, d = inp
        return h * d[..., None, None] + s, h

    h0 = jnp.zeros_like(states[:, 0])
    _, h_enter = lax.scan(step, h0, (jnp.moveaxis(states, 1, 0),
                                     jnp.moveaxis(chunk_decay, 1, 0)))
    h_enter = jnp.moveaxis(h_enter, 0, 1)
    y_off = jnp.einsum("bcign,bcgrpn,bcigr->bcigrp", cc, h_enter, jnp.exp(a_cs))
    return (y_diag + y_off).reshape(bsz, seqlen, nh, hp).astype(xh.dtype)


def moe_ffn(v, router_w, router_b, w_gu, b_gu, w_down, b_down):
    bsz, seqlen, d = v.shape
    t = bsz * seqlen
    vf = v.reshape(t, d)
    logits = (vf @ router_w + router_b).astype(jnp.float32)
    top_v, top_i = lax.top_k(logits, TOP_K)
    top_w = jax.nn.softmax(top_v, axis=-1).astype(v.dtype)
    n_assign = t * TOP_K
    n_blocks = -(-n_assign // MOE_BLOCK) + N_EXPERTS
    flat_e = top_i.reshape(-1)
    flat_tok = jnp.repeat(jnp.arange(t, dtype=jnp.int32), TOP_K)
    flat_w = top_w.reshape(-1)
    order = jnp.argsort(flat_e)
    sorted_e = flat_e[order]
    counts = jnp.bincount(flat_e, length=N_EXPERTS)
    padded = ((counts + MOE_BLOCK - 1) // MOE_BLOCK) * MOE_BLOCK
    pad_end = jnp.cumsum(padded)
    pad_start = pad_end - padded
    grp_start = jnp.cumsum(counts) - counts
    rank = jnp.arange(n_assign, dtype=jnp.int32) - grp_start[sorted_e]
    dest = pad_start[sorted_e] + rank
    buf_tok = jnp.full((n_blocks * MOE_BLOCK,), t, jnp.int32).at[dest].set(flat_tok[order])
    buf_w = jnp.zeros((n_blocks * MOE_BLOCK,), v.dtype).at[dest].set(flat_w[order])
    block_start = jnp.arange(n_blocks, dtype=pad_end.dtype) * MOE_BLOCK
    block_expert = jnp.minimum(jnp.searchsorted(pad_end, block_start, side="right"),
                               N_EXPERTS - 1).astype(jnp.int32)
    vpad = jnp.concatenate([vf, jnp.zeros((1, d), vf.dtype)], axis=0)
    xs = vpad[buf_tok].reshape(n_blocks, MOE_BLOCK, d)

    def expert_block(args):
        xb, e = args
        gu = xb @ w_gu[e] + b_gu[e]
        gate, up = jnp.split(gu, 2, axis=-1)
        gate = jnp.minimum(gate, SWIGLU_LIMIT)
        up = jnp.clip(up, -SWIGLU_LIMIT, SWIGLU_LIMIT)
        glu = gate * jax.nn.sigmoid(SWIGLU_ALPHA * gate)
        return ((up + 1.0) * glu) @ w_down[e] + b_down[e]

    ys = lax.map(expert_block, (xs, block_expert)).reshape(n_blocks * MOE_BLOCK, d)
    out = jax.ops.segment_sum(ys * buf_w[:, None], buf_tok, num_segments=t + 1)[:t]
    return out.reshape(bsz, seqlen, d)


def setup_inputs(seed: int = 0) -> dict:
    key = jax.random.key(seed)
    ks = iter(jax.random.split(key, 40))

    def nrm(shape, scale):
        return jax.random.normal(next(ks), shape, jnp.float32) * scale

    def gain(shape):
        return 1.0 + nrm(shape, 0.02)

    L, D = DEPTH, D_MODEL
    dt0 = jnp.exp(jax.random.uniform(next(ks), (2, L, SSM_HEADS),
                                     minval=math.log(1e-3), maxval=math.log(1e-1)))
    dt_bias = dt0 + jnp.log(-jnp.expm1(-dt0))
    a_log = jnp.log(jax.random.uniform(next(ks), (2, L, SSM_HEADS), minval=1.0, maxval=16.0))
    return {
        "x": nrm((BATCH, SEQ, D), 1.0),
        "c": nrm((BATCH, D), 1.0),
        "ada_w": nrm((L, D, 6 * D), 0.5 * D ** -0.5),
        "ada_b": nrm((L, 6 * D), 0.02),
        "norm_mix_g": gain((L, D)),
        "w_in": nrm((L, D, IN_COLS), D ** -0.5),
        "conf_dw_w": nrm((L, CONF_KERNEL, CONF_DIM), CONF_KERNEL ** -0.5),
        "conf_dw_b": nrm((L, CONF_DIM), 0.02),
        "conf_ln_g": gain((L, CONF_DIM)),
        "conf_ln_b": nrm((L, CONF_DIM), 0.02),
        "conf_out_w": nrm((L, CONF_DIM, D), CONF_DIM ** -0.5),
        "conf_out_b": nrm((L, D), 0.02),
        "ssm_conv_w": nrm((L, SSM_CONV, SSM_CONV_CH), SSM_CONV ** -0.5),
        "ssm_conv_b": nrm((L, SSM_CONV_CH), 0.02),
        "dt_bias_f": dt_bias[0],
        "dt_bias_b": dt_bias[1],
        "a_log_f": a_log[0],
        "a_log_b": a_log[1],
        "ssm_d": gain((L, SSM_HEADS)),
        "ssm_norm_g": gain((L, SSM_INNER)),
        "ssm_out_w": nrm((L, SSM_INNER, D), SSM_INNER ** -0.5),
        "w_o": nrm((L, D, D), D ** -0.5),
        "norm_ffn_g": gain((L, D)),
        "router_w": nrm((L, D, N_EXPERTS), D ** -0.5),
        "router_b": nrm((L, N_EXPERTS), 0.01),
        "w_gu": nrm((L, N_EXPERTS, D, 2 * D_FF), D ** -0.5),
        "b_gu": nrm((L, N_EXPERTS, 2 * D_FF), 0.02),
        "w_down": nrm((L, N_EXPERTS, D_FF, D), D_FF ** -0.5),
        "b_down": nrm((L, N_EXPERTS, D), 0.02),
        "final_ada_w": nrm((D, 2 * D), 0.5 * D ** -0.5),
        "final_ada_b": nrm((2 * D,), 0.02),
        "final_norm_g": gain((D,)),
    }


def reference(x, c, ada_w, ada_b, norm_mix_g, w_in, conf_dw_w, conf_dw_b, conf_ln_g,
              conf_ln_b, conf_out_w, conf_out_b, ssm_conv_w, ssm_conv_b, dt_bias_f,
              dt_bias_b, a_log_f, a_log_b, ssm_d, ssm_norm_g, ssm_out_w, w_o,
              norm_ffn_g, router_w, router_b, w_gu, b_gu, w_down, b_down,
              final_ada_w, final_ada_b, final_norm_g):
    bsz, seqlen, d = x.shape
    c_act = jax.nn.silu(c)
    split_pts = list(np.cumsum(IN_SPLITS)[:-1])
    for l in range(DEPTH):
        ada = c_act @ ada_w[l] + ada_b[l]
        sh1, sc1, g1, sh2, sc2, g2 = jnp.split(ada, 6, axis=-1)
        u = modulate(x, norm_mix_g[l], sh1, sc1)
        proj = u @ w_in[l]
        conf_in, z, xs, bm, cm, dtf, dtb, gate_conf, gate_ssm = jnp.split(proj, split_pts, axis=-1)
        a_half, g_half = jnp.split(conf_in, 2, axis=-1)
        hc = a_half * jax.nn.sigmoid(g_half)
        hc = depthwise_conv(hc, conf_dw_w[l], conf_dw_b[l])
        hc = jax.nn.silu(layer_norm(hc, conf_ln_g[l], conf_ln_b[l]))
        y_conf = hc @ conf_out_w[l] + conf_out_b[l]
        xbc = jnp.concatenate([xs, bm, cm], axis=-1)
        xbc = jax.nn.silu(depthwise_conv(xbc, ssm_conv_w[l], ssm_conv_b[l]))
        xs_c, bm_c, cm_c = jnp.split(xbc, [SSM_INNER, SSM_INNER + SSM_GROUPS * SSM_STATE], axis=-1)
        xh = xs_c.reshape(bsz, seqlen, SSM_HEADS, SSM_HEAD_DIM)
        bg = bm_c.reshape(bsz, seqlen, SSM_GROUPS, SSM_STATE)
        cg = cm_c.reshape(bsz, seqlen, SSM_GROUPS, SSM_STATE)
        dt_f = jax.nn.softplus((dtf + dt_bias_f[l]).astype(jnp.float32))
        dt_b = jax.nn.softplus((dtb + dt_bias_b[l]).astype(jnp.float32))
        a_f = -jnp.exp(a_log_f[l].astype(jnp.float32))
        a_b = -jnp.exp(a_log_b[l].astype(jnp.float32))
        y_fwd = ssd_scan(xh, dt_f, a_f, bg, cg)
        y_bwd = jnp.flip(ssd_scan(jnp.flip(xh, 1), jnp.flip(dt_b, 1), a_b,
                                  jnp.flip(bg, 1), jnp.flip(cg, 1)), 1)
        ys = y_fwd + y_bwd + xh * ssm_d[l][:, None]
        ys = ys.reshape(bsz, seqlen, SSM_INNER) * jax.nn.silu(z)
        ys = rms_norm(ys.reshape(bsz, seqlen, SSM_GROUPS, SSM_INNER // SSM_GROUPS), 1.0)
        ys = ys.reshape(bsz, seqlen, SSM_INNER) * ssm_norm_g[l]
        y_ssm = ys @ ssm_out_w[l]
        merged = jax.nn.sigmoid(gate_conf) * y_conf + jax.nn.sigmoid(gate_ssm) * y_ssm
        x = x + g1[:, None, :] * (merged @ w_o[l])
        v = modulate(x, norm_ffn_g[l], sh2, sc2)
        f = moe_ffn(v, router_w[l], router_b[l], w_gu[l], b_gu[l], w_down[l], b_down[l])
        x = x + g2[:, None, :] * f
    fin = c_act @ final_ada_w + final_ada_b
    sh_f, sc_f = jnp.split(fin, 2, axis=-1)
    return modulate(x, final_norm_g, sh_f, sc_f)
```

```python
import numpy as np
from contextlib import ExitStack
import concourse.bass as bass
import concourse.mybir as mybir
from concourse.bass_utils import run_bass_kernel_spmd

F32 = mybir.dt.float32
BF16 = mybir.dt.bfloat16
I32 = mybir.dt.int32
AF = mybir.ActivationFunctionType
ALU = mybir.AluOpType
AX = mybir.AxisListType

ENGS = ("pe", "act", "dve", "pool", "sp")
NDMA = 12

D = 1024
T = 2048
HALO = 16
TH = T + 2 * HALO
TS = T + 4
NCH = 16
EPS = 1e-6
NEXP = 32
C_A, C_G, C_Z, C_XS, C_B, C_C, C_DTF, C_DTB, C_GC, C_GS = 0, 1024, 2048, 4096, 6144, 6656, 7168, 7200, 7232, 8256


class Tag:
    __slots__ = ("name", "w", "r")

    def __init__(self, name=""):
        self.name = name
        self.w = None
        self.r = {}


class Prog:
    def __init__(self):
        self.ops = {e: [] for e in ENGS}
        self.cnt = {e: 0 for e in ENGS}
        self.seen = {e: {} for e in ENGS}
        self.dma_i = {"sp": 0, "pool": 0, "act": 0}
        self.dma_uses = {}
        self.fence_toks = {}

    def fence(self):
        f = {}
        for e in ENGS:
            if e != "sp" and self.cnt[e] > 0:
                f[("e", e)] = self.cnt[e]
        for k, u in self.dma_uses.items():
            f[k] = 16 * u
        self.fence_toks = f

    def _deps(self, eng, reads, writes):
        deps = dict(self.fence_toks)

        def add(k, v):
            if deps.get(k, 0) < v:
                deps[k] = v
        for t in reads:
            if t.w is not None:
                add(*t.w)
        for t in writes:
            if t.w is not None:
                add(*t.w)
            for k, v in t.r.items():
                add(k, v)
        waits = []
        for k, v in deps.items():
            if k == ("e", "pe") and eng == "pe":
                continue
            if self.seen[eng].get(k, 0) >= v:
                continue
            self.seen[eng][k] = v
            waits.append((k, v))
        return waits

    def _mark(self, tok, reads, writes):
        for t in reads:
            if t.r.get(tok[0], 0) < tok[1]:
                t.r[tok[0]] = tok[1]
        for t in writes:
            t.w = tok
            t.r = {}

    def op(self, eng, fn, reads=(), writes=()):
        waits = self._deps(eng, reads, writes)
        self.cnt[eng] += 1
        tok = (("e", eng), self.cnt[eng])
        self.ops[eng].append((waits, fn, (("e", eng), 1)))
        self._mark(tok, reads, writes)

    def dma(self, q, fn, reads=(), writes=()):
        waits = self._deps(q, reads, writes)
        i = self.dma_i[q]
        self.dma_i[q] += 1
        key = ("d", q, i % NDMA)
        uses = self.dma_uses.get(key, 0)
        if uses > 0 and self.seen[q].get(key, 0) < 16 * uses:
            waits.append((key, 16 * uses))
            self.seen[q][key] = 16 * uses
        self.dma_uses[key] = uses + 1
        tok = (key, 16 * (uses + 1))
        self.ops[q].append((waits, fn, (key, 16)))
        self._mark(tok, reads, writes)

    def finalize(self, nc, stack):
        keys = [("e", e) for e in ENGS if e != "sp"]
        for q in ("sp", "pool", "act"):
            for s in range(NDMA):
                keys.append(("d", q, s))
        sems = {k: stack.enter_context(nc.semaphore("s_" + "_".join(str(x) for x in k))) for k in keys}
        fin = [(k, 16 * u) for k, u in self.dma_uses.items()]
        fin += [(("e", e), self.cnt[e]) for e in ENGS if e != "sp" and self.cnt[e] > 0]
        ops = self.ops

        def replay(name, eng, final=False):
            for waits, fn, inc in ops[name]:
                for k, v in waits:
                    eng.wait_ge(sems[k], v)
                fn(eng).then_inc(sems[inc[0]], inc[1])
            if final:
                for k, v in fin:
                    eng.wait_ge(sems[k], v)

        with nc.Block() as block:
            @block.tensor
            def _(e):
                replay("pe", e)

            @block.scalar
            def _(e):
                replay("act", e)

            @block.vector
            def _(e):
                replay("dve", e)

            @block.gpsimd
            def _(e):
                replay("pool", e)

            @block.sync
            def _(e):
                replay("sp", e, final=True)


def _layout(items):
    off, o = {}, 0
    for n, w in items:
        off[n] = (o, w)
        o += w
    return off, o


SM, NS = _layout([("cT", 8), ("adab", 48), ("finb", 16), ("gmix", 8), ("gffn", 8), ("gfin", 8), ("mown", 32),
                  ("m3", 12), ("flg", 9), ("dw", 248), ("dwb", 8), ("lng", 8), ("lnb", 8), ("cw", 120), ("cb", 24),
                  ("cw3", 300), ("sng", 16), ("pidx", 1)])
BS = 256
NBLK = 8192 // BS + 32
RW, NR = _layout([("cob", 1024), ("rb", 32), ("dtb", 64), ("alog", 64), ("ssd", 32), ("dtb3", 96), ("alog3", 96), ("bstart", NBLK), ("thr16", 16)])


def build(upto=99, debug=False):
    nc = bass.Bass("TRN2", target_bir_lowering=False)
    P = Prog()

    def din(name, shape, dt=F32):
        return nc.dram_tensor(name, list(shape), dt, kind="ExternalInput").ap()

    def dscr(name, shape, dt):
        return nc.dram_tensor(name, list(shape), dt, kind="ExternalOutput" if debug else "Internal").ap()

    dbg_outs = {}

    def dbg(name, ap_sb, shape, dt, reads):
        if not debug:
            return
        d = nc.dram_tensor("dbg_" + name, list(shape), dt, kind="ExternalOutput").ap()
        dbg_outs[name] = d
        P.dma("sp", lambda e: e.dma_start(out=d, in_=ap_sb), reads, [Tag("dbgd")])

    xo = din("xo", [TH, D])
    xs3 = din("xs3", [3, TS, D])
    small_d = din("small", [128, NS])
    rows_d = din("rows", [1, NR])
    ada_w = din("ada_w", [1, D, 6 * D])
    fada_w = din("final_ada_w", [D, 2 * D])
    w_in = din("w_in", [1, D, 9280])
    wdt3 = din("wdt3", [3, D, 32])
    conf_out_w = din("conf_out_w", [1, D, D])
    ssm_out_w = din("ssm_out_w", [1, 2 * D, D])
    w_o = din("w_o", [1, D, D])
    router_w = din("router_w", [1, D, NEXP])
    w_gu = din("w_gu", [1, NEXP, D, 2 * D])
    w_down = din("w_down", [1, NEXP, D, D])
    b_down = din("b_down", [1, NEXP, D])
    bgu_tab = din("bgu_tab", [NEXP * 128, 16])
    out_d = nc.dram_tensor("out", [T, D], F32, kind="ExternalOutput").ap()

    xs_tok = [dscr(f"xs_tok{i}", [T, 2048], BF16) for i in range(4)]
    b_tok = [dscr(f"b_tok{i}", [T, 512], BF16) for i in range(4)]
    bT_d = dscr("bT", [512, T], BF16)
    cT_d = dscr("cTd", [512, T], BF16)
    hF_d = dscr("hF", [NCH, 128, 2048], BF16)
    hB_d = dscr("hB", [NCH, 128, 2048], BF16)
    yconf_d = dscr("yconf", [T, D], BF16)
    ysn_d = dscr("ysn", [T, 2048], BF16)
    x1_d = dscr("x1", [T, D], F32)
    vbuf_d = dscr("vbuf", [NBLK * BS, D], BF16)
    ybuf_d = dscr("ybuf", [NBLK * BS, D], F32)
    wgu_bf = nc.dram_tensor("wgu_bf", [NEXP * 128, 8 * 2 * D], BF16, kind="Internal").ap()
    wdn_bf = nc.dram_tensor("wdn_bf", [NEXP * 128, 8 * D], BF16, kind="Internal").ap()

    with ExitStack() as st:
        def sb(name, shape, dt):
            return st.enter_context(nc.sbuf_tensor("sb_" + name, list(shape), dt))

        tags = {}

        def tg(name):
            if name not in tags:
                tags[name] = Tag(name)
            return tags[name]

        banks = [st.enter_context(nc.psum_tensor(f"ps{i}", [128, 512], F32)) for i in range(8)]
        bank_i = [0]

        bank_rng = [0, 8]

        def psum():
            lo, hi = bank_rng
            i = lo + bank_i[0] % (hi - lo)
            bank_i[0] += 1
            return banks[i], tg(f"ps{i}")

        small = sb("small", [128, NS], F32)
        rows = sb("rows", [128, NR], F32)
        identf = sb("identf", [128, 128], F32)
        ident = sb("ident", [128, 128], BF16)
        onesf = sb("onesf", [128, 128], F32)
        onesb = sb("onesb", [128, 128], BF16)
        tri_f = sb("tri_f", [128, 128], F32)
        tri_b = sb("tri_b", [128, 128], F32)
        ustr_f = sb("ustr_f", [128, 128], F32)
        ustr_b = sb("ustr_b", [128, 128], F32)
        mk_f = sb("mk_f", [128, 128], BF16)
        mk_b = sb("mk_b", [128, 128], BF16)
        adaP = sb("adaP", [128, 64], F32)
        modP = sb("modP", [128, 24], F32)
        rowsC = sb("rowsC", [128, 6, D], F32)
        uT = sb("uT", [128, 8, TH], BF16)
        wbuf = [sb(f"wbuf{i}", [128, 8, 512], BF16) for i in range(3)]
        wbuf_i = [0]
        ARENA = 101 * 1024
        arena = sb("arena", [128, ARENA // 4], F32)
        ar_off = [0]

        def carve(shape, dt):
            n = int(np.prod(shape[1:]))
            nb = n * (4 if dt == F32 else 2)
            nb = (nb + 63) // 64 * 64
            o = ar_off[0]
            assert o + nb <= ARENA, (o, nb, ARENA)
            ar_off[0] = o + nb
            ap = arena[0:shape[0], o // 4:(o + nb) // 4]
            if dt != F32:
                ap = ap.bitcast(dt)
            ap = ap[:, 0:n]
            if len(shape) == 3:
                ap = ap.rearrange("p (a b) -> p a b", a=shape[1])
            elif len(shape) == 4:
                ap = ap.rearrange("p (a b c) -> p a b c", a=shape[1], b=shape[2])
            return ap

        def new_phase():
            ar_off[0] = 0
            P.fence()

        def S(name, i=0, n=1):
            o, w = SM[name]
            return small[:, o + i:o + i + n]

        def Rr(name, i=0, n=None):
            o, w = RW[name]
            n = w - i if n is None else n
            return rows[:, o + i:o + i + n]

        def mm(out, lhsT, rhs, start, stop, reads, writes, skip=False):
            P.op("pe", lambda e: e.matmul(out, lhsT=lhsT, rhs=rhs, start=start, stop=stop, skip_group_check=skip), reads, writes)

        def tr(out, in_, idt, reads, writes):
            P.op("pe", lambda e: e.transpose(out=out, in_=in_, identity=idt), reads, writes)

        def act(out, in_, func, reads, writes, bias=None, scale=None, accum=None):
            kw = {}
            if bias is not None:
                kw["bias"] = bias
            if scale is not None:
                kw["scale"] = scale
            if accum is not None:
                kw["accum_out"] = accum
            P.op("act", lambda e: e.activation(out=out, in_=in_, func=func, **kw), reads, writes)

        def tt(eng, out, in0, in1, op, reads, writes):
            P.op(eng, lambda e: e.tensor_tensor(out=out, in0=in0, in1=in1, op=op), reads, writes)

        def ts(eng, out, in0, s1, s2, op0, op1, reads, writes):
            if op1 is None:
                P.op(eng, lambda e: e.tensor_scalar(out=out, in0=in0, scalar1=s1, scalar2=None, op0=op0), reads, writes)
            else:
                P.op(eng, lambda e: e.tensor_scalar(out=out, in0=in0, scalar1=s1, scalar2=s2, op0=op0, op1=op1), reads, writes)

        def stt(eng, out, in0, sc, in1, op0, op1, reads, writes):
            P.op(eng, lambda e: e.scalar_tensor_tensor(out=out, in0=in0, scalar=sc, in1=in1, op0=op0, op1=op1), reads, writes)

        def cp(eng, out, in_, reads, writes):
            if eng == "act":
                P.op("act", lambda e: e.activation(out=out, in_=in_, func=AF.Copy), reads, writes)
            else:
                P.op(eng, lambda e: e.tensor_copy(out=out, in_=in_), reads, writes)

        def dma(q, out, in_, reads, writes):
            P.dma(q, lambda e: e.dma_start(out=out, in_=in_), reads, writes)

        def load_w(src, ncols=512):
            i = wbuf_i[0] % 3
            wbuf_i[0] += 1
            t = tg(f"wbuf{i}")
            dma("pool", wbuf[i][:, :, 0:ncols], src.rearrange("(kc p) n -> p kc n", p=128), [], [t])
            return wbuf[i], t

        tC = tg("const")
        dma("sp", small[:], small_d, [], [tC])
        dma("sp", rows[:], rows_d.partition_broadcast(128), [], [tC])
        P.op("pool", lambda e: e.memset(identf[:], 0.0), [], [tC])
        P.op("pool", lambda e: e.affine_select(out=identf[:], in_=identf[:], pattern=[[-1, 128]], compare_op=ALU.not_equal,
                                               fill=1.0, base=0, channel_multiplier=1), [tC], [tC])
        cp("pool", ident[:], identf[:], [tC], [tC])
        P.op("pool", lambda e: e.memset(onesf[:], 1.0), [], [tC])
        cp("pool", onesb[:], onesf[:], [tC], [tC])
        P.op("pool", lambda e: e.affine_select(out=tri_f[:], in_=onesf[:], pattern=[[1, 128]], compare_op=ALU.is_ge,
                                               fill=0.0, base=0, channel_multiplier=-1), [tC], [tC])
        P.op("pool", lambda e: e.affine_select(out=tri_b[:], in_=onesf[:], pattern=[[-1, 128]], compare_op=ALU.is_ge,
                                               fill=0.0, base=0, channel_multiplier=1), [tC], [tC])
        P.op("pool", lambda e: e.affine_select(out=ustr_f[:], in_=onesf[:], pattern=[[-1, 128]], compare_op=ALU.is_gt,
                                               fill=0.0, base=0, channel_multiplier=1), [tC], [tC])
        P.op("pool", lambda e: e.affine_select(out=ustr_b[:], in_=onesf[:], pattern=[[1, 128]], compare_op=ALU.is_gt,
                                               fill=0.0, base=0, channel_multiplier=-1), [tC], [tC])
        cp("pool", mk_f[:], tri_f[:], [tC], [tC])
        cp("pool", mk_b[:], tri_b[:], [tC], [tC])

        new_phase()
        cact = carve([128, 8], F32)
        wA = [carve([128, 8, 512], F32) for _ in range(2)]
        tA = tg("adaP")
        act(cact, S("cT", 0, 8), AF.Silu, [tC], [tg("cact")])
        pa, pat = psum()
        for blk in range(16):
            src = ada_w[0][:, blk * 512:(blk + 1) * 512] if blk < 12 else fada_w[:, (blk - 12) * 512:(blk - 11) * 512]
            wt = tg(f"wA{blk % 2}")
            dma("sp", wA[blk % 2], src.rearrange("(kc p) n -> p kc n", p=128), [], [wt])
            for cc in range(4):
                col = blk * 4 + cc
                for kc in range(8):
                    mm(pa[:, col:col + 1], wA[blk % 2][:, kc, cc * 128:(cc + 1) * 128], cact[:, kc:kc + 1], kc == 0, kc == 7,
                       [wt, tg("cact")], [pat])
        tt("dve", adaP[:, 0:48], pa[:, 0:48], S("adab", 0, 48), ALU.add, [pat, tC], [tA])
        tt("dve", adaP[:, 48:64], pa[:, 48:64], S("finb", 0, 16), ALU.add, [pat, tC], [tA])
        for i, (gname, c0) in enumerate((("gmix", 8), ("gffn", 32), ("gfin", 56))):
            stt("dve", modP[:, i * 8:(i + 1) * 8], adaP[:, c0:c0 + 8], 1.0, S(gname, 0, 8), ALU.add, ALU.mult, [tA, tC], [tg("modP")])
        A1, A2, Af = modP[:, 0:8], modP[:, 8:16], modP[:, 16:24]
        sh1, sh2 = adaP[:, 0:8], adaP[:, 24:32]
        dg = carve([128, 128], F32)
        for ri, vec in enumerate((adaP[:, 16:24], adaP[:, 40:48], Af, adaP[:, 48:56], A2, sh2)):
            for half in range(2):
                pr, prt = psum()
                for q in range(4):
                    kc = half * 4 + q
                    ts("dve", dg, identf[:], vec[:, kc:kc + 1], None, ALU.mult, None, [tC, tA, tg("modP")], [tg("dg")])
                    mm(pr[:, q * 128:(q + 1) * 128], onesf[:], dg, True, True, [tC, tg("dg")], [prt])
                cp("act", rowsC[:, ri, half * 512:(half + 1) * 512], pr[:, :], [prt], [tg("rowsC")])
        G1r, G2r, Afr, SHfr, A2r, SH2r = (rowsC[:, i, :] for i in range(6))
        if upto <= 0:
            dbg("adaP", adaP[:], [128, 64], F32, [tA])
            dbg("rowsC", rowsC[:], [128, 6, D], F32, [tg("rowsC")])
            dbg("rows", rows[:], [128, NR], F32, [tC])
            dbg("tri", tri_f[:], [128, 128], F32, [tC])
            P.finalize(nc, st)
            return nc

        def norm_T(src, n_tok, A_pp, sh_pp, dstT, dst_tag, bufs, want_f32=None):
            xt_l, junk, xn_l, st_l = bufs
            nt = (n_tok + 127) // 128

            def s1(t):
                r = min(128, n_tok - t * 128)
                xt, sq = xt_l[t % 2], st_l[t % 2]
                txt, tsq = tg(f"nT_xt{t % 2}"), tg(f"nT_sq{t % 2}")
                dma("sp", xt[0:r, :], src[t * 128:t * 128 + r, :], [], [txt])
                act(junk[0:r, :], xt[0:r, :], AF.Square, [txt], [tg("nT_junk"), tsq], accum=sq[0:r, 0:1])
                ts("dve", sq[0:r, 1:2], sq[0:r, 0:1], 1.0 / D, EPS, ALU.mult, ALU.add, [tsq], [tsq])
                act(sq[0:r, 2:3], sq[0:r, 1:2], AF.Sqrt, [tsq], [tsq])
                P.op("dve", lambda e, sq=sq, r=r: e.reciprocal(out=sq[0:r, 3:4], in_=sq[0:r, 2:3]), [tsq], [tsq])

            def s2(t):
                r = min(128, n_tok - t * 128)
                xt, xn, sq = xt_l[t % 2], xn_l[t % 2], st_l[t % 2]
                txt, txn, tsq = tg(f"nT_xt{t % 2}"), tg(f"nT_xn{t % 2}"), tg(f"nT_sq{t % 2}")
                act(xn[0:r, :], xt[0:r, :], AF.Copy, [txt, tsq], [txn], scale=sq[0:r, 3:4])
                pb, pbt = psum()
                pbb = pb[:].bitcast(BF16)
                for kc in range(8):
                    tr(pbb[:, kc * 128:kc * 128 + r], xn[0:r, kc * 128:(kc + 1) * 128], ident[0:r, 0:r], [txn, tC], [pbt])
                for kc in range(8):
                    ts("dve", dstT[:, kc, t * 128:t * 128 + r], pbb[:, kc * 128:kc * 128 + r], A_pp[:, kc:kc + 1], sh_pp[:, kc:kc + 1],
                       ALU.mult, ALU.add, [pbt, tA, tg("modP")], [dst_tag])

            s1(0)
            for t in range(nt):
                if t + 1 < nt:
                    s1(t + 1)
                s2(t)

        def blocks(W):
            out, t0 = [], 0
            while t0 < W:
                n = min(512, W - t0)
                out.append((t0, n))
                t0 += n
            return out

        def front(seq, srcT, src_tag, W, off, nchunk, col0, cw_name, cw_base, mL, mR, nm, bufs, hook=None):
            raw_l, cv_l, dg5, stage_l = bufs
            wstate = {}

            def stA(ch):
                if hook is not None:
                    hook()
                if ch % 4 == 0:
                    wstate["w"] = load_w(w_in[0][:, col0 + ch * 128:col0 + ch * 128 + 512])
                wtile, wt = wstate["w"]
                raw = raw_l[ch % 2]
                traw = tg(f"fr_raw{ch % 2}")
                for bi, (t0, n) in enumerate(blocks(W)):
                    pb, pbt = psum()
                    for k in range(8):
                        mm(pb[:, 0:n], wtile[:, k, (ch % 4) * 128:(ch % 4 + 1) * 128], srcT[:, k, t0:t0 + n], k == 0, k == 7,
                           [wt, src_tag], [pbt])
                    cp("act" if bi % 2 == 0 else "dve", raw[:, t0:t0 + n], pb[:, 0:n], [pbt], [traw])
                tt("pool", raw[:, 0:nm], raw[:, 0:nm], mL, ALU.mult, [traw, tC], [traw])
                tt("pool", raw[:, W - nm:W], raw[:, W - nm:W], mR, ALU.mult, [traw, tC], [traw])

            def stB(ch):
                raw = raw_l[ch % 2]
                traw = tg(f"fr_raw{ch % 2}")
                tdg = tg("fr_dg")
                for k in range(5):
                    ts("dve", dg5[:, k, :], ident[:], S(cw_name, cw_base + ch * 5 + k, 1), None, ALU.mult, None, [tC], [tdg])
                cv = cv_l[ch % 2]
                tcv = tg(f"fr_cv{ch % 2}")
                for tb in range(4):
                    pb, pbt = psum()
                    for k in range(5):
                        s0 = off - 2 + k + tb * 512
                        mm(pb[:, :], dg5[:, k, :], raw[:, s0:s0 + 512], k == 0, k == 4, [tdg, traw], [pbt])
                    act(cv[:, tb * 512:(tb + 1) * 512], pb[:, :], AF.Silu, [pbt, tC], [tcv], bias=S("cb", ch, 1))

            def stC(ch):
                cv = cv_l[ch % 2]
                tcv = tg(f"fr_cv{ch % 2}")
                if ch < 20:
                    stg = stage_l[ch % 2]
                    tstg = tg(f"fr_stg{ch % 2}")
                    for q in range(4):
                        pb, pbt = psum()
                        pbb = pb[:].bitcast(BF16)
                        for i in range(4):
                            tl = q * 4 + i
                            tr(pbb[:, i * 128:(i + 1) * 128], cv[:, tl * 128:(tl + 1) * 128], ident[:], [tcv, tC], [pbt])
                        cp("dve" if q % 2 == 0 else "act", stg[:, q * 4:(q + 1) * 4, :], pbb[:, 0:512].rearrange("p (a b) -> p a b", a=4),
                           [pbt], [tstg])
                    if ch < 16:
                        dst = xs_tok[seq].rearrange("(t p) c -> p t c", p=128)[:, :, ch * 128:(ch + 1) * 128]
                    else:
                        dst = b_tok[seq].rearrange("(t p) c -> p t c", p=128)[:, :, (ch - 16) * 128:(ch - 15) * 128]
                    dma("sp", dst, stg[:, :, :], [tstg], [tg(f"d_tok{seq}")])
                if seq == 3 and ch >= 16:
                    dstd = bT_d if ch < 20 else cT_d
                    c4 = (ch - 16) % 4
                    dma("sp", dstd[c4 * 128:(c4 + 1) * 128, :], cv[:, :], [tcv], [tg("d_bc")])

            for step in range(nchunk + 2):
                if step < nchunk:
                    stA(step)
                if 0 <= step - 1 < nchunk:
                    stB(step - 1)
                if 0 <= step - 2 < nchunk:
                    stC(step - 2)

        def dt_pass(srcT, src_tag, off, wdt_tile, wdt_tag, ncol, bias_row, dst, dst_tag, tmp):
            for t in range(NCH):
                pb, pbt = psum()
                for k in range(8):
                    mm(pb[:, 0:ncol], srcT[:, k, off + t * 128:off + (t + 1) * 128], wdt_tile[:, k, 0:ncol], k == 0, k == 7,
                       [src_tag, wdt_tag], [pbt])
                tt("dve", tmp[:, 0:ncol], pb[:, 0:ncol], bias_row, ALU.add, [pbt, tC], [tg("dt_tmp")])
                act(tmp[:, 0:ncol], tmp[:, 0:ncol], AF.Exp, [tg("dt_tmp")], [tg("dt_tmp")])
                act(dst[:, t, 0:ncol], tmp[:, 0:ncol], AF.Ln, [tg("dt_tmp")], [dst_tag], bias=1.0)

        new_phase()
        nb = ([carve([128, D], F32) for _ in range(2)], carve([128, D], BF16), [carve([128, D], BF16) for _ in range(2)],
              [carve([128, 4], F32) for _ in range(2)])
        tuT = tg("uT")
        norm_T(xo, TH, A1, sh1, uT, tuT, nb)
        dbg("adaP", adaP[:], [128, 64], F32, [tA])
        dbg("rowsC", rowsC[:], [128, 6, D], F32, [tg("rowsC")])
        dbg("uT", uT[:], [128, 8, TH], BF16, [tuT])
        if upto <= 1:
            P.finalize(nc, st)
            return nc

        pre_jobs = []
        for e in range(NEXP):
            for blk in range(4):
                pre_jobs.append((w_gu[0][e][:, blk * 512:(blk + 1) * 512],
                                 wgu_bf[e * 128:(e + 1) * 128, :].rearrange("p (kc n) -> p kc n", kc=8)[:, :, blk * 512:(blk + 1) * 512]))
            for blk in range(2):
                pre_jobs.append((w_down[0][e][:, blk * 512:(blk + 1) * 512],
                                 wdn_bf[e * 128:(e + 1) * 128, :].rearrange("p (kc n) -> p kc n", kc=8)[:, :, blk * 512:(blk + 1) * 512]))
        pre_i = [0]
        pre_pending = [None]
        pstage = []

        def prepass_step(n=1):
            for _ in range(n):
                if pre_pending[0] is not None:
                    buf, tag, dst = pre_pending[0]
                    dma("sp", dst, buf, [tag], [tg("d_wbf")])
                    pre_pending[0] = None
                if pre_i[0] < len(pre_jobs):
                    src, dst = pre_jobs[pre_i[0]]
                    buf, tname = pstage[pre_i[0] % len(pstage)]
                    pre_i[0] += 1
                    tag = tg(tname)
                    dma("pool", buf, src.rearrange("(kc p) n -> p kc n", p=128), [], [tag])
                    pre_pending[0] = (buf, tag, dst)

        def prepass_flush_pending():
            if pre_pending[0] is not None:
                buf, tag, dst = pre_pending[0]
                dma("sp", dst, buf, [tag], [tg("d_wbf")])
                pre_pending[0] = None

        new_phase()
        pstage[:] = [(carve([128, 8, 512], BF16), f"pstage{i}") for i in range(2)]
        hcT = carve([128, 8, T], BF16)
        sg_l = [carve([128, TH], BF16) for _ in range(2)]
        h_l = [carve([128, TH], BF16) for _ in range(2)]
        dg31 = carve([128, 31, 128], BF16)
        sqb = carve([128, 8, 512], BF16)
        lnA = carve([128, 512], F32)
        lnB = carve([128, 512], F32)
        lnC = carve([128, 512], F32)
        lnT = [carve([128, 512], F32) for _ in range(2)]
        ycs = [carve([128, D], BF16) for _ in range(2)]
        for j in range(8):
            prepass_step(5)
            if j % 4 == 0:
                wg_t, wg_tag = load_w(w_in[0][:, C_G + j * 128:C_G + j * 128 + 512])
                wa_t, wa_tag = load_w(w_in[0][:, C_A + j * 128:C_A + j * 128 + 512])
            sg, h = sg_l[j % 2], h_l[j % 2]
            tsg, th = tg(f"cf_sg{j % 2}"), tg(f"cf_h{j % 2}")
            for (t0, n) in blocks(TH):
                pb, pbt = psum()
                for k in range(8):
                    mm(pb[:, 0:n], wg_t[:, k, (j % 4) * 128:(j % 4 + 1) * 128], uT[:, k, t0:t0 + n], k == 0, k == 7, [wg_tag, tuT], [pbt])
                act(sg[:, t0:t0 + n], pb[:, 0:n], AF.Sigmoid, [pbt], [tsg])
            for (t0, n) in blocks(TH):
                pb, pbt = psum()
                for k in range(8):
                    mm(pb[:, 0:n], wa_t[:, k, (j % 4) * 128:(j % 4 + 1) * 128], uT[:, k, t0:t0 + n], k == 0, k == 7, [wa_tag, tuT], [pbt])
                tt("dve", h[:, t0:t0 + n], pb[:, 0:n], sg[:, t0:t0 + n], ALU.mult, [pbt, tsg], [th])
            tt("pool", h[:, 0:16], h[:, 0:16], S("mown", 0, 16), ALU.mult, [th, tC], [th])
            tt("pool", h[:, TH - 16:TH], h[:, TH - 16:TH], S("mown", 16, 16), ALU.mult, [th, tC], [th])
            tdg = tg("cf_dg")
            for k in range(31):
                ts("dve", dg31[:, k, :], ident[:], S("dw", j * 31 + k, 1), None, ALU.mult, None, [tC], [tdg])
            for tb in range(4):
                pb, pbt = psum()
                for k in range(31):
                    s0 = HALO - 15 + k + tb * 512
                    mm(pb[:, :], dg31[:, k, :], h[:, s0:s0 + 512], k == 0, k == 30, [tdg, th], [pbt])
                act(hcT[:, j, tb * 512:(tb + 1) * 512], pb[:, :], AF.Identity, [pbt, tC], [tg(f"hcT{tb}")], bias=S("dwb", j, 1))
        for tb in range(4):
            thc = tg(f"hcT{tb}")
            sl = slice(tb * 512, (tb + 1) * 512)
            tt("pool", sqb[:, :, :], hcT[:, :, sl], hcT[:, :, sl], ALU.mult, [thc], [tg("sqb")])
            p1, p1t = psum()
            for j in range(8):
                mm(p1[:, :], onesb[:], hcT[:, j, sl], j == 0, j == 7, [tC, thc], [p1t])
            p2, p2t = psum()
            for j in range(8):
                mm(p2[:, :], onesb[:], sqb[:, j, :], j == 0, j == 7, [tC, tg("sqb")], [p2t])
            tln = tg("ln")
            ts("dve", lnA, p1[:, :], 1.0 / D, None, ALU.mult, None, [p1t], [tln])
            tt("dve", lnB, lnA, lnA, ALU.mult, [tln], [tln])
            stt("dve", lnB, p2[:, :], 1.0 / D, lnB, ALU.mult, ALU.subtract, [p2t, tln], [tln])
            ts("dve", lnB, lnB, EPS, None, ALU.add, None, [tln], [tln])
            act(lnB, lnB, AF.Sqrt, [tln], [tln])
            P.op("dve", lambda e: e.reciprocal(out=lnB, in_=lnB), [tln], [tln])
            tt("dve", lnC, lnA, lnB, ALU.mult, [tln], [tln])
            for j in range(8):
                lt = lnT[j % 2]
                tlt = tg(f"lnT{j % 2}")
                tt("dve", lt, hcT[:, j, sl], lnB, ALU.mult, [thc, tln], [tlt])
                tt("pool", lt, lt, lnC, ALU.subtract, [tlt, tln], [tlt])
                act(hcT[:, j, sl], lt, AF.Silu, [tlt, tC], [thc], bias=S("lnb", j, 1), scale=S("lng", j, 1))
        prepass_flush_pending()
        wco_h = [load_w(conf_out_w[0][:, h_ * 512:(h_ + 1) * 512]) for h_ in range(2)]
        for t in range(NCH):
            yc = ycs[t % 2]
            tyc = tg(f"ycs{t % 2}")
            for half in range(2):
                pb, pbt = psum()
                for k in range(8):
                    mm(pb[:, :], hcT[:, k, t * 128:(t + 1) * 128], wco_h[half][0][:, k, :], k == 0, k == 7,
                       [tg(f"hcT{t // 4}"), wco_h[half][1]], [pbt])
                tt("dve", yc[:, half * 512:(half + 1) * 512], pb[:, :], Rr("cob", half * 512, 512), ALU.add, [pbt, tC], [tyc])
            dma("sp", yconf_d[t * 128:(t + 1) * 128, :], yc[:, :], [tyc], [tg("d_yconf")])
        if upto <= 2:
            P.finalize(nc, st)
            return nc

        new_phase()
        pstage[:] = [(carve([128, 8, 512], BF16), f"pstage{i}") for i in range(2)]
        fb = ([carve([128, TH], BF16) for _ in range(2)], [carve([128, T], BF16) for _ in range(2)], carve([128, 5, 128], BF16),
              [carve([128, 16, 128], BF16) for _ in range(2)])
        dt_own = sb("dt_own", [128, NCH, 64], F32)
        dt_sl = sb("dt_sl", [128, 3, NCH, 32], F32)
        dt_tmp = sb("dt_tmp", [128, 64], F32)
        wdt_t = carve([128, 8, 64], BF16)
        twdt = tg("wdt")
        dma("pool", wdt_t, w_in[0][:, C_DTF:C_DTF + 64].rearrange("(kc p) n -> p kc n", p=128), [], [twdt])
        front(3, uT, tuT, TH, HALO, 24, C_XS, "cw", 0, S("mown", 0, 16), S("mown", 16, 16), 16, fb, hook=prepass_step)
        dt_pass(uT, tuT, HALO, wdt_t, twdt, 64, Rr("dtb"), dt_own, tg("dt_own"), dt_tmp)
        if upto <= 3:
            dbg("dt_own", dt_own[:], [128, NCH, 64], F32, [tg("dt_own")])
            P.finalize(nc, st)
            return nc
        uTs = carve([128, 8, TS], BF16)
        nb3 = ([carve([128, D], F32) for _ in range(2)], carve([128, D], BF16), [carve([128, D], BF16) for _ in range(2)],
               [carve([128, 4], F32) for _ in range(2)])
        wdt_s = carve([128, 8, 32], BF16)
        for k in range(3):
            tus = tg("uTs")
            norm_T(xs3[k], TS, A1, sh1, uTs, tus, nb3)
            dma("pool", wdt_s, wdt3[k].rearrange("(kc p) n -> p kc n", p=128), [], [tg("wdts")])
            front(k, uTs, tus, TS, 2, 20, C_XS, "cw3", k * 100, S("m3", k * 4, 2), S("m3", k * 4 + 2, 2), 2, fb, hook=prepass_step)
            dt_pass(uTs, tus, 2, wdt_s, tg("wdts"), 32, Rr("dtb3", k * 32, 32), dt_sl[:, k], tg("dt_sl"), dt_tmp)

        prepass_flush_pending()
        new_phase()
        pstage[:] = [(wbuf[i][:, :, :], f"wbuf{i}") for i in range(3)]
        Arow = carve([128, 64], F32)
        Arow3 = carve([128, 96], F32)
        act(Arow, Rr("alog"), AF.Exp, [tC], [tg("Arow")])
        ts("dve", Arow, Arow, -1.0, None, ALU.mult, None, [tg("Arow")], [tg("Arow")])
        act(Arow3, Rr("alog3"), AF.Exp, [tC], [tg("Arow")])
        ts("dve", Arow3, Arow3, -1.0, None, ALU.mult, None, [tg("Arow")], [tg("Arow")])
        xs_c = [carve([128, 2048], BF16) for _ in range(2)]
        b_c = [carve([128, 512], BF16) for _ in range(2)]
        xdtd = [carve([128, 2048], BF16) for _ in range(2)]
        sm_l = [carve([128, 6, 32], F32) for _ in range(2)]
        Rst = carve([128, 2048], F32)
        hFs = carve([128, 2048], F32)
        hBs = carve([128, 2048], F32)
        hbf = [carve([128, 2048], BF16) for _ in range(2)]
        cs_i = [0]

        def cs_front(seq, c, dt_ap, A_ap, tri):
            i = cs_i[0] % 2
            cs_i[0] += 1
            if cs_i[0] % 2 == 0:
                prepass_step(1)
            xc, bc, xd, smt = xs_c[i], b_c[i], xdtd[i], sm_l[i]
            txc, tsm, txd = tg(f"cs_x{i}"), tg(f"cs_sm{i}"), tg(f"cs_xd{i}")
            dma("sp", xc, xs_tok[seq][c * 128:(c + 1) * 128, :], [tg(f"d_tok{seq}")], [txc])
            dma("sp", bc, b_tok[seq][c * 128:(c + 1) * 128, :], [tg(f"d_tok{seq}")], [txc])
            a_t, tot, dec, cdb, sc = smt[:, 0, :], smt[:, 1, :], smt[:, 2, :], smt[:, 3, :], smt[:, 4, :]
            tt("dve", a_t, dt_ap, A_ap, ALU.mult, [tg("dt_own"), tg("dt_sl"), tg("Arow")], [tsm])
            pa_, pat_ = psum()
            mm(pa_[:, 0:32], tri[:], a_t, True, True, [tC, tsm], [pat_])
            mm(pa_[:, 32:64], onesf[:], a_t, True, True, [tC, tsm], [pat_])
            cp("act", tot, pa_[:, 32:64], [pat_], [tsm])
            tt("dve", dec, tot, pa_[:, 0:32], ALU.subtract, [pat_, tsm], [tsm])
            act(dec, dec, AF.Exp, [tsm], [tsm])
            act(cdb, tot, AF.Exp, [tsm], [tsm])
            tt("dve", sc, dec, dt_ap, ALU.mult, [tsm, tg("dt_own"), tg("dt_sl")], [tsm])
            tt("dve", xd.rearrange("p (h d) -> p h d", h=32), xc.rearrange("p (h d) -> p h d", h=32),
               sc.unsqueeze(2).to_broadcast([128, 32, 64]), ALU.mult, [txc, tsm], [txd])
            return bc, xd, cdb, txc, txd, tsm

        def cs_back(ctx, Racc, tR):
            bc, xd, cdb, txc, txd, tsm = ctx
            for g in range(4):
                pb, pbt = psum()
                mm(pb[:, :], bc[:, g * 128:(g + 1) * 128], xd[:, g * 512:(g + 1) * 512], True, True, [txc, txd], [pbt])
                Rg = Racc[:, g * 512:(g + 1) * 512]
                tt("dve", Rg.rearrange("p (h d) -> p h d", h=8), Rg.rearrange("p (h d) -> p h d", h=8),
                   cdb[:, g * 8:(g + 1) * 8].unsqueeze(2).to_broadcast([128, 8, 64]), ALU.mult, [tR, tsm], [tR])
                tt("dve", Rg, Rg, pb[:, :], ALU.add, [tR, pbt], [tR])

        tRs, tHF, tHB = tg("Rst"), tg("hFs"), tg("hBs")
        P.op("pool", lambda e: e.memset(Rst, 0.0), [], [tRs])
        P.op("pool", lambda e: e.memset(hFs, 0.0), [], [tHF])
        P.op("pool", lambda e: e.memset(hBs, 0.0), [], [tHB])
        items = []
        for k in range(3):
            for c in range(NCH):
                pre = (lambda k=k: ts("dve", Rst, Rst, S("flg", k, 1), None, ALU.mult, None, [tRs, tC], [tRs])) if c == 0 else None

                def post(k=k):
                    stt("dve", hFs, Rst, S("flg", 3 + k, 1), hFs, ALU.mult, ALU.add, [tRs, tHF, tC], [tHF])
                    stt("dve", hBs, Rst, S("flg", 6 + k, 1), hBs, ALU.mult, ALU.add, [tRs, tHB, tC], [tHB])
                items.append(((k, c, dt_sl[:, k, c, :], Arow3[:, k * 32:(k + 1) * 32], tri_f), Rst, tRs, pre, post if c == NCH - 1 else None))
        for c in range(NCH):
            def pre(c=c):
                hb_ = hbf[c % 2]
                cp("act", hb_, hFs, [tHF], [tg(f"hbf{c % 2}")])
                dma("sp", hF_d[c], hb_, [tg(f"hbf{c % 2}")], [tg("d_hF")])
            items.append(((3, c, dt_own[:, c, 0:32], Arow[:, 0:32], tri_f), hFs, tHF, pre, None))
        for c in range(NCH - 1, -1, -1):
            def pre(c=c):
                hb_ = hbf[c % 2]
                cp("act", hb_, hBs, [tHB], [tg(f"hbf{c % 2}")])
                dma("sp", hB_d[c], hb_, [tg(f"hbf{c % 2}")], [tg("d_hB")])
            items.append(((3, c, dt_own[:, c, 32:64], Arow[:, 32:64], tri_b), hBs, tHB, pre, None))

        def run_back(it, ctx):
            _, Racc, tR, pre, post = it
            if pre is not None:
                pre()
            cs_back(ctx, Racc, tR)
            if post is not None:
                post()
        prev = None
        for it in items:
            ctx = cs_front(*it[0])
            if prev is not None:
                run_back(*prev)
            prev = (it, ctx)
        run_back(*prev)
        if upto <= 4:
            P.finalize(nc, st)
            return nc

        new_phase()
        Arow = carve([128, 64], F32)
        act(Arow, Rr("alog"), AF.Exp, [tC], [tg("Arow")])
        ts("dve", Arow, Arow, -1.0, None, ALU.mult, None, [tg("Arow")], [tg("Arow")])
        wz = carve([128, 8, 2048], BF16)
        twz = tg("wz")
        for q in range(4):
            dma("pool", wz[:, :, q * 512:(q + 1) * 512], w_in[0][:, C_Z + q * 512:C_Z + (q + 1) * 512].rearrange("(kc p) n -> p kc n", p=128),
                [], [twz])
        xsc_l = [carve([128, 2048], BF16) for _ in range(2)]
        bTc_l = [carve([128, 4, 128], BF16) for _ in range(2)]
        cTc_l = [carve([128, 4, 128], BF16) for _ in range(2)]
        hst = [carve([128, 2048], BF16) for _ in range(2)]
        xdt = [carve([128, 2048], BF16) for _ in range(2)]
        rsegp = [carve([128, 4, 128], F32) for _ in range(3)]
        eseg = [carve([128, 512], BF16) for _ in range(2)]
        MTl = [carve([128, 4, 128], BF16) for _ in range(2)]
        CBm = [carve([128, 4, 128], BF16) for _ in range(2)]
        yo = carve([128, 2048], F32)
        ytmp_l = [carve([128, 512], F32) for _ in range(2)]
        szl = [carve([128, 512], BF16) for _ in range(4)]
        ysl = [carve([128, 2048], BF16) for _ in range(2)]
        sm5 = carve([128, 8, 32], F32)
        ss5 = carve([128, 8], F32)
        jk5 = carve([128, 512], BF16)
        tris = (tri_f, tri_b)
        ustrs = (ustr_f, ustr_b)
        mks = (mk_f, mk_b)
        hds = (hF_d, hB_d)
        rs_i = 0
        yt_i = 0
        for c in range(NCH):
            prepass_step(2)
            bank_rng[:] = [4, 8]
            cb = c % 2
            xsc, bTc, cTc = xsc_l[cb], bTc_l[cb], cTc_l[cb]
            tx, tbc, tyo = tg(f"p5_x{cb}"), tg(f"p5_bc{cb}"), tg("p5_yo")
            dma("sp", xsc, xs_tok[3][c * 128:(c + 1) * 128, :], [tg("d_tok3")], [tx])
            dma("sp", bTc, bT_d.rearrange("(g n) t -> n g t", n=128)[:, :, c * 128:(c + 1) * 128], [tg("d_bc")], [tbc])
            dma("sp", cTc, cT_d.rearrange("(g n) t -> n g t", n=128)[:, :, c * 128:(c + 1) * 128], [tg("d_bc")], [tbc])
            for d in range(2):
                dma("sp", hst[d], hds[d][c], [tg("d_hF"), tg("d_hB")], [tg(f"p5_h{d}")])
            for g in range(4):
                sl = slice(g * 512, (g + 1) * 512)
                pz, pzt = psum()
                for k in range(8):
                    mm(pz[:, :], uT[:, k, HALO + c * 128:HALO + (c + 1) * 128], wz[:, k, sl], k == 0, k == 7, [tuT, twz], [pzt])
                act(szl[g], pz[:, :], AF.Silu, [pzt], [tg(f"p5_sz{g}")])
            pcb, pcbt = psum()
            for g in range(4):
                mm(pcb[:, g * 128:(g + 1) * 128], bTc[:, g, :], cTc[:, g, :], True, True, [tbc], [pcbt])
            for d in range(2):
                tt("dve", CBm[d], pcb[:, :].rearrange("p (g i) -> p g i", g=4), mks[d][:, :].unsqueeze(1).to_broadcast([128, 4, 128]),
                   ALU.mult, [pcbt, tC], [tg(f"p5_cbm{d}")])
            for d in range(2):
                tsm = tg(f"p5_sm{d}")
                dt_ap = dt_own[:, c, d * 32:(d + 1) * 32]
                a_t, e_t = sm5[:, d * 4 + 0, :], sm5[:, d * 4 + 1, :]
                tt("dve", a_t, dt_ap, Arow[:, d * 32:(d + 1) * 32], ALU.mult, [tg("dt_own"), tg("Arow")], [tsm])
                pa_, pat_ = psum()
                mm(pa_[:, 0:32], tris[d][:], a_t, True, True, [tC, tsm], [pat_])
                act(e_t, pa_[:, 0:32], AF.Exp, [pat_], [tsm])
                tt("dve" if d == 0 else "pool", xdt[d].rearrange("p (h d) -> p h d", h=32), xsc.rearrange("p (h d) -> p h d", h=32),
                   dt_ap.unsqueeze(2).to_broadcast([128, 32, 64]), ALU.mult, [tx, tg("dt_own")], [tg(f"p5_xdt{d}")])
            tt("pool", yo.rearrange("p (h d) -> p h d", h=32), xsc.rearrange("p (h d) -> p h d", h=32),
               Rr("ssd").unsqueeze(2).to_broadcast([128, 32, 64]), ALU.mult, [tx, tC], [tyo])
            for d in range(2):
                tsm = tg(f"p5_sm{d}")
                txd = tg(f"p5_xdt{d}")
                a_t, e_t = sm5[:, d * 4 + 0, :], sm5[:, d * 4 + 1, :]

                def seg(hq, d=d, a_t=a_t, tsm=tsm):
                    nonlocal rs_i
                    rsp, trs = rsegp[rs_i % 3], tg(f"p5_rs{rs_i % 3}")
                    rs_i += 1
                    tt("dve", rsp, tris[d][:, :].unsqueeze(1).to_broadcast([128, 4, 128]),
                       a_t[:, hq * 4:(hq + 1) * 4].unsqueeze(2).to_broadcast([128, 4, 128]), ALU.mult, [tC, tsm], [trs])
                    pseg, psegt = psum()
                    mm(pseg[:, :], ustrs[d][:], rsp.rearrange("p a b -> p (a b)"), True, True, [tC, trs], [psegt])
                    return pseg, psegt
                nxt = seg(0)
                for hq in range(8):
                    g = hq // 2
                    pseg, psegt = nxt
                    if hq + 1 < 8:
                        nxt = seg(hq + 1)
                    es, tes = eseg[hq % 2], tg(f"p5_es{hq % 2}")
                    act(es, pseg[:, :], AF.Exp, [psegt], [tes])
                    MT, tmt = MTl[hq % 2], tg(f"p5_mt{hq % 2}")
                    tt("dve", MT, es.rearrange("p (a b) -> p a b", a=4), CBm[d][:, g, :].unsqueeze(1).to_broadcast([128, 4, 128]), ALU.mult,
                       [tes, tg(f"p5_cbm{d}")], [tmt])
                    for hh in range(4):
                        h = hq * 4 + hh
                        mm(banks[g][:, (h % 8) * 64:(h % 8 + 1) * 64], MT[:, hh, :], xdt[d][:, h * 64:(h + 1) * 64], d == 0 and h % 8 == 0,
                           d == 1 and h % 8 == 7, [tmt, txd], [tg(f"ps{g}")], skip=True)
                for g in range(4):
                    po, pot = psum()
                    mm(po[:, :], cTc[:, g, :], hst[d][:, g * 512:(g + 1) * 512], True, True, [tbc, tg(f"p5_h{d}")], [pot])
                    ytmp, tyt = ytmp_l[yt_i % 2], tg(f"p5_ytmp{yt_i % 2}")
                    yt_i += 1
                    tt("dve", ytmp.rearrange("p (h d) -> p h d", h=8), po[:, :].rearrange("p (h d) -> p h d", h=8),
                       e_t[:, g * 8:(g + 1) * 8].unsqueeze(2).to_broadcast([128, 8, 64]), ALU.mult, [pot, tsm], [tyt])
                    tt("dve", yo[:, g * 512:(g + 1) * 512], yo[:, g * 512:(g + 1) * 512], ytmp, ALU.add, [tyt, tyo], [tyo])
            ysn_t, tys = ysl[c % 2], tg(f"p5_ys{c % 2}")
            for g in range(4):
                sl = slice(g * 512, (g + 1) * 512)
                tt("dve", yo[:, sl], yo[:, sl], banks[g][:, :], ALU.add, [tyo, tg(f"ps{g}")], [tyo])
                tt("dve", yo[:, sl], yo[:, sl], szl[g], ALU.mult, [tyo, tg(f"p5_sz{g}")], [tyo])
                act(jk5, yo[:, sl], AF.Square, [tyo], [tg("p5_jk"), tg("p5_ss")], accum=ss5[:, g:g + 1])
            tss = tg("p5_ss")
            ts("dve", ss5[:, 4:8], ss5[:, 0:4], 1.0 / 512, EPS, ALU.mult, ALU.add, [tss], [tss])
            act(ss5[:, 4:8], ss5[:, 4:8], AF.Sqrt, [tss], [tss])
            P.op("dve", lambda e: e.reciprocal(out=ss5[:, 4:8], in_=ss5[:, 4:8]), [tss], [tss])
            for g in range(4):
                sl = slice(g * 512, (g + 1) * 512)
                if g % 2 == 0:
                    ts("dve", ysn_t[:, sl], yo[:, sl], ss5[:, 4 + g:5 + g], None, ALU.mult, None, [tyo, tss], [tys])
                else:
                    act(ysn_t[:, sl], yo[:, sl], AF.Copy, [tyo, tss], [tys], scale=ss5[:, 4 + g:5 + g])
            dma("sp", ysn_d[c * 128:(c + 1) * 128, :], ysn_t, [tys], [tg("d_ysn")])
        bank_rng[:] = [0, 8]
        while pre_i[0] < len(pre_jobs) or pre_pending[0] is not None:
            prepass_step(1)
        if upto <= 5:
            P.finalize(nc, st)
            return nc

        new_phase()
        wgs = carve([128, 8, 2048], BF16)
        wso = carve([128, 16, D], BF16)
        tw6 = tg("w6")
        wo_h = [load_w(w_o[0][:, q * 512:(q + 1) * 512]) for q in range(2)]
        for q in range(4):
            dma("pool", wgs[:, :, q * 512:(q + 1) * 512], w_in[0][:, C_GC + q * 512:C_GC + (q + 1) * 512].rearrange("(kc p) n -> p kc n", p=128),
                [], [tw6])
        for q in range(2):
            dma("pool", wso[:, :, q * 512:(q + 1) * 512], ssm_out_w[0][:, q * 512:(q + 1) * 512].rearrange("(kc p) n -> p kc n", p=128), [], [tw6])
        for kc in range(16):
            ts("dve", wso[:, kc, :], wso[:, kc, :], S("sng", kc, 1), None, ALU.mult, None, [tw6, tC], [tw6])
        ysn_tl = [carve([128, 2048], BF16) for _ in range(2)]
        yc_tl = [carve([128, D], BF16) for _ in range(2)]
        x_tl = [carve([128, D], F32) for _ in range(2)]
        ysnT = carve([128, 16, 128], BF16)
        sgt = carve([128, 2048], BF16)
        m1 = carve([128, D], BF16)
        mg = carve([128, D], BF16)
        mT = carve([128, 8, 128], BF16)
        tmp6 = carve([128, 512], F32)
        for t in range(NCH):
            tin, tys_, tsg6, tm1, tmg, tmT, ttmp = tg(f"p6_in{t % 2}"), tg("p6_ysnT"), tg("p6_sg"), tg("p6_m1"), tg("p6_mg"), tg("p6_mT"), tg("p6_tmp")
            ysn_t, yc_t, x_t = ysn_tl[t % 2], yc_tl[t % 2], x_tl[t % 2]
            tpx = tg(f"p6_x{t % 2}")
            dma("sp", ysn_t, ysn_d[t * 128:(t + 1) * 128, :], [tg("d_ysn")], [tin])
            dma("sp", yc_t, yconf_d[t * 128:(t + 1) * 128, :], [tg("d_yconf")], [tin])
            dma("sp", x_t, xo[HALO + t * 128:HALO + (t + 1) * 128, :], [], [tpx])
            for q in range(2):
                pb, pbt = psum()
                pbb = pb[:].bitcast(BF16)
                for i in range(8):
                    tr(pbb[:, i * 128:(i + 1) * 128], ysn_t[:, (q * 8 + i) * 128:(q * 8 + i + 1) * 128], ident[:], [tin, tC], [pbt])
                cp("dve" if q == 0 else "act", ysnT[:, q * 8:(q + 1) * 8, :], pbb.rearrange("p (a b) -> p a b", a=8), [pbt], [tys_])
            for q in range(4):
                pb, pbt = psum()
                for k in range(8):
                    mm(pb[:, :], uT[:, k, HALO + t * 128:HALO + (t + 1) * 128], wgs[:, k, q * 512:(q + 1) * 512], k == 0, k == 7, [tuT, tw6], [pbt])
                act(sgt[:, q * 512:(q + 1) * 512], pb[:, :], AF.Sigmoid, [pbt], [tsg6])
            tt("pool", m1, sgt[:, 0:D], yc_t, ALU.mult, [tsg6, tin], [tm1])
            for half in range(2):
                sl = slice(half * 512, (half + 1) * 512)
                pb, pbt = psum()
                for k in range(16):
                    mm(pb[:, :], ysnT[:, k, :], wso[:, k, sl], k == 0, k == 15, [tys_, tw6], [pbt])
                tt("dve", tmp6, pb[:, :], sgt[:, D + half * 512:D + (half + 1) * 512], ALU.mult, [pbt, tsg6], [ttmp])
                tt("pool", mg[:, sl], tmp6, m1[:, sl], ALU.add, [ttmp, tm1], [tmg])
            pb, pbt = psum()
            pbb = pb[:].bitcast(BF16)
            for i in range(8):
                tr(pbb[:, i * 128:(i + 1) * 128], mg[:, i * 128:(i + 1) * 128], ident[:], [tmg, tC], [pbt])
            cp("act", mT, pbb.rearrange("p (a b) -> p a b", a=8), [pbt], [tmT])
            for half in range(2):
                sl = slice(half * 512, (half + 1) * 512)
                pb, pbt = psum()
                for k in range(8):
                    mm(pb[:, :], mT[:, k, :], wo_h[half][0][:, k, :], k == 0, k == 7, [tmT, wo_h[half][1]], [pbt])
                tt("dve", tmp6, pb[:, :], G1r[:, sl], ALU.mult, [pbt, tg("rowsC")], [ttmp])
                tt("pool", x_t[:, sl], tmp6, x_t[:, sl], ALU.add, [ttmp, tpx], [tpx])
            dma("sp", x1_d[t * 128:(t + 1) * 128, :], x_t, [tpx], [tg("d_x1")])
        if upto <= 6:
            P.finalize(nc, st)
            return nc

        new_phase()
        xt_l = [carve([128, D], F32) for _ in range(2)]
        junk = carve([128, D], BF16)
        xn = carve([128, D], F32)
        vtok_all = carve([128, NCH, D], BF16)
        vTt_l = [carve([128, 8, 128], BF16) for _ in range(2)]
        st_l = [carve([128, 4], F32) for _ in range(2)]
        rwb = carve([128, 8, NEXP], BF16)
        lg_all = carve([128, NCH, 32], F32)
        wr_all = carve([128, NCH, 32], F32)
        sl_all = carve([128, NCH, 32], F32)
        m8_all = carve([128, NCH, 8], F32)
        rt = carve([128, 8, 32], F32)
        base = carve([128, 32], F32)
        big = carve([128, NBLK, 32], F32)
        bexp = carve([128, NBLK], F32)
        idxw_f = carve([128, NBLK], F32)
        dma("pool", rwb, router_w[0].rearrange("(kc p) n -> p kc n", p=128), [], [tg("rwb")])
        wk_all = dt_own[:, :, 0:4]
        idx_f = dt_own[:, :, 4:8]
        idx_i = sb("idx_i", [128, NCH * 4], I32)
        idxw_i = sb("idxw_i", [128, NBLK], I32)
        bexp_i = sb("bexp_i", [128, NBLK], I32)
        twk, trt, tbase, trA = tg("wk_all"), tg("rt"), tg("base"), tg("routeA")
        P.op("pool", lambda e: e.memset(base, 0.0), [], [tbase])
        xn_l = [xn, carve([128, D], F32)]
        rt_l = [rt, carve([128, 8, 32], F32)]

        def rA1(t):
            xt, sq, vTt, xn_ = xt_l[t % 2], st_l[t % 2], vTt_l[t % 2], xn_l[t % 2]
            txt, txn, tsq, tvTt = tg(f"m_xt{t % 2}"), tg(f"m_xn{t % 2}"), tg(f"m_sq{t % 2}"), tg(f"m_vTt{t % 2}")
            tvt = tg(f"vtok{t}")
            dma("sp", xt, x1_d[t * 128:(t + 1) * 128, :], [tg("d_x1")], [txt])
            act(junk, xt, AF.Square, [txt], [tg("m_junk"), tsq], accum=sq[:, 0:1])
            ts("dve", sq[:, 1:2], sq[:, 0:1], 1.0 / D, EPS, ALU.mult, ALU.add, [tsq], [tsq])
            act(sq[:, 2:3], sq[:, 1:2], AF.Sqrt, [tsq], [tsq])
            P.op("dve", lambda e, sq=sq: e.reciprocal(out=sq[:, 3:4], in_=sq[:, 2:3]), [tsq], [tsq])
            stt("dve", xn_, xt, sq[:, 3:4], A2r, ALU.mult, ALU.mult, [txt, tsq, tg("rowsC")], [txn])
            tt("pool", vtok_all[:, t, :], xn_, SH2r, ALU.add, [txn, tg("rowsC")], [tvt])
            pb, pbt = psum()
            pbb = pb[:].bitcast(BF16)
            for kc in range(8):
                tr(pbb[:, kc * 128:(kc + 1) * 128], vtok_all[:, t, kc * 128:(kc + 1) * 128], ident[:], [tvt, tC], [pbt])
            cp("act", vTt, pbb.rearrange("p (a b) -> p a b", a=8), [pbt], [tvTt])
            pl, plt = psum()
            for k in range(8):
                mm(pl[:, 0:32], vTt[:, k, :], rwb[:, k, :], k == 0, k == 7, [tvTt, tg("rwb")], [plt])
            return pl, plt

        def rA2(t, pl, plt):
            rt_ = rt_l[t % 2]
            trt_, trA_ = tg(f"rt{t % 2}"), tg(f"routeA{t}")
            lg, m8, wr = lg_all[:, t, :], m8_all[:, t, :], wr_all[:, t, :]
            ex, msk = rt_[:, 0, :], rt_[:, 1, :]
            sc1 = rt_[:, 2, 0:4]
            tt("dve", lg, pl[:, 0:32], Rr("rb"), ALU.add, [plt, tC], [trA_])
            P.op("dve", lambda e, m8=m8, lg=lg: e.max(out=m8, in_=lg), [trA_], [trA_])
            ts("dve", sc1[:, 0:1], m8[:, 0:1], -1.0, None, ALU.mult, None, [trA_], [trt_])
            ts("dve", msk, lg, m8[:, 3:4], None, ALU.is_ge, None, [trA_], [trt_])
            act(ex, lg, AF.Exp, [trA_, trt_], [trt_], bias=sc1[:, 0:1])
            tt("dve", ex, ex, msk, ALU.mult, [trt_], [trt_])
            P.op("dve", lambda e, ex=ex, sc1=sc1: e.tensor_reduce(out=sc1[:, 1:2], in_=ex, axis=AX.X, op=ALU.add), [trt_], [trt_])
            P.op("dve", lambda e, sc1=sc1: e.reciprocal(out=sc1[:, 2:3], in_=sc1[:, 1:2]), [trt_], [trt_])
            ts("dve", wr, ex, sc1[:, 2:3], None, ALU.mult, None, [trt_], [trA_])
            pp, ppt = psum()
            mm(pp[:, 0:32], ustr_b[:], msk, True, True, [tC, trt_], [ppt])
            mm(pp[:, 32:64], onesf[:], msk, True, True, [tC, trt_], [ppt])
            tt("dve", sl_all[:, t, :], pp[:, 0:32], base, ALU.add, [ppt, tbase], [trA_])
            tt("dve", base, base, pp[:, 32:64], ALU.add, [ppt, tbase], [tbase])

        cur = rA1(0)
        for t in range(NCH):
            nxt = rA1(t + 1) if t + 1 < NCH else None
            rA2(t, *cur)
            cur = nxt
        trt = tg("rt0")
        tpb = tg("padblk")
        cnt3 = big[:, 0:32, 0:16]
        tt("dve", cnt3, base.unsqueeze(2).to_broadcast([128, 32, 16]), Rr("thr16").unsqueeze(1).to_broadcast([128, 32, 16]), ALU.is_gt,
           [tbase, tC], [tpb])
        padded, pend, ptmp, pstart = rt[:, 3, :], rt[:, 4, :], rt[:, 5, :], rt[:, 6, :]
        P.op("dve", lambda e: e.tensor_reduce(out=padded, in_=cnt3, axis=AX.X, op=ALU.add), [tpb], [trt])
        ts("dve", padded, padded, float(BS), None, ALU.mult, None, [trt], [trt])
        cp("dve", pend, padded, [trt], [trt])
        for sh in (1, 2, 4, 8, 16):
            cp("dve", ptmp, pend, [trt], [trt])
            tt("dve", pend[:, sh:32], ptmp[:, sh:32], ptmp[:, 0:32 - sh], ALU.add, [trt], [trt])
        tt("dve", pstart, pend, padded, ALU.subtract, [trt], [trt])
        tt("dve", big, pend.unsqueeze(1).to_broadcast([128, NBLK, 32]), Rr("bstart").unsqueeze(2).to_broadcast([128, NBLK, 32]), ALU.is_le,
           [trt, tC, tpb], [tpb])
        P.op("dve", lambda e: e.tensor_reduce(out=bexp, in_=big, axis=AX.X, op=ALU.add), [tpb], [tpb])
        ts("dve", bexp, bexp, 31.0, None, ALU.min, None, [tpb], [tpb])
        ts("dve", idxw_f, bexp, 128.0, S("pidx", 0, 1), ALU.mult, ALU.add, [tpb, tC], [tpb])
        cp("dve", idxw_i[:, :], idxw_f, [tpb], [tg("idxw")])
        cp("dve", bexp_i[:, :], bexp, [tpb], [tg("idxw")])
        allA = [tg(f"routeA{t}") for t in range(NCH)]
        oh4 = carve([128, NCH, 4, 32], F32)
        tmp4 = carve([128, NCH, 4, 32], F32)
        slot_all = carve([128, NCH, 32], F32)
        t4 = tg("rc4")
        shp = [128, NCH, 4, 32]
        tt("dve", slot_all, sl_all, pstart.unsqueeze(1).to_broadcast([128, NCH, 32]), ALU.add, allA + [trt], [t4])
        tt("dve", oh4, lg_all.unsqueeze(2).to_broadcast(shp), m8_all[:, :, 0:4].unsqueeze(3).to_broadcast(shp), ALU.is_equal, allA, [t4])
        tt("dve", tmp4, oh4, slot_all.unsqueeze(2).to_broadcast(shp), ALU.mult, [t4], [t4])
        P.op("dve", lambda e: e.tensor_reduce(out=idx_f, in_=tmp4, axis=AX.X, op=ALU.add), [t4], [twk])
        tt("dve", tmp4, oh4, wr_all.unsqueeze(2).to_broadcast(shp), ALU.mult, [t4, twk] + allA, [t4])
        P.op("dve", lambda e: e.tensor_reduce(out=wk_all, in_=tmp4, axis=AX.X, op=ALU.add), [t4], [twk])
        cp("dve", idx_i[:, :].rearrange("p (t k) -> p t k", k=4), idx_f, [twk], [twk])
        for t in range(NCH):
            for k in range(4):
                P.dma("pool", lambda e, t=t, k=k: e.indirect_dma_start(
                    out=vbuf_d[:, :], out_offset=bass.IndirectOffsetOnAxis(ap=idx_i[:, t * 4 + k:t * 4 + k + 1], axis=0),
                    in_=vtok_all[:, t, :], in_offset=None, bounds_check=None), [tg(f"vtok{t}"), twk], [tg("d_vbuf")])
        dbg("wk", dt_own[:], [128, NCH, 64], F32, [twk])
        dbg("bexp", bexp, [128, NBLK], F32, [tpb])
        new_phase()
        wg_l = [uT[:].rearrange("p a b -> p (a b)")[:, 0:16 * D].rearrange("p (a b) -> p a b", a=8), carve([128, 8, 2 * D], BF16)]
        wd_l = [carve([128, 8, D], BF16) for _ in range(2)]
        bg_l = [carve([128, 16], F32) for _ in range(2)]
        bd_l = [carve([128, D], F32) for _ in range(2)]
        NT = BS // 128
        xin = carve([128, NT, D], BF16)
        actT = carve([128, 8, BS], BF16)
        gcb = [carve([128, BS], BF16) for _ in range(2)]
        sgb = [carve([128, BS], BF16) for _ in range(2)]
        ucb = [carve([128, BS], BF16) for _ in range(2)]
        yst = [carve([128, D], F32) for _ in range(2)]
        xT_l = [carve([128, 8, BS], BF16) for _ in range(2)]
        ei = 0
        yi = 0

        def blk_bufs(i):
            b2 = i % 2
            return (wg_l[b2], wd_l[b2], bg_l[b2], bd_l[b2], xT_l[b2],
                    (tuT if b2 == 0 else tg("wg1")), tg(f"wd{b2}"), tg(f"bg{b2}"), tg(f"bd{b2}"), tg(f"xT{b2}"))

        def blk_load(i):
            wg, wd, bg, bd, xTb, twg, twd, tbg, tbd, txT = blk_bufs(i)
            iw = idxw_i[:, i:i + 1]
            P.dma("pool", lambda e, wg=wg, iw=iw: e.indirect_dma_start(
                out=wg.rearrange("p a b -> p (a b)"), out_offset=None, in_=wgu_bf[:, :], in_offset=bass.IndirectOffsetOnAxis(ap=iw, axis=0),
                bounds_check=None), [tg("d_wbf"), tg("idxw")], [twg])
            P.dma("pool", lambda e, wd=wd, iw=iw: e.indirect_dma_start(
                out=wd.rearrange("p a b -> p (a b)"), out_offset=None, in_=wdn_bf[:, :], in_offset=bass.IndirectOffsetOnAxis(ap=iw, axis=0),
                bounds_check=None), [tg("d_wbf"), tg("idxw")], [twd])
            P.dma("pool", lambda e, bg=bg, iw=iw: e.indirect_dma_start(
                out=bg[:, :], out_offset=None, in_=bgu_tab[:, :], in_offset=bass.IndirectOffsetOnAxis(ap=iw, axis=0),
                bounds_check=None), [tg("idxw")], [tbg])
            P.dma("pool", lambda e, bd=bd, i=i: e.indirect_dma_start(
                out=bd[:, :], out_offset=None, in_=b_down[0], in_offset=bass.IndirectOffsetOnAxis(ap=bexp_i[:, i:i + 1], axis=0),
                bounds_check=None), [tg("idxw")], [tbd])
            dma("sp", xin, vbuf_d[i * BS:(i + 1) * BS, :].rearrange("(a p) d -> p a d", p=128), [tg("d_vbuf")], [tg("xin")])

        def blk_prep(i):
            wg, wd, bg, bd, xTb, twg, twd, tbg, tbd, txT = blk_bufs(i)
            for a in range(NT):
                pb, pbt = psum()
                pbb = pb[:].bitcast(BF16)
                for kc in range(8):
                    tr(pbb[:, kc * 128:(kc + 1) * 128], xin[:, a, kc * 128:(kc + 1) * 128], ident[:], [tg("xin"), tC], [pbt])
                cp("act", xTb[:, :, a * 128:(a + 1) * 128], pbb.rearrange("p (a b) -> p a b", a=8), [pbt], [txT])

        def blk_gu(i):
            nonlocal ei
            wg, wd, bg, bd, xTb, twg, twd, tbg, tbd, txT = blk_bufs(i)
            for j in range(8):
                pg, pgt = psum()
                for k in range(8):
                    mm(pg[:, 0:BS], wg[:, k, j * 128:(j + 1) * 128], xTb[:, k, :], k == 0, k == 7, [twg, txT], [pgt])
                for k in range(8):
                    mm(pg[:, BS:2 * BS], wg[:, k, D + j * 128:D + (j + 1) * 128], xTb[:, k, :], k == 0, k == 7, [twg, txT], [pgt], skip=True)
                i2 = ei % 2
                ei += 1
                g_, s_, u_ = gcb[i2], sgb[i2], ucb[i2]
                tg_, ts_, tu_ = tg(f"m_g{i2}"), tg(f"m_s{i2}"), tg(f"m_u{i2}")
                ts("dve", g_, pg[:, 0:BS], bg[:, j:j + 1], 7.0, ALU.add, ALU.min, [pgt, tbg], [tg_])
                act(s_, g_, AF.Sigmoid, [tg_], [ts_], scale=1.702)
                tt("dve", s_, g_, s_, ALU.mult, [tg_, ts_], [ts_])
                ts("dve", u_, pg[:, BS:2 * BS], bg[:, 8 + j:9 + j], 7.0, ALU.add, ALU.min, [pgt, tbg], [tu_])
                ts("dve", u_, u_, -7.0, 1.0, ALU.max, ALU.add, [tu_], [tu_])
                tt("dve", actT[:, j, :], u_, s_, ALU.mult, [tu_, ts_], [tg("actT")])

        def blk_down(i):
            nonlocal yi
            wg, wd, bg, bd, xTb, twg, twd, tbg, tbd, txT = blk_bufs(i)
            for a in range(NT):
                ys_, tys2 = yst[yi % 2], tg(f"yst{yi % 2}")
                yi += 1
                for half in range(2):
                    sl = slice(half * 512, (half + 1) * 512)
                    pb, pbt = psum()
                    for k in range(8):
                        mm(pb[:, :], actT[:, k, a * 128:(a + 1) * 128], wd[:, k, sl], k == 0, k == 7, [tg("actT"), twd], [pbt])
                    tt("dve", ys_[:, sl], pb[:, :], bd[:, sl], ALU.add, [pbt, tbd], [tys2])
                dma("sp", ybuf_d[i * BS + a * 128:i * BS + (a + 1) * 128, :], ys_, [tys2], [tg("d_ybuf")])

        blk_load(0)
        blk_prep(0)
        for i in range(NBLK):
            if i + 1 < NBLK:
                blk_load(i + 1)
            blk_gu(i)
            if i + 1 < NBLK:
                blk_prep(i + 1)
            blk_down(i)
        new_phase()
        yk_l = [[carve([128, D], F32) for _ in range(4)] for _ in range(2)]
        x1l = [carve([128, D], F32) for _ in range(2)]
        o8 = [carve([128, D], F32) for _ in range(2)]
        j8 = carve([128, D], BF16)
        s8 = [carve([128, 4], F32) for _ in range(2)]
        for t in range(NCH):
            xt_, ot, sq, yks = x1l[t % 2], o8[t % 2], s8[t % 2], yk_l[t % 2]
            txt, tot_, tsq = tg(f"p8_x{t % 2}"), tg(f"p8_o{t % 2}"), tg(f"p8_s{t % 2}")
            dma("sp", xt_, x1_d[t * 128:(t + 1) * 128, :], [tg("d_x1")], [txt])
            for k in range(4):
                P.dma("pool", lambda e, t=t, k=k, yk=yks[k]: e.indirect_dma_start(
                    out=yk[:, :], out_offset=None, in_=ybuf_d[:, :], in_offset=bass.IndirectOffsetOnAxis(ap=idx_i[:, t * 4 + k:t * 4 + k + 1], axis=0),
                    bounds_check=None), [tg("d_ybuf"), twk], [tg(f"p8_yk{t % 2}_{k}")])
            ts("dve", ot, yks[0], wk_all[:, t, 0:1], None, ALU.mult, None, [tg(f"p8_yk{t % 2}_0"), twk], [tot_])
            for k in range(1, 4):
                stt("dve", ot, yks[k], wk_all[:, t, k:k + 1], ot, ALU.mult, ALU.add, [tg(f"p8_yk{t % 2}_{k}"), twk, tot_], [tot_])
            tt("dve", ot, ot, G2r, ALU.mult, [tot_, tg("rowsC")], [tot_])
            tt("dve", xt_, xt_, ot, ALU.add, [txt, tot_], [txt])
            act(j8, xt_, AF.Square, [txt], [tg("p8_j"), tsq], accum=sq[:, 0:1])
            ts("dve", sq[:, 1:2], sq[:, 0:1], 1.0 / D, EPS, ALU.mult, ALU.add, [tsq], [tsq])
            act(sq[:, 2:3], sq[:, 1:2], AF.Sqrt, [tsq], [tsq])
            P.op("dve", lambda e, sq=sq: e.reciprocal(out=sq[:, 3:4], in_=sq[:, 2:3]), [tsq], [tsq])
            stt("dve", ot, xt_, sq[:, 3:4], Afr, ALU.mult, ALU.mult, [txt, tsq, tg("rowsC")], [tot_])
            tt("dve", ot, ot, SHfr, ALU.add, [tot_, tg("rowsC")], [tot_])
            dma("sp", out_d[t * 128:(t + 1) * 128, :], ot, [tot_], [tg("d_out")])
        P.finalize(nc, st)
    return nc


def _pp(v, n):
    return np.ascontiguousarray(np.asarray(v, np.float32).reshape(n, 128).T)


def make_in_maps(inp):
    f = lambda k: np.asarray(inp[k], np.float32)
    x, c, w_in = f("x"), f("c"), f("w_in")
    conv_w, conv_b = f("ssm_conv_w")[0], f("ssm_conv_b")[0]
    shared = {k: np.ascontiguousarray(f(k)) for k in ("ada_w", "final_ada_w", "w_in", "conf_out_w", "ssm_out_w", "w_o",
                                                       "router_w", "w_gu", "w_down", "b_down")}
    shared["bgu_tab"] = np.ascontiguousarray(f("b_gu")[0].reshape(32, 16, 128).transpose(0, 2, 1).reshape(32 * 128, 16))
    dtb = (f("dt_bias_f")[0], f("dt_bias_b")[0])
    alog = (f("a_log_f")[0], f("a_log_b")[0])
    wdt = (w_in[0][:, C_DTF:C_DTF + 32], w_in[0][:, C_DTB:C_DTB + 32])
    maps = []
    for j in range(8):
        b, s = j // 4, j % 4
        xb = x[b]
        L = xb.shape[0]

        def rows_of(lo, hi):
            o = np.zeros((hi - lo, D), np.float32)
            a, e = max(lo, 0), min(hi, L)
            o[a - lo:e - lo] = xb[a:e]
            return o
        xo = rows_of(T * s - HALO, T * s + T + HALO)
        slots = [(q, 0) for q in range(s)] + [(q, 1) for q in range(3, s, -1)]
        xs3 = np.zeros((3, TS, D), np.float32)
        m3 = np.zeros((3, 4), np.float32)
        wdt3 = np.zeros((3, D, 32), np.float32)
        dtb3 = np.zeros((3, 32), np.float32)
        alog3 = np.zeros((3, 32), np.float32)
        cw3 = np.zeros((128, 3, 20, 5), np.float32)
        for k, (q, d) in enumerate(slots):
            r = rows_of(T * q - 2, T * q + T + 2)
            v = np.array([T * q - 2 >= 0, T * q - 1 >= 0, T * q + T < L, T * q + T + 1 < L], np.float32)
            cw = conv_w[:, :2560]
            if d == 1:
                r, v, cw = r[::-1], v[::-1], cw[::-1]
            xs3[k], m3[k] = r, v
            wdt3[k], dtb3[k], alog3[k] = wdt[d], dtb[d], alog[d]
            cw3[:, k] = cw.T.reshape(20, 128, 5).transpose(1, 0, 2)
        keep = [0.0 if (k == 0 or k == s) else 1.0 for k in range(3)]
        selF = [1.0 if (s >= 1 and k == s - 1) else 0.0 for k in range(3)]
        selB = [1.0 if (s <= 2 and k == 2) else 0.0 for k in range(3)]
        small = np.zeros((128, NS), np.float32)

        def put(name, arr):
            o, w = SM[name]
            small[:, o:o + w] = np.asarray(arr, np.float32).reshape(-1, w) if np.ndim(arr) > 1 else np.tile(np.asarray(arr, np.float32), (128, 1))
        put("cT", _pp(c[b], 8))
        put("adab", _pp(f("ada_b")[0], 48))
        put("finb", _pp(f("final_ada_b"), 16))
        put("gmix", _pp(f("norm_mix_g")[0], 8))
        put("gffn", _pp(f("norm_ffn_g")[0], 8))
        put("gfin", _pp(f("final_norm_g"), 8))
        put("mown", np.concatenate([np.full(16, 1.0 if s > 0 else 0.0), np.full(16, 1.0 if s < 3 else 0.0)]))
        put("m3", m3.reshape(-1))
        put("flg", np.array(keep + selF + selB))
        put("dw", f("conf_dw_w")[0].T.reshape(8, 128, 31).transpose(1, 0, 2).reshape(128, 248))
        put("dwb", _pp(f("conf_dw_b")[0], 8))
        put("lng", _pp(f("conf_ln_g")[0], 8))
        put("lnb", _pp(f("conf_ln_b")[0], 8))
        put("cw", conv_w.T.reshape(24, 128, 5).transpose(1, 0, 2).reshape(128, 120))
        put("cb", _pp(conv_b, 24))
        put("cw3", cw3.reshape(128, 300))
        put("pidx", np.arange(128, dtype=np.float32).reshape(128, 1))
        put("sng", _pp(f("ssm_norm_g")[0], 16))
        rows = np.zeros((1, NR), np.float32)

        def putr(name, arr):
            o, w = RW[name]
            rows[0, o:o + w] = np.asarray(arr, np.float32).reshape(-1)
        putr("cob", f("conf_out_b")[0])
        putr("rb", f("router_b")[0])
        putr("dtb", np.concatenate(dtb))
        putr("alog", np.concatenate(alog))
        putr("ssd", f("ssm_d")[0])
        putr("dtb3", dtb3)
        putr("alog3", alog3)
        putr("bstart", np.arange(NBLK, dtype=np.float32) * BS)
        putr("thr16", np.arange(16, dtype=np.float32) * BS)
        m = dict(shared)
        m.update(xo=xo, xs3=np.ascontiguousarray(xs3), small=small, rows=rows, wdt3=wdt3)
        maps.append(m)
    return maps


_NC_CACHE = {}


def kernel(**inputs):
    if "nc" not in _NC_CACHE:
        _NC_CACHE["nc"] = build()
    nc = _NC_CACHE["nc"]
    maps = make_in_maps(inputs)
    res = run_bass_kernel_spmd(nc, maps, core_ids=list(range(8)))
    out = np.zeros((2, 4 * T, D), np.float32)
    for j in range(8):
        out[j // 4, (j % 4) * T:(j % 4 + 1) * T] = res.results[j]["out"]
    return out
```

```python
import numpy as np
from contextlib import ExitStack
import concourse.bass as bass
import concourse.mybir as mybir
from concourse.bass_utils import run_bass_kernel_spmd

F32 = mybir.dt.float32
BF16 = mybir.dt.bfloat16
I32 = mybir.dt.int32
AF = mybir.ActivationFunctionType
ALU = mybir.AluOpType
AX = mybir.AxisListType

ENGS = ("pe", "act", "dve", "pool", "sp")
NDMA = 12

D = 1024
T = 2048
HALO = 16
TH = T + 2 * HALO
TS = T + 4
NCH = 16
EPS = 1e-6
NEXP = 32
C_A, C_G, C_Z, C_XS, C_B, C_C, C_DTF, C_DTB, C_GC, C_GS = 0, 1024, 2048, 4096, 6144, 6656, 7168, 7200, 7232, 8256


class Tag:
    __slots__ = ("name", "w", "r")

    def __init__(self, name=""):
        self.name = name
        self.w = None
        self.r = {}


class Prog:
    def __init__(self):
        self.ops = {e: [] for e in ENGS}
        self.cnt = {e: 0 for e in ENGS}
        self.seen = {e: {} for e in ENGS}
        self.dma_i = {"sp": 0, "pool": 0, "act": 0}
        self.dma_uses = {}
        self.fence_toks = {}

    def fence(self):
        f = {}
        for e in ENGS:
            if e != "sp" and self.cnt[e] > 0:
                f[("e", e)] = self.cnt[e]
        for k, u in self.dma_uses.items():
            f[k] = 16 * u
        self.fence_toks = f

    def _deps(self, eng, reads, writes):
        deps = dict(self.fence_toks)

        def add(k, v):
            if deps.get(k, 0) < v:
                deps[k] = v
        for t in reads:
            if t.w is not None:
                add(*t.w)
        for t in writes:
            if t.w is not None:
                add(*t.w)
            for k, v in t.r.items():
                add(k, v)
        waits = []
        for k, v in deps.items():
            if k == ("e", "pe") and eng == "pe":
                continue
            if self.seen[eng].get(k, 0) >= v:
                continue
            self.seen[eng][k] = v
            waits.append((k, v))
        return waits

    def _mark(self, tok, reads, writes):
        for t in reads:
            if t.r.get(tok[0], 0) < tok[1]:
                t.r[tok[0]] = tok[1]
        for t in writes:
            t.w = tok
            t.r = {}

    def op(self, eng, fn, reads=(), writes=()):
        waits = self._deps(eng, reads, writes)
        self.cnt[eng] += 1
        tok = (("e", eng), self.cnt[eng])
        self.ops[eng].append((waits, fn, (("e", eng), 1)))
        self._mark(tok, reads, writes)

    def dma(self, q, fn, reads=(), writes=()):
        waits = self._deps(q, reads, writes)
        i = self.dma_i[q]
        self.dma_i[q] += 1
        key = ("d", q, i % NDMA)
        uses = self.dma_uses.get(key, 0)
        if uses > 0 and self.seen[q].get(key, 0) < 16 * uses:
            waits.append((key, 16 * uses))
            self.seen[q][key] = 16 * uses
        self.dma_uses[key] = uses + 1
        tok = (key, 16 * (uses + 1))
        self.ops[q].append((waits, fn, (key, 16)))
        self._mark(tok, reads, writes)

    def finalize(self, nc, stack):
        keys = [("e", e) for e in ENGS if e != "sp"]
        for q in ("sp", "pool", "act"):
            for s in range(NDMA):
                keys.append(("d", q, s))
        sems = {k: stack.enter_context(nc.semaphore("s_" + "_".join(str(x) for x in k))) for k in keys}
        fin = [(k, 16 * u) for k, u in self.dma_uses.items()]
        fin += [(("e", e), self.cnt[e]) for e in ENGS if e != "sp" and self.cnt[e] > 0]
        ops = self.ops

        def replay(name, eng, final=False):
            for waits, fn, inc in ops[name]:
                for k, v in waits:
                    eng.wait_ge(sems[k], v)
                fn(eng).then_inc(sems[inc[0]], inc[1])
            if final:
                for k, v in fin:
                    eng.wait_ge(sems[k], v)

        with nc.Block() as block:
            @block.tensor
            def _(e):
                replay("pe", e)

            @block.scalar
            def _(e):
                replay("act", e)

            @block.vector
            def _(e):
                replay("dve", e)

            @block.gpsimd
            def _(e):
                replay("pool", e)

            @block.sync
            def _(e):
                replay("sp", e, final=True)


def _layout(items):
    off, o = {}, 0
    for n, w in items:
        off[n] = (o, w)
        o += w
    return off, o


SM, NS = _layout([("cT", 8), ("adab", 48), ("finb", 16), ("gmix", 8), ("gffn", 8), ("gfin", 8), ("mown", 32),
                  ("m3", 12), ("flg", 9), ("dw", 248), ("dwb", 8), ("lng", 8), ("lnb", 8), ("cw", 120), ("cb", 24),
                  ("cw3", 300), ("sng", 16), ("pidx", 1)])
BS = 256
NBLK = 8192 // BS + 32
RW, NR = _layout([("cob", 1024), ("rb", 32), ("dtb", 64), ("alog", 64), ("ssd", 32), ("dtb3", 96), ("alog3", 96), ("bstart", NBLK), ("thr16", 16)])


def build(upto=99, debug=False):
    nc = bass.Bass("TRN2", target_bir_lowering=False)
    P = Prog()

    def din(name, shape, dt=F32):
        return nc.dram_tensor(name, list(shape), dt, kind="ExternalInput").ap()

    def dscr(name, shape, dt):
        return nc.dram_tensor(name, list(shape), dt, kind="ExternalOutput" if debug else "Internal").ap()

    dbg_outs = {}

    def dbg(name, ap_sb, shape, dt, reads):
        if not debug:
            return
        d = nc.dram_tensor("dbg_" + name, list(shape), dt, kind="ExternalOutput").ap()
        dbg_outs[name] = d
        P.dma("sp", lambda e: e.dma_start(out=d, in_=ap_sb), reads, [Tag("dbgd")])

    xo = din("xo", [TH, D])
    xs3 = din("xs3", [3, TS, D])
    small_d = din("small", [128, NS])
    rows_d = din("rows", [1, NR])
    ada_w = din("ada_w", [1, D, 6 * D])
    fada_w = din("final_ada_w", [D, 2 * D])
    w_in = din("w_in", [1, D, 9280])
    wdt3 = din("wdt3", [3, D, 32])
    conf_out_w = din("conf_out_w", [1, D, D])
    ssm_out_w = din("ssm_out_w", [1, 2 * D, D])
    w_o = din("w_o", [1, D, D])
    router_w = din("router_w", [1, D, NEXP])
    w_gu = din("w_gu", [1, NEXP, D, 2 * D])
    w_down = din("w_down", [1, NEXP, D, D])
    b_down = din("b_down", [1, NEXP, D])
    bgu_tab = din("bgu_tab", [NEXP * 128, 16])
    out_d = nc.dram_tensor("out", [T, D], F32, kind="ExternalOutput").ap()

    xs_tok = [dscr(f"xs_tok{i}", [T, 2048], BF16) for i in range(4)]
    b_tok = [dscr(f"b_tok{i}", [T, 512], BF16) for i in range(4)]
    bT_d = dscr("bT", [512, T], BF16)
    cT_d = dscr("cTd", [512, T], BF16)
    hF_d = dscr("hF", [NCH, 128, 2048], BF16)
    hB_d = dscr("hB", [NCH, 128, 2048], BF16)
    yconf_d = dscr("yconf", [T, D], BF16)
    ysn_d = dscr("ysn", [T, 2048], BF16)
    x1_d = dscr("x1", [T, D], F32)
    vbuf_d = dscr("vbuf", [NBLK * BS, D], BF16)
    ybuf_d = dscr("ybuf", [NBLK * BS, D], F32)
    wgu_bf = nc.dram_tensor("wgu_bf", [NEXP * 128, 8 * 2 * D], BF16, kind="Internal").ap()
    wdn_bf = nc.dram_tensor("wdn_bf", [NEXP * 128, 8 * D], BF16, kind="Internal").ap()

    with ExitStack() as st:
        def sb(name, shape, dt):
            return st.enter_context(nc.sbuf_tensor("sb_" + name, list(shape), dt))

        tags = {}

        def tg(name):
            if name not in tags:
                tags[name] = Tag(name)
            return tags[name]

        banks = [st.enter_context(nc.psum_tensor(f"ps{i}", [128, 512], F32)) for i in range(8)]
        bank_i = [0]

        bank_rng = [0, 8]

        def psum():
            lo, hi = bank_rng
            i = lo + bank_i[0] % (hi - lo)
            bank_i[0] += 1
            return banks[i], tg(f"ps{i}")

        small = sb("small", [128, NS], F32)
        rows = sb("rows", [128, NR], F32)
        identf = sb("identf", [128, 128], F32)
        ident = sb("ident", [128, 128], BF16)
        onesf = sb("onesf", [128, 128], F32)
        onesb = sb("onesb", [128, 128], BF16)
        tri_f = sb("tri_f", [128, 128], F32)
        tri_b = sb("tri_b", [128, 128], F32)
        ustr_f = sb("ustr_f", [128, 128], F32)
        ustr_b = sb("ustr_b", [128, 128], F32)
        mk_f = sb("mk_f", [128, 128], BF16)
        mk_b = sb("mk_b", [128, 128], BF16)
        adaP = sb("adaP", [128, 64], F32)
        modP = sb("modP", [128, 24], F32)
        rowsC = sb("rowsC", [128, 6, D], F32)
        uT = sb("uT", [128, 8, TH], BF16)
        wbuf = [sb(f"wbuf{i}", [128, 8, 512], BF16) for i in range(3)]
        wbuf_i = [0]
        ARENA = 101 * 1024
        arena = sb("arena", [128, ARENA // 4], F32)
        ar_off = [0]

        def carve(shape, dt):
            n = int(np.prod(shape[1:]))
            nb = n * (4 if dt == F32 else 2)
            nb = (nb + 63) // 64 * 64
            o = ar_off[0]
            assert o + nb <= ARENA, (o, nb, ARENA)
            ar_off[0] = o + nb
            ap = arena[0:shape[0], o // 4:(o + nb) // 4]
            if dt != F32:
                ap = ap.bitcast(dt)
            ap = ap[:, 0:n]
            if len(shape) == 3:
                ap = ap.rearrange("p (a b) -> p a b", a=shape[1])
            elif len(shape) == 4:
                ap = ap.rearrange("p (a b c) -> p a b c", a=shape[1], b=shape[2])
            return ap

        def new_phase():
            ar_off[0] = 0
            P.fence()

        def S(name, i=0, n=1):
            o, w = SM[name]
            return small[:, o + i:o + i + n]

        def Rr(name, i=0, n=None):
            o, w = RW[name]
            n = w - i if n is None else n
            return rows[:, o + i:o + i + n]

        def mm(out, lhsT, rhs, start, stop, reads, writes, skip=False):
            P.op("pe", lambda e: e.matmul(out, lhsT=lhsT, rhs=rhs, start=start, stop=stop, skip_group_check=skip), reads, writes)

        def tr(out, in_, idt, reads, writes):
            P.op("pe", lambda e: e.transpose(out=out, in_=in_, identity=idt), reads, writes)

        def act(out, in_, func, reads, writes, bias=None, scale=None, accum=None):
            kw = {}
            if bias is not None:
                kw["bias"] = bias
            if scale is not None:
                kw["scale"] = scale
            if accum is not None:
                kw["accum_out"] = accum
            P.op("act", lambda e: e.activation(out=out, in_=in_, func=func, **kw), reads, writes)

        def tt(eng, out, in0, in1, op, reads, writes):
            P.op(eng, lambda e: e.tensor_tensor(out=out, in0=in0, in1=in1, op=op), reads, writes)

        def ts(eng, out, in0, s1, s2, op0, op1, reads, writes):
            if op1 is None:
                P.op(eng, lambda e: e.tensor_scalar(out=out, in0=in0, scalar1=s1, scalar2=None, op0=op0), reads, writes)
            else:
                P.op(eng, lambda e: e.tensor_scalar(out=out, in0=in0, scalar1=s1, scalar2=s2, op0=op0, op1=op1), reads, writes)

        def stt(eng, out, in0, sc, in1, op0, op1, reads, writes):
            P.op(eng, lambda e: e.scalar_tensor_tensor(out=out, in0=in0, scalar=sc, in1=in1, op0=op0, op1=op1), reads, writes)

        def cp(eng, out, in_, reads, writes):
            if eng == "act":
                P.op("act", lambda e: e.activation(out=out, in_=in_, func=AF.Copy), reads, writes)
            else:
                P.op(eng, lambda e: e.tensor_copy(out=out, in_=in_), reads, writes)

        def dma(q, out, in_, reads, writes):
            P.dma(q, lambda e: e.dma_start(out=out, in_=in_), reads, writes)

        def load_w(src, ncols=512):
            i = wbuf_i[0] % 3
            wbuf_i[0] += 1
            t = tg(f"wbuf{i}")
            dma("pool", wbuf[i][:, :, 0:ncols], src.rearrange("(kc p) n -> p kc n", p=128), [], [t])
            return wbuf[i], t

        tC = tg("const")
        dma("sp", small[:], small_d, [], [tC])
        dma("sp", rows[:], rows_d.partition_broadcast(128), [], [tC])
        P.op("pool", lambda e: e.memset(identf[:], 0.0), [], [tC])
        P.op("pool", lambda e: e.affine_select(out=identf[:], in_=identf[:], pattern=[[-1, 128]], compare_op=ALU.not_equal,
                                               fill=1.0, base=0, channel_multiplier=1), [tC], [tC])
        cp("pool", ident[:], identf[:], [tC], [tC])
        P.op("pool", lambda e: e.memset(onesf[:], 1.0), [], [tC])
        cp("pool", onesb[:], onesf[:], [tC], [tC])
        P.op("pool", lambda e: e.affine_select(out=tri_f[:], in_=onesf[:], pattern=[[1, 128]], compare_op=ALU.is_ge,
                                               fill=0.0, base=0, channel_multiplier=-1), [tC], [tC])
        P.op("pool", lambda e: e.affine_select(out=tri_b[:], in_=onesf[:], pattern=[[-1, 128]], compare_op=ALU.is_ge,
                                               fill=0.0, base=0, channel_multiplier=1), [tC], [tC])
        P.op("pool", lambda e: e.affine_select(out=ustr_f[:], in_=onesf[:], pattern=[[-1, 128]], compare_op=ALU.is_gt,
                                               fill=0.0, base=0, channel_multiplier=1), [tC], [tC])
        P.op("pool", lambda e: e.affine_select(out=ustr_b[:], in_=onesf[:], pattern=[[1, 128]], compare_op=ALU.is_gt,
                                               fill=0.0, base=0, channel_multiplier=-1), [tC], [tC])
        cp("pool", mk_f[:], tri_f[:], [tC], [tC])
        cp("pool", mk_b[:], tri_b[:], [tC], [tC])

        new_phase()
        cact = carve([128, 8], BF16)
        wA = [carve([128, 8, 512], BF16) for _ in range(3)]
        tA = tg("adaP")
        act(cact, S("cT", 0, 8), AF.Silu, [tC], [tg("cact")])
        pa, pat = psum()
        for blk in range(16):
            src = ada_w[0][:, blk * 512:(blk + 1) * 512] if blk < 12 else fada_w[:, (blk - 12) * 512:(blk - 11) * 512]
            wt = tg(f"wA{blk % 3}")
            dma("pool", wA[blk % 3], src.rearrange("(kc p) n -> p kc n", p=128), [], [wt])
            for cc in range(4):
                col = blk * 4 + cc
                for kc in range(8):
                    mm(pa[:, col:col + 1], wA[blk % 3][:, kc, cc * 128:(cc + 1) * 128], cact[:, kc:kc + 1], kc == 0, kc == 7,
                       [wt, tg("cact")], [pat])
        tt("dve", adaP[:, 0:48], pa[:, 0:48], S("adab", 0, 48), ALU.add, [pat, tC], [tA])
        tt("dve", adaP[:, 48:64], pa[:, 48:64], S("finb", 0, 16), ALU.add, [pat, tC], [tA])
        for i, (gname, c0) in enumerate((("gmix", 8), ("gffn", 32), ("gfin", 56))):
            stt("dve", modP[:, i * 8:(i + 1) * 8], adaP[:, c0:c0 + 8], 1.0, S(gname, 0, 8), ALU.add, ALU.mult, [tA, tC], [tg("modP")])
        A1, A2, Af = modP[:, 0:8], modP[:, 8:16], modP[:, 16:24]
        sh1, sh2 = adaP[:, 0:8], adaP[:, 24:32]
        dg = carve([128, 128], F32)
        for ri, vec in enumerate((adaP[:, 16:24], adaP[:, 40:48], Af, adaP[:, 48:56], A2, sh2)):
            for half in range(2):
                pr, prt = psum()
                for q in range(4):
                    kc = half * 4 + q
                    ts("dve", dg, identf[:], vec[:, kc:kc + 1], None, ALU.mult, None, [tC, tA, tg("modP")], [tg("dg")])
                    mm(pr[:, q * 128:(q + 1) * 128], onesf[:], dg, True, True, [tC, tg("dg")], [prt])
                cp("act", rowsC[:, ri, half * 512:(half + 1) * 512], pr[:, :], [prt], [tg("rowsC")])
        G1r, G2r, Afr, SHfr, A2r, SH2r = (rowsC[:, i, :] for i in range(6))
        if upto <= 0:
            dbg("adaP", adaP[:], [128, 64], F32, [tA])
            dbg("rowsC", rowsC[:], [128, 6, D], F32, [tg("rowsC")])
            dbg("rows", rows[:], [128, NR], F32, [tC])
            dbg("tri", tri_f[:], [128, 128], F32, [tC])
            P.finalize(nc, st)
            return nc

        def norm_T(src, n_tok, A_pp, sh_pp, dstT, dst_tag, bufs, want_f32=None):
            xt_l, junk, xn_l, st_l = bufs
            nt = (n_tok + 127) // 128

            def s1(t):
                r = min(128, n_tok - t * 128)
                xt, sq = xt_l[t % 2], st_l[t % 2]
                txt, tsq = tg(f"nT_xt{t % 2}"), tg(f"nT_sq{t % 2}")
                dma("sp", xt[0:r, :], src[t * 128:t * 128 + r, :], [], [txt])
                act(junk[0:r, :], xt[0:r, :], AF.Square, [txt], [tg("nT_junk"), tsq], accum=sq[0:r, 0:1])
                ts("dve", sq[0:r, 1:2], sq[0:r, 0:1], 1.0 / D, EPS, ALU.mult, ALU.add, [tsq], [tsq])
                act(sq[0:r, 2:3], sq[0:r, 1:2], AF.Sqrt, [tsq], [tsq])
                P.op("dve", lambda e, sq=sq, r=r: e.reciprocal(out=sq[0:r, 3:4], in_=sq[0:r, 2:3]), [tsq], [tsq])

            def s2(t):
                r = min(128, n_tok - t * 128)
                xt, xn, sq = xt_l[t % 2], xn_l[t % 2], st_l[t % 2]
                txt, txn, tsq = tg(f"nT_xt{t % 2}"), tg(f"nT_xn{t % 2}"), tg(f"nT_sq{t % 2}")
                act(xn[0:r, :], xt[0:r, :], AF.Copy, [txt, tsq], [txn], scale=sq[0:r, 3:4])
                pb, pbt = psum()
                pbb = pb[:].bitcast(BF16)
                for kc in range(8):
                    tr(pbb[:, kc * 128:kc * 128 + r], xn[0:r, kc * 128:(kc + 1) * 128], ident[0:r, 0:r], [txn, tC], [pbt])
                for kc in range(8):
                    ts("dve", dstT[:, kc, t * 128:t * 128 + r], pbb[:, kc * 128:kc * 128 + r], A_pp[:, kc:kc + 1], sh_pp[:, kc:kc + 1],
                       ALU.mult, ALU.add, [pbt, tA, tg("modP")], [dst_tag])

            s1(0)
            for t in range(nt):
                if t + 1 < nt:
                    s1(t + 1)
                s2(t)

        def blocks(W):
            out, t0 = [], 0
            while t0 < W:
                n = min(512, W - t0)
                out.append((t0, n))
                t0 += n
            return out

        def front(seq, srcT, src_tag, W, off, nchunk, col0, cw_name, cw_base, mL, mR, nm, bufs, hook=None):
            raw_l, cv_l, dg5, stage_l = bufs
            wstate = {}

            def stA(ch):
                if hook is not None:
                    hook()
                if ch % 4 == 0:
                    wstate["w"] = load_w(w_in[0][:, col0 + ch * 128:col0 + ch * 128 + 512])
                wtile, wt = wstate["w"]
                raw = raw_l[ch % 2]
                traw = tg(f"fr_raw{ch % 2}")
                for bi, (t0, n) in enumerate(blocks(W)):
                    pb, pbt = psum()
                    for k in range(8):
                        mm(pb[:, 0:n], wtile[:, k, (ch % 4) * 128:(ch % 4 + 1) * 128], srcT[:, k, t0:t0 + n], k == 0, k == 7,
                           [wt, src_tag], [pbt])
                    cp("act" if bi % 2 == 0 else "dve", raw[:, t0:t0 + n], pb[:, 0:n], [pbt], [traw])
                tt("pool", raw[:, 0:nm], raw[:, 0:nm], mL, ALU.mult, [traw, tC], [traw])
                tt("pool", raw[:, W - nm:W], raw[:, W - nm:W], mR, ALU.mult, [traw, tC], [traw])

            def stB(ch):
                raw = raw_l[ch % 2]
                traw = tg(f"fr_raw{ch % 2}")
                tdg = tg("fr_dg")
                for k in range(5):
                    ts("dve", dg5[:, k, :], ident[:], S(cw_name, cw_base + ch * 5 + k, 1), None, ALU.mult, None, [tC], [tdg])
                cv = cv_l[ch % 2]
                tcv = tg(f"fr_cv{ch % 2}")
                for tb in range(4):
                    pb, pbt = psum()
                    for k in range(5):
                        s0 = off - 2 + k + tb * 512
                        mm(pb[:, :], dg5[:, k, :], raw[:, s0:s0 + 512], k == 0, k == 4, [tdg, traw], [pbt])
                    act(cv[:, tb * 512:(tb + 1) * 512], pb[:, :], AF.Silu, [pbt, tC], [tcv], bias=S("cb", ch, 1))

            def stC(ch):
                cv = cv_l[ch % 2]
                tcv = tg(f"fr_cv{ch % 2}")
                if ch < 20:
                    stg = stage_l[ch % 2]
                    tstg = tg(f"fr_stg{ch % 2}")
                    for q in range(4):
                        pb, pbt = psum()
                        pbb = pb[:].bitcast(BF16)
                        for i in range(4):
                            tl = q * 4 + i
                            tr(pbb[:, i * 128:(i + 1) * 128], cv[:, tl * 128:(tl + 1) * 128], ident[:], [tcv, tC], [pbt])
                        cp("dve" if q % 2 == 0 else "act", stg[:, q * 4:(q + 1) * 4, :], pbb[:, 0:512].rearrange("p (a b) -> p a b", a=4),
                           [pbt], [tstg])
                    if ch < 16:
                        dst = xs_tok[seq].rearrange("(t p) c -> p t c", p=128)[:, :, ch * 128:(ch + 1) * 128]
                    else:
                        dst = b_tok[seq].rearrange("(t p) c -> p t c", p=128)[:, :, (ch - 16) * 128:(ch - 15) * 128]
                    dma("sp", dst, stg[:, :, :], [tstg], [tg(f"d_tok{seq}")])
                if seq == 3 and ch >= 16:
                    dstd = bT_d if ch < 20 else cT_d
                    c4 = (ch - 16) % 4
                    dma("sp", dstd[c4 * 128:(c4 + 1) * 128, :], cv[:, :], [tcv], [tg("d_bc")])

            for step in range(nchunk + 2):
                if step < nchunk:
                    stA(step)
                if 0 <= step - 1 < nchunk:
                    stB(step - 1)
                if 0 <= step - 2 < nchunk:
                    stC(step - 2)

        def dt_pass(srcT, src_tag, off, wdt_tile, wdt_tag, ncol, bias_row, dst, dst_tag, tmp):
            for t in range(NCH):
                pb, pbt = psum()
                for k in range(8):
                    mm(pb[:, 0:ncol], srcT[:, k, off + t * 128:off + (t + 1) * 128], wdt_tile[:, k, 0:ncol], k == 0, k == 7,
                       [src_tag, wdt_tag], [pbt])
                tt("dve", tmp[:, 0:ncol], pb[:, 0:ncol], bias_row, ALU.add, [pbt, tC], [tg("dt_tmp")])
                act(tmp[:, 0:ncol], tmp[:, 0:ncol], AF.Exp, [tg("dt_tmp")], [tg("dt_tmp")])
                act(dst[:, t, 0:ncol], tmp[:, 0:ncol], AF.Ln, [tg("dt_tmp")], [dst_tag], bias=1.0)

        new_phase()
        nb = ([carve([128, D], F32) for _ in range(2)], carve([128, D], BF16), [carve([128, D], BF16) for _ in range(2)],
              [carve([128, 4], F32) for _ in range(2)])
        tuT = tg("uT")
        norm_T(xo, TH, A1, sh1, uT, tuT, nb)
        dbg("adaP", adaP[:], [128, 64], F32, [tA])
        dbg("rowsC", rowsC[:], [128, 6, D], F32, [tg("rowsC")])
        dbg("uT", uT[:], [128, 8, TH], BF16, [tuT])
        if upto <= 1:
            P.finalize(nc, st)
            return nc

        pre_jobs = []
        for e in range(NEXP):
            for blk in range(4):
                pre_jobs.append((w_gu[0][e][:, blk * 512:(blk + 1) * 512],
                                 wgu_bf[e * 128:(e + 1) * 128, :].rearrange("p (kc n) -> p kc n", kc=8)[:, :, blk * 512:(blk + 1) * 512]))
            for blk in range(2):
                pre_jobs.append((w_down[0][e][:, blk * 512:(blk + 1) * 512],
                                 wdn_bf[e * 128:(e + 1) * 128, :].rearrange("p (kc n) -> p kc n", kc=8)[:, :, blk * 512:(blk + 1) * 512]))
        pre_i = [0]
        pre_pending = [None]
        pstage = []

        def prepass_step(n=1):
            for _ in range(n):
                if pre_pending[0] is not None:
                    buf, tag, dst = pre_pending[0]
                    dma("sp", dst, buf, [tag], [tg("d_wbf")])
                    pre_pending[0] = None
                if pre_i[0] < len(pre_jobs):
                    src, dst = pre_jobs[pre_i[0]]
                    buf, tname = pstage[pre_i[0] % len(pstage)]
                    pre_i[0] += 1
                    tag = tg(tname)
                    dma("pool", buf, src.rearrange("(kc p) n -> p kc n", p=128), [], [tag])
                    pre_pending[0] = (buf, tag, dst)

        def prepass_flush_pending():
            if pre_pending[0] is not None:
                buf, tag, dst = pre_pending[0]
                dma("sp", dst, buf, [tag], [tg("d_wbf")])
                pre_pending[0] = None

        new_phase()
        pstage[:] = [(carve([128, 8, 512], BF16), f"pstage{i}") for i in range(2)]
        hcT = carve([128, 8, T], BF16)
        sg_l = [carve([128, TH], BF16) for _ in range(2)]
        h_l = [carve([128, TH], BF16) for _ in range(2)]
        dg31 = carve([128, 31, 128], BF16)
        sqb = carve([128, 8, 512], BF16)
        lnA = carve([128, 512], F32)
        lnB = carve([128, 512], F32)
        lnC = carve([128, 512], F32)
        lnT = [carve([128, 512], F32) for _ in range(2)]
        ycs = [carve([128, D], BF16) for _ in range(2)]
        for j in range(8):
            prepass_step(5)
            if j % 4 == 0:
                wg_t, wg_tag = load_w(w_in[0][:, C_G + j * 128:C_G + j * 128 + 512])
                wa_t, wa_tag = load_w(w_in[0][:, C_A + j * 128:C_A + j * 128 + 512])
            sg, h = sg_l[j % 2], h_l[j % 2]
            tsg, th = tg(f"cf_sg{j % 2}"), tg(f"cf_h{j % 2}")
            for (t0, n) in blocks(TH):
                pb, pbt = psum()
                for k in range(8):
                    mm(pb[:, 0:n], wg_t[:, k, (j % 4) * 128:(j % 4 + 1) * 128], uT[:, k, t0:t0 + n], k == 0, k == 7, [wg_tag, tuT], [pbt])
                act(sg[:, t0:t0 + n], pb[:, 0:n], AF.Sigmoid, [pbt], [tsg])
            for (t0, n) in blocks(TH):
                pb, pbt = psum()
                for k in range(8):
                    mm(pb[:, 0:n], wa_t[:, k, (j % 4) * 128:(j % 4 + 1) * 128], uT[:, k, t0:t0 + n], k == 0, k == 7, [wa_tag, tuT], [pbt])
                tt("dve", h[:, t0:t0 + n], pb[:, 0:n], sg[:, t0:t0 + n], ALU.mult, [pbt, tsg], [th])
            tt("pool", h[:, 0:16], h[:, 0:16], S("mown", 0, 16), ALU.mult, [th, tC], [th])
            tt("pool", h[:, TH - 16:TH], h[:, TH - 16:TH], S("mown", 16, 16), ALU.mult, [th, tC], [th])
            tdg = tg("cf_dg")
            for k in range(31):
                ts("dve", dg31[:, k, :], ident[:], S("dw", j * 31 + k, 1), None, ALU.mult, None, [tC], [tdg])
            for tb in range(4):
                pb, pbt = psum()
                for k in range(31):
                    s0 = HALO - 15 + k + tb * 512
                    mm(pb[:, :], dg31[:, k, :], h[:, s0:s0 + 512], k == 0, k == 30, [tdg, th], [pbt])
                act(hcT[:, j, tb * 512:(tb + 1) * 512], pb[:, :], AF.Identity, [pbt, tC], [tg(f"hcT{tb}")], bias=S("dwb", j, 1))
        for tb in range(4):
            thc = tg(f"hcT{tb}")
            sl = slice(tb * 512, (tb + 1) * 512)
            tt("pool", sqb[:, :, :], hcT[:, :, sl], hcT[:, :, sl], ALU.mult, [thc], [tg("sqb")])
            p1, p1t = psum()
            for j in range(8):
                mm(p1[:, :], onesb[:], hcT[:, j, sl], j == 0, j == 7, [tC, thc], [p1t])
            p2, p2t = psum()
            for j in range(8):
                mm(p2[:, :], onesb[:], sqb[:, j, :], j == 0, j == 7, [tC, tg("sqb")], [p2t])
            tln = tg("ln")
            ts("dve", lnA, p1[:, :], 1.0 / D, None, ALU.mult, None, [p1t], [tln])
            tt("dve", lnB, lnA, lnA, ALU.mult, [tln], [tln])
            stt("dve", lnB, p2[:, :], 1.0 / D, lnB, ALU.mult, ALU.subtract, [p2t, tln], [tln])
            ts("dve", lnB, lnB, EPS, None, ALU.add, None, [tln], [tln])
            act(lnB, lnB, AF.Sqrt, [tln], [tln])
            P.op("dve", lambda e: e.reciprocal(out=lnB, in_=lnB), [tln], [tln])
            tt("dve", lnC, lnA, lnB, ALU.mult, [tln], [tln])
            for j in range(8):
                lt = lnT[j % 2]
                tlt = tg(f"lnT{j % 2}")
                tt("dve", lt, hcT[:, j, sl], lnB, ALU.mult, [thc, tln], [tlt])
                tt("pool", lt, lt, lnC, ALU.subtract, [tlt, tln], [tlt])
                act(hcT[:, j, sl], lt, AF.Silu, [tlt, tC], [thc], bias=S("lnb", j, 1), scale=S("lng", j, 1))
        prepass_flush_pending()
        wco_h = [load_w(conf_out_w[0][:, h_ * 512:(h_ + 1) * 512]) for h_ in range(2)]
        for t in range(NCH):
            yc = ycs[t % 2]
            tyc = tg(f"ycs{t % 2}")
            for half in range(2):
                pb, pbt = psum()
                for k in range(8):
                    mm(pb[:, :], hcT[:, k, t * 128:(t + 1) * 128], wco_h[half][0][:, k, :], k == 0, k == 7,
                       [tg(f"hcT{t // 4}"), wco_h[half][1]], [pbt])
                tt("dve", yc[:, half * 512:(half + 1) * 512], pb[:, :], Rr("cob", half * 512, 512), ALU.add, [pbt, tC], [tyc])
            dma("sp", yconf_d[t * 128:(t + 1) * 128, :], yc[:, :], [tyc], [tg("d_yconf")])
        if upto <= 2:
            P.finalize(nc, st)
            return nc

        new_phase()
        pstage[:] = [(carve([128, 8, 512], BF16), f"pstage{i}") for i in range(2)]
        fb = ([carve([128, TH], BF16) for _ in range(2)], [carve([128, T], BF16) for _ in range(2)], carve([128, 5, 128], BF16),
              [carve([128, 16, 128], BF16) for _ in range(2)])
        dt_own = sb("dt_own", [128, NCH, 64], F32)
        dt_sl = sb("dt_sl", [128, 3, NCH, 32], F32)
        dt_tmp = sb("dt_tmp", [128, 64], F32)
        wdt_t = carve([128, 8, 64], BF16)
        twdt = tg("wdt")
        dma("pool", wdt_t, w_in[0][:, C_DTF:C_DTF + 64].rearrange("(kc p) n -> p kc n", p=128), [], [twdt])
        front(3, uT, tuT, TH, HALO, 24, C_XS, "cw", 0, S("mown", 0, 16), S("mown", 16, 16), 16, fb, hook=prepass_step)
        dt_pass(uT, tuT, HALO, wdt_t, twdt, 64, Rr("dtb"), dt_own, tg("dt_own"), dt_tmp)
        if upto <= 3:
            dbg("dt_own", dt_own[:], [128, NCH, 64], F32, [tg("dt_own")])
            P.finalize(nc, st)
            return nc
        uTs = carve([128, 8, TS], BF16)
        nb3 = ([carve([128, D], F32) for _ in range(2)], carve([128, D], BF16), [carve([128, D], BF16) for _ in range(2)],
               [carve([128, 4], F32) for _ in range(2)])
        wdt_s = carve([128, 8, 32], BF16)
        for k in range(3):
            tus = tg("uTs")
            norm_T(xs3[k], TS, A1, sh1, uTs, tus, nb3)
            dma("pool", wdt_s, wdt3[k].rearrange("(kc p) n -> p kc n", p=128), [], [tg("wdts")])
            front(k, uTs, tus, TS, 2, 20, C_XS, "cw3", k * 100, S("m3", k * 4, 2), S("m3", k * 4 + 2, 2), 2, fb, hook=prepass_step)
            dt_pass(uTs, tus, 2, wdt_s, tg("wdts"), 32, Rr("dtb3", k * 32, 32), dt_sl[:, k], tg("dt_sl"), dt_tmp)

        prepass_flush_pending()
        new_phase()
        pstage[:] = [(wbuf[i][:, :, :], f"wbuf{i}") for i in range(3)]
        Arow = carve([128, 64], F32)
        Arow3 = carve([128, 96], F32)
        act(Arow, Rr("alog"), AF.Exp, [tC], [tg("Arow")])
        ts("dve", Arow, Arow, -1.0, None, ALU.mult, None, [tg("Arow")], [tg("Arow")])
        act(Arow3, Rr("alog3"), AF.Exp, [tC], [tg("Arow")])
        ts("dve", Arow3, Arow3, -1.0, None, ALU.mult, None, [tg("Arow")], [tg("Arow")])
        xs_c = [carve([128, 2048], BF16) for _ in range(2)]
        b_c = [carve([128, 512], BF16) for _ in range(2)]
        xdtd = [carve([128, 2048], BF16) for _ in range(2)]
        sm_l = [carve([128, 6, 32], F32) for _ in range(2)]
        Rst = carve([128, 2048], F32)
        hFs = carve([128, 2048], F32)
        hBs = carve([128, 2048], F32)
        hbf = [carve([128, 2048], BF16) for _ in range(2)]
        cs_i = [0]

        def cs_front(seq, c, dt_ap, A_ap, tri):
            i = cs_i[0] % 2
            cs_i[0] += 1
            if cs_i[0] % 2 == 0:
                prepass_step(1)
            xc, bc, xd, smt = xs_c[i], b_c[i], xdtd[i], sm_l[i]
            txc, tsm, txd = tg(f"cs_x{i}"), tg(f"cs_sm{i}"), tg(f"cs_xd{i}")
            dma("sp", xc, xs_tok[seq][c * 128:(c + 1) * 128, :], [tg(f"d_tok{seq}")], [txc])
            dma("sp", bc, b_tok[seq][c * 128:(c + 1) * 128, :], [tg(f"d_tok{seq}")], [txc])
            a_t, tot, dec, cdb, sc = smt[:, 0, :], smt[:, 1, :], smt[:, 2, :], smt[:, 3, :], smt[:, 4, :]
            tt("dve", a_t, dt_ap, A_ap, ALU.mult, [tg("dt_own"), tg("dt_sl"), tg("Arow")], [tsm])
            pa_, pat_ = psum()
            mm(pa_[:, 0:32], tri[:], a_t, True, True, [tC, tsm], [pat_])
            mm(pa_[:, 32:64], onesf[:], a_t, True, True, [tC, tsm], [pat_])
            cp("act", tot, pa_[:, 32:64], [pat_], [tsm])
            tt("dve", dec, tot, pa_[:, 0:32], ALU.subtract, [pat_, tsm], [tsm])
            act(dec, dec, AF.Exp, [tsm], [tsm])
            act(cdb, tot, AF.Exp, [tsm], [tsm])
            tt("dve", sc, dec, dt_ap, ALU.mult, [tsm, tg("dt_own"), tg("dt_sl")], [tsm])
            tt("dve", xd.rearrange("p (h d) -> p h d", h=32), xc.rearrange("p (h d) -> p h d", h=32),
               sc.unsqueeze(2).to_broadcast([128, 32, 64]), ALU.mult, [txc, tsm], [txd])
            return bc, xd, cdb, txc, txd, tsm

        def cs_back(ctx, Racc, tR):
            bc, xd, cdb, txc, txd, tsm = ctx
            for g in range(4):
                pb, pbt = psum()
                mm(pb[:, :], bc[:, g * 128:(g + 1) * 128], xd[:, g * 512:(g + 1) * 512], True, True, [txc, txd], [pbt])
                Rg = Racc[:, g * 512:(g + 1) * 512]
                tt("dve", Rg.rearrange("p (h d) -> p h d", h=8), Rg.rearrange("p (h d) -> p h d", h=8),
                   cdb[:, g * 8:(g + 1) * 8].unsqueeze(2).to_broadcast([128, 8, 64]), ALU.mult, [tR, tsm], [tR])
                tt("dve", Rg, Rg, pb[:, :], ALU.add, [tR, pbt], [tR])

        tRs, tHF, tHB = tg("Rst"), tg("hFs"), tg("hBs")
        P.op("pool", lambda e: e.memset(Rst, 0.0), [], [tRs])
        P.op("pool", lambda e: e.memset(hFs, 0.0), [], [tHF])
        P.op("pool", lambda e: e.memset(hBs, 0.0), [], [tHB])
        items = []
        for k in range(3):
            for c in range(NCH):
                pre = (lambda k=k: ts("dve", Rst, Rst, S("flg", k, 1), None, ALU.mult, None, [tRs, tC], [tRs])) if c == 0 else None

                def post(k=k):
                    stt("dve", hFs, Rst, S("flg", 3 + k, 1), hFs, ALU.mult, ALU.add, [tRs, tHF, tC], [tHF])
                    stt("dve", hBs, Rst, S("flg", 6 + k, 1), hBs, ALU.mult, ALU.add, [tRs, tHB, tC], [tHB])
                items.append(((k, c, dt_sl[:, k, c, :], Arow3[:, k * 32:(k + 1) * 32], tri_f), Rst, tRs, pre, post if c == NCH - 1 else None))
        for c in range(NCH):
            def pre(c=c):
                hb_ = hbf[c % 2]
                cp("act", hb_, hFs, [tHF], [tg(f"hbf{c % 2}")])
                dma("sp", hF_d[c], hb_, [tg(f"hbf{c % 2}")], [tg("d_hF")])
            items.append(((3, c, dt_own[:, c, 0:32], Arow[:, 0:32], tri_f), hFs, tHF, pre, None))
        for c in range(NCH - 1, -1, -1):
            def pre(c=c):
                hb_ = hbf[c % 2]
                cp("act", hb_, hBs, [tHB], [tg(f"hbf{c % 2}")])
                dma("sp", hB_d[c], hb_, [tg(f"hbf{c % 2}")], [tg("d_hB")])
            items.append(((3, c, dt_own[:, c, 32:64], Arow[:, 32:64], tri_b), hBs, tHB, pre, None))

        def run_back(it, ctx):
            _, Racc, tR, pre, post = it
            if pre is not None:
                pre()
            cs_back(ctx, Racc, tR)
            if post is not None:
                post()
        prev = None
        for it in items:
            ctx = cs_front(*it[0])
            if prev is not None:
                run_back(*prev)
            prev = (it, ctx)
        run_back(*prev)
        if upto <= 4:
            P.finalize(nc, st)
            return nc

        new_phase()
        Arow = carve([128, 64], F32)
        act(Arow, Rr("alog"), AF.Exp, [tC], [tg("Arow")])
        ts("dve", Arow, Arow, -1.0, None, ALU.mult, None, [tg("Arow")], [tg("Arow")])
        wz = carve([128, 8, 2048], BF16)
        twz = tg("wz")
        for q in range(4):
            dma("pool", wz[:, :, q * 512:(q + 1) * 512], w_in[0][:, C_Z + q * 512:C_Z + (q + 1) * 512].rearrange("(kc p) n -> p kc n", p=128),
                [], [twz])
        xsc_l = [carve([128, 2048], BF16) for _ in range(2)]
        bTc_l = [carve([128, 4, 128], BF16) for _ in range(2)]
        cTc_l = [carve([128, 4, 128], BF16) for _ in range(2)]
        hst = [carve([128, 2048], BF16) for _ in range(2)]
        xdt = [carve([128, 2048], BF16) for _ in range(2)]
        rsegp = [carve([128, 4, 128], F32) for _ in range(3)]
        eseg = [carve([128, 512], BF16) for _ in range(2)]
        MTl = [carve([128, 4, 128], BF16) for _ in range(2)]
        CBm = [carve([128, 4, 128], BF16) for _ in range(2)]
        yo = carve([128, 2048], F32)
        ytmp_l = [carve([128, 512], F32) for _ in range(2)]
        szl = [carve([128, 512], BF16) for _ in range(4)]
        ysl = [carve([128, 2048], BF16) for _ in range(2)]
        sm5 = carve([128, 8, 32], F32)
        ss5 = carve([128, 8], F32)
        jk5 = carve([128, 512], BF16)
        tris = (tri_f, tri_b)
        ustrs = (ustr_f, ustr_b)
        mks = (mk_f, mk_b)
        hds = (hF_d, hB_d)
        rs_i = 0
        yt_i = 0
        for c in range(NCH):
            prepass_step(2)
            bank_rng[:] = [4, 8]
            cb = c % 2
            xsc, bTc, cTc = xsc_l[cb], bTc_l[cb], cTc_l[cb]
            tx, tbc, tyo = tg(f"p5_x{cb}"), tg(f"p5_bc{cb}"), tg("p5_yo")
            dma("sp", xsc, xs_tok[3][c * 128:(c + 1) * 128, :], [tg("d_tok3")], [tx])
            dma("sp", bTc, bT_d.rearrange("(g n) t -> n g t", n=128)[:, :, c * 128:(c + 1) * 128], [tg("d_bc")], [tbc])
            dma("sp", cTc, cT_d.rearrange("(g n) t -> n g t", n=128)[:, :, c * 128:(c + 1) * 128], [tg("d_bc")], [tbc])
            for d in range(2):
                dma("sp", hst[d], hds[d][c], [tg("d_hF"), tg("d_hB")], [tg(f"p5_h{d}")])
            for g in range(4):
                sl = slice(g * 512, (g + 1) * 512)
                pz, pzt = psum()
                for k in range(8):
                    mm(pz[:, :], uT[:, k, HALO + c * 128:HALO + (c + 1) * 128], wz[:, k, sl], k == 0, k == 7, [tuT, twz], [pzt])
                act(szl[g], pz[:, :], AF.Silu, [pzt], [tg(f"p5_sz{g}")])
            pcb, pcbt = psum()
            for g in range(4):
                mm(pcb[:, g * 128:(g + 1) * 128], bTc[:, g, :], cTc[:, g, :], True, True, [tbc], [pcbt])
            for d in range(2):
                tt("dve", CBm[d], pcb[:, :].rearrange("p (g i) -> p g i", g=4), mks[d][:, :].unsqueeze(1).to_broadcast([128, 4, 128]),
                   ALU.mult, [pcbt, tC], [tg(f"p5_cbm{d}")])
            for d in range(2):
                tsm = tg(f"p5_sm{d}")
                dt_ap = dt_own[:, c, d * 32:(d + 1) * 32]
                a_t, e_t = sm5[:, d * 4 + 0, :], sm5[:, d * 4 + 1, :]
                tt("dve", a_t, dt_ap, Arow[:, d * 32:(d + 1) * 32], ALU.mult, [tg("dt_own"), tg("Arow")], [tsm])
                pa_, pat_ = psum()
                mm(pa_[:, 0:32], tris[d][:], a_t, True, True, [tC, tsm], [pat_])
                act(e_t, pa_[:, 0:32], AF.Exp, [pat_], [tsm])
                tt("dve" if d == 0 else "pool", xdt[d].rearrange("p (h d) -> p h d", h=32), xsc.rearrange("p (h d) -> p h d", h=32),
                   dt_ap.unsqueeze(2).to_broadcast([128, 32, 64]), ALU.mult, [tx, tg("dt_own")], [tg(f"p5_xdt{d}")])
            tt("pool", yo.rearrange("p (h d) -> p h d", h=32), xsc.rearrange("p (h d) -> p h d", h=32),
               Rr("ssd").unsqueeze(2).to_broadcast([128, 32, 64]), ALU.mult, [tx, tC], [tyo])
            for d in range(2):
                tsm = tg(f"p5_sm{d}")
                txd = tg(f"p5_xdt{d}")
                a_t, e_t = sm5[:, d * 4 + 0, :], sm5[:, d * 4 + 1, :]

                def seg(hq, d=d, a_t=a_t, tsm=tsm):
                    nonlocal rs_i
                    rsp, trs = rsegp[rs_i % 3], tg(f"p5_rs{rs_i % 3}")
                    rs_i += 1
                    tt("dve", rsp, tris[d][:, :].unsqueeze(1).to_broadcast([128, 4, 128]),
                       a_t[:, hq * 4:(hq + 1) * 4].unsqueeze(2).to_broadcast([128, 4, 128]), ALU.mult, [tC, tsm], [trs])
                    pseg, psegt = psum()
                    mm(pseg[:, :], ustrs[d][:], rsp.rearrange("p a b -> p (a b)"), True, True, [tC, trs], [psegt])
                    return pseg, psegt
                nxt = seg(0)
                for hq in range(8):
                    g = hq // 2
                    pseg, psegt = nxt
                    if hq + 1 < 8:
                        nxt = seg(hq + 1)
                    es, tes = eseg[hq % 2], tg(f"p5_es{hq % 2}")
                    act(es, pseg[:, :], AF.Exp, [psegt], [tes])
                    MT, tmt = MTl[hq % 2], tg(f"p5_mt{hq % 2}")
                    tt("dve", MT, es.rearrange("p (a b) -> p a b", a=4), CBm[d][:, g, :].unsqueeze(1).to_broadcast([128, 4, 128]), ALU.mult,
                       [tes, tg(f"p5_cbm{d}")], [tmt])
                    for hh in range(4):
                        h = hq * 4 + hh
                        mm(banks[g][:, (h % 8) * 64:(h % 8 + 1) * 64], MT[:, hh, :], xdt[d][:, h * 64:(h + 1) * 64], d == 0 and h % 8 == 0,
                           d == 1 and h % 8 == 7, [tmt, txd], [tg(f"ps{g}")], skip=True)
                for g in range(4):
                    po, pot = psum()
                    mm(po[:, :], cTc[:, g, :], hst[d][:, g * 512:(g + 1) * 512], True, True, [tbc, tg(f"p5_h{d}")], [pot])
                    ytmp, tyt = ytmp_l[yt_i % 2], tg(f"p5_ytmp{yt_i % 2}")
                    yt_i += 1
                    tt("dve", ytmp.rearrange("p (h d) -> p h d", h=8), po[:, :].rearrange("p (h d) -> p h d", h=8),
                       e_t[:, g * 8:(g + 1) * 8].unsqueeze(2).to_broadcast([128, 8, 64]), ALU.mult, [pot, tsm], [tyt])
                    tt("dve", yo[:, g * 512:(g + 1) * 512], yo[:, g * 512:(g + 1) * 512], ytmp, ALU.add, [tyt, tyo], [tyo])
            ysn_t, tys = ysl[c % 2], tg(f"p5_ys{c % 2}")
            for g in range(4):
                sl = slice(g * 512, (g + 1) * 512)
                tt("dve", yo[:, sl], yo[:, sl], banks[g][:, :], ALU.add, [tyo, tg(f"ps{g}")], [tyo])
                tt("dve", yo[:, sl], yo[:, sl], szl[g], ALU.mult, [tyo, tg(f"p5_sz{g}")], [tyo])
                act(jk5, yo[:, sl], AF.Square, [tyo], [tg("p5_jk"), tg("p5_ss")], accum=ss5[:, g:g + 1])
            tss = tg("p5_ss")
            ts("dve", ss5[:, 4:8], ss5[:, 0:4], 1.0 / 512, EPS, ALU.mult, ALU.add, [tss], [tss])
            act(ss5[:, 4:8], ss5[:, 4:8], AF.Sqrt, [tss], [tss])
            P.op("dve", lambda e: e.reciprocal(out=ss5[:, 4:8], in_=ss5[:, 4:8]), [tss], [tss])
            for g in range(4):
                sl = slice(g * 512, (g + 1) * 512)
                if g % 2 == 0:
                    ts("dve", ysn_t[:, sl], yo[:, sl], ss5[:, 4 + g:5 + g], None, ALU.mult, None, [tyo, tss], [tys])
                else:
                    act(ysn_t[:, sl], yo[:, sl], AF.Copy, [tyo, tss], [tys], scale=ss5[:, 4 + g:5 + g])
            dma("sp", ysn_d[c * 128:(c + 1) * 128, :], ysn_t, [tys], [tg("d_ysn")])
        bank_rng[:] = [0, 8]
        while pre_i[0] < len(pre_jobs) or pre_pending[0] is not None:
            prepass_step(1)
        if upto <= 5:
            P.finalize(nc, st)
            return nc

        new_phase()
        wgs = carve([128, 8, 2048], BF16)
        wso = carve([128, 16, D], BF16)
        tw6 = tg("w6")
        wo_h = [load_w(w_o[0][:, q * 512:(q + 1) * 512]) for q in range(2)]
        for q in range(4):
            dma("pool", wgs[:, :, q * 512:(q + 1) * 512], w_in[0][:, C_GC + q * 512:C_GC + (q + 1) * 512].rearrange("(kc p) n -> p kc n", p=128),
                [], [tw6])
        for q in range(2):
            dma("pool", wso[:, :, q * 512:(q + 1) * 512], ssm_out_w[0][:, q * 512:(q + 1) * 512].rearrange("(kc p) n -> p kc n", p=128), [], [tw6])
        for kc in range(16):
            ts("dve", wso[:, kc, :], wso[:, kc, :], S("sng", kc, 1), None, ALU.mult, None, [tw6, tC], [tw6])
        ysn_tl = [carve([128, 2048], BF16) for _ in range(2)]
        yc_tl = [carve([128, D], BF16) for _ in range(2)]
        x_tl = [carve([128, D], F32) for _ in range(2)]
        ysnT = carve([128, 16, 128], BF16)
        sgt = carve([128, 2048], BF16)
        m1 = carve([128, D], BF16)
        mg = carve([128, D], BF16)
        mT = carve([128, 8, 128], BF16)
        tmp6 = carve([128, 512], F32)
        for t in range(NCH):
            tin, tys_, tsg6, tm1, tmg, tmT, ttmp = tg(f"p6_in{t % 2}"), tg("p6_ysnT"), tg("p6_sg"), tg("p6_m1"), tg("p6_mg"), tg("p6_mT"), tg("p6_tmp")
            ysn_t, yc_t, x_t = ysn_tl[t % 2], yc_tl[t % 2], x_tl[t % 2]
            tpx = tg(f"p6_x{t % 2}")
            dma("sp", ysn_t, ysn_d[t * 128:(t + 1) * 128, :], [tg("d_ysn")], [tin])
            dma("sp", yc_t, yconf_d[t * 128:(t + 1) * 128, :], [tg("d_yconf")], [tin])
            dma("sp", x_t, xo[HALO + t * 128:HALO + (t + 1) * 128, :], [], [tpx])
            for q in range(2):
                pb, pbt = psum()
                pbb = pb[:].bitcast(BF16)
                for i in range(8):
                    tr(pbb[:, i * 128:(i + 1) * 128], ysn_t[:, (q * 8 + i) * 128:(q * 8 + i + 1) * 128], ident[:], [tin, tC], [pbt])
                cp("dve" if q == 0 else "act", ysnT[:, q * 8:(q + 1) * 8, :], pbb.rearrange("p (a b) -> p a b", a=8), [pbt], [tys_])
            for q in range(4):
                pb, pbt = psum()
                for k in range(8):
                    mm(pb[:, :], uT[:, k, HALO + t * 128:HALO + (t + 1) * 128], wgs[:, k, q * 512:(q + 1) * 512], k == 0, k == 7, [tuT, tw6], [pbt])
                act(sgt[:, q * 512:(q + 1) * 512], pb[:, :], AF.Sigmoid, [pbt], [tsg6])
            tt("pool", m1, sgt[:, 0:D], yc_t, ALU.mult, [tsg6, tin], [tm1])
            for half in range(2):
                sl = slice(half * 512, (half + 1) * 512)
                pb, pbt = psum()
                for k in range(16):
                    mm(pb[:, :], ysnT[:, k, :], wso[:, k, sl], k == 0, k == 15, [tys_, tw6], [pbt])
                tt("dve", tmp6, pb[:, :], sgt[:, D + half * 512:D + (half + 1) * 512], ALU.mult, [pbt, tsg6], [ttmp])
                tt("pool", mg[:, sl], tmp6, m1[:, sl], ALU.add, [ttmp, tm1], [tmg])
            pb, pbt = psum()
            pbb = pb[:].bitcast(BF16)
            for i in range(8):
                tr(pbb[:, i * 128:(i + 1) * 128], mg[:, i * 128:(i + 1) * 128], ident[:], [tmg, tC], [pbt])
            cp("act", mT, pbb.rearrange("p (a b) -> p a b", a=8), [pbt], [tmT])
            for half in range(2):
                sl = slice(half * 512, (half + 1) * 512)
                pb, pbt = psum()
                for k in range(8):
                    mm(pb[:, :], mT[:, k, :], wo_h[half][0][:, k, :], k == 0, k == 7, [tmT, wo_h[half][1]], [pbt])
                tt("dve", tmp6, pb[:, :], G1r[:, sl], ALU.mult, [pbt, tg("rowsC")], [ttmp])
                tt("pool", x_t[:, sl], tmp6, x_t[:, sl], ALU.add, [ttmp, tpx], [tpx])
            dma("sp", x1_d[t * 128:(t + 1) * 128, :], x_t, [tpx], [tg("d_x1")])
        if upto <= 6:
            P.finalize(nc, st)
            return nc

        new_phase()
        xt_l = [carve([128, D], F32) for _ in range(2)]
        junk = carve([128, D], BF16)
        xn = carve([128, D], F32)
        vtok_all = carve([128, NCH, D], BF16)
        vTt_l = [carve([128, 8, 128], BF16) for _ in range(2)]
        st_l = [carve([128, 4], F32) for _ in range(2)]
        rwb = carve([128, 8, NEXP], BF16)
        lg_all = carve([128, NCH, 32], F32)
        wr_all = carve([128, NCH, 32], F32)
        sl_all = carve([128, NCH, 32], F32)
        m8_all = carve([128, NCH, 8], F32)
        rt = carve([128, 8, 32], F32)
        base = carve([128, 32], F32)
        big = carve([128, NBLK, 32], F32)
        bexp = carve([128, NBLK], F32)
        idxw_f = carve([128, NBLK], F32)
        dma("pool", rwb, router_w[0].rearrange("(kc p) n -> p kc n", p=128), [], [tg("rwb")])
        wk_all = dt_own[:, :, 0:4]
        idx_f = dt_own[:, :, 4:8]
        idx_i = sb("idx_i", [128, NCH * 4], I32)
        idxw_i = sb("idxw_i", [128, NBLK], I32)
        bexp_i = sb("bexp_i", [128, NBLK], I32)
        twk, trt, tbase, trA = tg("wk_all"), tg("rt"), tg("base"), tg("routeA")
        P.op("pool", lambda e: e.memset(base, 0.0), [], [tbase])
        xn_l = [xn, carve([128, D], F32)]
        rt_l = [rt, carve([128, 8, 32], F32)]

        def rA1(t):
            xt, sq, vTt, xn_ = xt_l[t % 2], st_l[t % 2], vTt_l[t % 2], xn_l[t % 2]
            txt, txn, tsq, tvTt = tg(f"m_xt{t % 2}"), tg(f"m_xn{t % 2}"), tg(f"m_sq{t % 2}"), tg(f"m_vTt{t % 2}")
            tvt = tg(f"vtok{t}")
            dma("sp", xt, x1_d[t * 128:(t + 1) * 128, :], [tg("d_x1")], [txt])
            act(junk, xt, AF.Square, [txt], [tg("m_junk"), tsq], accum=sq[:, 0:1])
            ts("dve", sq[:, 1:2], sq[:, 0:1], 1.0 / D, EPS, ALU.mult, ALU.add, [tsq], [tsq])
            act(sq[:, 2:3], sq[:, 1:2], AF.Sqrt, [tsq], [tsq])
            P.op("dve", lambda e, sq=sq: e.reciprocal(out=sq[:, 3:4], in_=sq[:, 2:3]), [tsq], [tsq])
            stt("dve", xn_, xt, sq[:, 3:4], A2r, ALU.mult, ALU.mult, [txt, tsq, tg("rowsC")], [txn])
            tt("pool", vtok_all[:, t, :], xn_, SH2r, ALU.add, [txn, tg("rowsC")], [tvt])
            pb, pbt = psum()
            pbb = pb[:].bitcast(BF16)
            for kc in range(8):
                tr(pbb[:, kc * 128:(kc + 1) * 128], vtok_all[:, t, kc * 128:(kc + 1) * 128], ident[:], [tvt, tC], [pbt])
            cp("act", vTt, pbb.rearrange("p (a b) -> p a b", a=8), [pbt], [tvTt])
            pl, plt = psum()
            for k in range(8):
                mm(pl[:, 0:32], vTt[:, k, :], rwb[:, k, :], k == 0, k == 7, [tvTt, tg("rwb")], [plt])
            return pl, plt

        def rA2(t, pl, plt):
            rt_ = rt_l[t % 2]
            trt_, trA_ = tg(f"rt{t % 2}"), tg(f"routeA{t}")
            lg, m8, wr = lg_all[:, t, :], m8_all[:, t, :], wr_all[:, t, :]
            ex, msk = rt_[:, 0, :], rt_[:, 1, :]
            sc1 = rt_[:, 2, 0:4]
            tt("dve", lg, pl[:, 0:32], Rr("rb"), ALU.add, [plt, tC], [trA_])
            P.op("dve", lambda e, m8=m8, lg=lg: e.max(out=m8, in_=lg), [trA_], [trA_])
            ts("dve", sc1[:, 0:1], m8[:, 0:1], -1.0, None, ALU.mult, None, [trA_], [trt_])
            ts("dve", msk, lg, m8[:, 3:4], None, ALU.is_ge, None, [trA_], [trt_])
            act(ex, lg, AF.Exp, [trA_, trt_], [trt_], bias=sc1[:, 0:1])
            tt("dve", ex, ex, msk, ALU.mult, [trt_], [trt_])
            P.op("dve", lambda e, ex=ex, sc1=sc1: e.tensor_reduce(out=sc1[:, 1:2], in_=ex, axis=AX.X, op=ALU.add), [trt_], [trt_])
            P.op("dve", lambda e, sc1=sc1: e.reciprocal(out=sc1[:, 2:3], in_=sc1[:, 1:2]), [trt_], [trt_])
            ts("dve", wr, ex, sc1[:, 2:3], None, ALU.mult, None, [trt_], [trA_])
            pp, ppt = psum()
            mm(pp[:, 0:32], ustr_b[:], msk, True, True, [tC, trt_], [ppt])
            mm(pp[:, 32:64], onesf[:], msk, True, True, [tC, trt_], [ppt])
            tt("dve", sl_all[:, t, :], pp[:, 0:32], base, ALU.add, [ppt, tbase], [trA_])
            tt("dve", base, base, pp[:, 32:64], ALU.add, [ppt, tbase], [tbase])

        cur = rA1(0)
        for t in range(NCH):
            nxt = rA1(t + 1) if t + 1 < NCH else None
            rA2(t, *cur)
            cur = nxt
        trt = tg("rt0")
        tpb = tg("padblk")
        cnt3 = big[:, 0:32, 0:16]
        tt("dve", cnt3, base.unsqueeze(2).to_broadcast([128, 32, 16]), Rr("thr16").unsqueeze(1).to_broadcast([128, 32, 16]), ALU.is_gt,
           [tbase, tC], [tpb])
        padded, pend, ptmp, pstart = rt[:, 3, :], rt[:, 4, :], rt[:, 5, :], rt[:, 6, :]
        P.op("dve", lambda e: e.tensor_reduce(out=padded, in_=cnt3, axis=AX.X, op=ALU.add), [tpb], [trt])
        ts("dve", padded, padded, float(BS), None, ALU.mult, None, [trt], [trt])
        cp("dve", pend, padded, [trt], [trt])
        for sh in (1, 2, 4, 8, 16):
            cp("dve", ptmp, pend, [trt], [trt])
            tt("dve", pend[:, sh:32], ptmp[:, sh:32], ptmp[:, 0:32 - sh], ALU.add, [trt], [trt])
        tt("dve", pstart, pend, padded, ALU.subtract, [trt], [trt])
        tt("dve", big, pend.unsqueeze(1).to_broadcast([128, NBLK, 32]), Rr("bstart").unsqueeze(2).to_broadcast([128, NBLK, 32]), ALU.is_le,
           [trt, tC, tpb], [tpb])
        P.op("dve", lambda e: e.tensor_reduce(out=bexp, in_=big, axis=AX.X, op=ALU.add), [tpb], [tpb])
        ts("dve", bexp, bexp, 31.0, None, ALU.min, None, [tpb], [tpb])
        ts("dve", idxw_f, bexp, 128.0, S("pidx", 0, 1), ALU.mult, ALU.add, [tpb, tC], [tpb])
        cp("dve", idxw_i[:, :], idxw_f, [tpb], [tg("idxw")])
        cp("dve", bexp_i[:, :], bexp, [tpb], [tg("idxw")])
        allA = [tg(f"routeA{t}") for t in range(NCH)]
        oh4 = carve([128, NCH, 4, 32], F32)
        tmp4 = carve([128, NCH, 4, 32], F32)
        slot_all = carve([128, NCH, 32], F32)
        t4 = tg("rc4")
        shp = [128, NCH, 4, 32]
        tt("dve", slot_all, sl_all, pstart.unsqueeze(1).to_broadcast([128, NCH, 32]), ALU.add, allA + [trt], [t4])
        tt("dve", oh4, lg_all.unsqueeze(2).to_broadcast(shp), m8_all[:, :, 0:4].unsqueeze(3).to_broadcast(shp), ALU.is_equal, allA, [t4])
        tt("dve", tmp4, oh4, slot_all.unsqueeze(2).to_broadcast(shp), ALU.mult, [t4], [t4])
        P.op("dve", lambda e: e.tensor_reduce(out=idx_f, in_=tmp4, axis=AX.X, op=ALU.add), [t4], [twk])
        tt("dve", tmp4, oh4, wr_all.unsqueeze(2).to_broadcast(shp), ALU.mult, [t4, twk] + allA, [t4])
        P.op("dve", lambda e: e.tensor_reduce(out=wk_all, in_=tmp4, axis=AX.X, op=ALU.add), [t4], [twk])
        cp("dve", idx_i[:, :].rearrange("p (t k) -> p t k", k=4), idx_f, [twk], [twk])
        for t in range(NCH):
            for k in range(4):
                P.dma("pool", lambda e, t=t, k=k: e.indirect_dma_start(
                    out=vbuf_d[:, :], out_offset=bass.IndirectOffsetOnAxis(ap=idx_i[:, t * 4 + k:t * 4 + k + 1], axis=0),
                    in_=vtok_all[:, t, :], in_offset=None, bounds_check=None), [tg(f"vtok{t}"), twk], [tg("d_vbuf")])
        dbg("wk", dt_own[:], [128, NCH, 64], F32, [twk])
        dbg("bexp", bexp, [128, NBLK], F32, [tpb])
        new_phase()
        wg_l = [uT[:].rearrange("p a b -> p (a b)")[:, 0:16 * D].rearrange("p (a b) -> p a b", a=8), carve([128, 8, 2 * D], BF16)]
        wd_l = [carve([128, 8, D], BF16) for _ in range(2)]
        bg_l = [carve([128, 16], F32) for _ in range(2)]
        bd_l = [carve([128, D], F32) for _ in range(2)]
        NT = BS // 128
        xin = carve([128, NT, D], BF16)
        actT = carve([128, 8, BS], BF16)
        gcb = [carve([128, BS], BF16) for _ in range(2)]
        sgb = [carve([128, BS], BF16) for _ in range(2)]
        ucb = [carve([128, BS], BF16) for _ in range(2)]
        yst = [carve([128, D], F32) for _ in range(2)]
        xT_l = [carve([128, 8, BS], BF16) for _ in range(2)]
        ei = 0
        yi = 0

        def blk_bufs(i):
            b2 = i % 2
            return (wg_l[b2], wd_l[b2], bg_l[b2], bd_l[b2], xT_l[b2],
                    (tuT if b2 == 0 else tg("wg1")), tg(f"wd{b2}"), tg(f"bg{b2}"), tg(f"bd{b2}"), tg(f"xT{b2}"))

        def blk_load(i):
            wg, wd, bg, bd, xTb, twg, twd, tbg, tbd, txT = blk_bufs(i)
            iw = idxw_i[:, i:i + 1]
            P.dma("pool", lambda e, wg=wg, iw=iw: e.indirect_dma_start(
                out=wg.rearrange("p a b -> p (a b)"), out_offset=None, in_=wgu_bf[:, :], in_offset=bass.IndirectOffsetOnAxis(ap=iw, axis=0),
                bounds_check=None), [tg("d_wbf"), tg("idxw")], [twg])
            P.dma("pool", lambda e, wd=wd, iw=iw: e.indirect_dma_start(
                out=wd.rearrange("p a b -> p (a b)"), out_offset=None, in_=wdn_bf[:, :], in_offset=bass.IndirectOffsetOnAxis(ap=iw, axis=0),
                bounds_check=None), [tg("d_wbf"), tg("idxw")], [twd])
            P.dma("pool", lambda e, bg=bg, iw=iw: e.indirect_dma_start(
                out=bg[:, :], out_offset=None, in_=bgu_tab[:, :], in_offset=bass.IndirectOffsetOnAxis(ap=iw, axis=0),
                bounds_check=None), [tg("idxw")], [tbg])
            P.dma("pool", lambda e, bd=bd, i=i: e.indirect_dma_start(
                out=bd[:, :], out_offset=None, in_=b_down[0], in_offset=bass.IndirectOffsetOnAxis(ap=bexp_i[:, i:i + 1], axis=0),
                bounds_check=None), [tg("idxw")], [tbd])
            dma("sp", xin, vbuf_d[i * BS:(i + 1) * BS, :].rearrange("(a p) d -> p a d", p=128), [tg("d_vbuf")], [tg("xin")])

        def blk_prep(i):
            wg, wd, bg, bd, xTb, twg, twd, tbg, tbd, txT = blk_bufs(i)
            for a in range(NT):
                pb, pbt = psum()
                pbb = pb[:].bitcast(BF16)
                for kc in range(8):
                    tr(pbb[:, kc * 128:(kc + 1) * 128], xin[:, a, kc * 128:(kc + 1) * 128], ident[:], [tg("xin"), tC], [pbt])
                cp("act", xTb[:, :, a * 128:(a + 1) * 128], pbb.rearrange("p (a b) -> p a b", a=8), [pbt], [txT])

        def blk_gu(i):
            nonlocal ei
            wg, wd, bg, bd, xTb, twg, twd, tbg, tbd, txT = blk_bufs(i)
            for j in range(8):
                pg, pgt = psum()
                for k in range(8):
                    mm(pg[:, 0:BS], wg[:, k, j * 128:(j + 1) * 128], xTb[:, k, :], k == 0, k == 7, [twg, txT], [pgt])
                for k in range(8):
                    mm(pg[:, BS:2 * BS], wg[:, k, D + j * 128:D + (j + 1) * 128], xTb[:, k, :], k == 0, k == 7, [twg, txT], [pgt], skip=True)
                i2 = ei % 2
                ei += 1
                g_, s_, u_ = gcb[i2], sgb[i2], ucb[i2]
                tg_, ts_, tu_ = tg(f"m_g{i2}"), tg(f"m_s{i2}"), tg(f"m_u{i2}")
                ts("dve", g_, pg[:, 0:BS], bg[:, j:j + 1], 7.0, ALU.add, ALU.min, [pgt, tbg], [tg_])
                act(s_, g_, AF.Sigmoid, [tg_], [ts_], scale=1.702)
                tt("dve", s_, g_, s_, ALU.mult, [tg_, ts_], [ts_])
                ts("dve", u_, pg[:, BS:2 * BS], bg[:, 8 + j:9 + j], 7.0, ALU.add, ALU.min, [pgt, tbg], [tu_])
                ts("dve", u_, u_, -7.0, 1.0, ALU.max, ALU.add, [tu_], [tu_])
                tt("dve", actT[:, j, :], u_, s_, ALU.mult, [tu_, ts_], [tg("actT")])

        def blk_down(i):
            nonlocal yi
            wg, wd, bg, bd, xTb, twg, twd, tbg, tbd, txT = blk_bufs(i)
            for a in range(NT):
                ys_, tys2 = yst[yi % 2], tg(f"yst{yi % 2}")
                yi += 1
                for half in range(2):
                    sl = slice(half * 512, (half + 1) * 512)
                    pb, pbt = psum()
                    for k in range(8):
                        mm(pb[:, :], actT[:, k, a * 128:(a + 1) * 128], wd[:, k, sl], k == 0, k == 7, [tg("actT"), twd], [pbt])
                    tt("dve", ys_[:, sl], pb[:, :], bd[:, sl], ALU.add, [pbt, tbd], [tys2])
                dma("sp", ybuf_d[i * BS + a * 128:i * BS + (a + 1) * 128, :], ys_, [tys2], [tg("d_ybuf")])

        blk_load(0)
        blk_prep(0)
        for i in range(NBLK):
            if i + 1 < NBLK:
                blk_load(i + 1)
            blk_gu(i)
            if i + 1 < NBLK:
                blk_prep(i + 1)
            blk_down(i)
        new_phase()
        yk_l = [[carve([128, D], F32) for _ in range(4)] for _ in range(2)]
        x1l = [carve([128, D], F32) for _ in range(2)]
        o8 = [carve([128, D], F32) for _ in range(2)]
        j8 = carve([128, D], BF16)
        s8 = [carve([128, 4], F32) for _ in range(2)]
        for t in range(NCH):
            xt_, ot, sq, yks = x1l[t % 2], o8[t % 2], s8[t % 2], yk_l[t % 2]
            txt, tot_, tsq = tg(f"p8_x{t % 2}"), tg(f"p8_o{t % 2}"), tg(f"p8_s{t % 2}")
            dma("sp", xt_, x1_d[t * 128:(t + 1) * 128, :], [tg("d_x1")], [txt])
            for k in range(4):
                P.dma("pool", lambda e, t=t, k=k, yk=yks[k]: e.indirect_dma_start(
                    out=yk[:, :], out_offset=None, in_=ybuf_d[:, :], in_offset=bass.IndirectOffsetOnAxis(ap=idx_i[:, t * 4 + k:t * 4 + k + 1], axis=0),
                    bounds_check=None), [tg("d_ybuf"), twk], [tg(f"p8_yk{t % 2}_{k}")])
            ts("dve", ot, yks[0], wk_all[:, t, 0:1], None, ALU.mult, None, [tg(f"p8_yk{t % 2}_0"), twk], [tot_])
            for k in range(1, 4):
                stt("dve", ot, yks[k], wk_all[:, t, k:k + 1], ot, ALU.mult, ALU.add, [tg(f"p8_yk{t % 2}_{k}"), twk, tot_], [tot_])
            tt("dve", ot, ot, G2r, ALU.mult, [tot_, tg("rowsC")], [tot_])
            tt("dve", xt_, xt_, ot, ALU.add, [txt, tot_], [txt])
            act(j8, xt_, AF.Square, [txt], [tg("p8_j"), tsq], accum=sq[:, 0:1])
            ts("dve", sq[:, 1:2], sq[:, 0:1], 1.0 / D, EPS, ALU.mult, ALU.add, [tsq], [tsq])
            act(sq[:, 2:3], sq[:, 1:2], AF.Sqrt, [tsq], [tsq])
            P.op("dve", lambda e, sq=sq: e.reciprocal(out=sq[:, 3:4], in_=sq[:, 2:3]), [tsq], [tsq])
            stt("dve", ot, xt_, sq[:, 3:4], Afr, ALU.mult, ALU.mult, [txt, tsq, tg("rowsC")], [tot_])
            tt("dve", ot, ot, SHfr, ALU.add, [tot_, tg("rowsC")], [tot_])
            dma("sp", out_d[t * 128:(t + 1) * 128, :], ot, [tot_], [tg("d_out")])
        P.finalize(nc, st)
    return nc


def _pp(v, n):
    return np.ascontiguousarray(np.asarray(v, np.float32).reshape(n, 128).T)


def make_in_maps(inp):
    f = lambda k: np.asarray(inp[k], np.float32)
    x, c, w_in = f("x"), f("c"), f("w_in")
    conv_w, conv_b = f("ssm_conv_w")[0], f("ssm_conv_b")[0]
    shared = {k: np.ascontiguousarray(f(k)) for k in ("ada_w", "final_ada_w", "w_in", "conf_out_w", "ssm_out_w", "w_o",
                                                       "router_w", "w_gu", "w_down", "b_down")}
    shared["bgu_tab"] = np.ascontiguousarray(f("b_gu")[0].reshape(32, 16, 128).transpose(0, 2, 1).reshape(32 * 128, 16))
    dtb = (f("dt_bias_f")[0], f("dt_bias_b")[0])
    alog = (f("a_log_f")[0], f("a_log_b")[0])
    wdt = (w_in[0][:, C_DTF:C_DTF + 32], w_in[0][:, C_DTB:C_DTB + 32])
    maps = []
    for j in range(8):
        b, s = j // 4, j % 4
        xb = x[b]
        L = xb.shape[0]

        def rows_of(lo, hi):
            o = np.zeros((hi - lo, D), np.float32)
            a, e = max(lo, 0), min(hi, L)
            o[a - lo:e - lo] = xb[a:e]
            return o
        xo = rows_of(T * s - HALO, T * s + T + HALO)
        slots = [(q, 0) for q in range(s)] + [(q, 1) for q in range(3, s, -1)]
        xs3 = np.zeros((3, TS, D), np.float32)
        m3 = np.zeros((3, 4), np.float32)
        wdt3 = np.zeros((3, D, 32), np.float32)
        dtb3 = np.zeros((3, 32), np.float32)
        alog3 = np.zeros((3, 32), np.float32)
        cw3 = np.zeros((128, 3, 20, 5), np.float32)
        for k, (q, d) in enumerate(slots):
            r = rows_of(T * q - 2, T * q + T + 2)
            v = np.array([T * q - 2 >= 0, T * q - 1 >= 0, T * q + T < L, T * q + T + 1 < L], np.float32)
            cw = conv_w[:, :2560]
            if d == 1:
                r, v, cw = r[::-1], v[::-1], cw[::-1]
            xs3[k], m3[k] = r, v
            wdt3[k], dtb3[k], alog3[k] = wdt[d], dtb[d], alog[d]
            cw3[:, k] = cw.T.reshape(20, 128, 5).transpose(1, 0, 2)
        keep = [0.0 if (k == 0 or k == s) else 1.0 for k in range(3)]
        selF = [1.0 if (s >= 1 and k == s - 1) else 0.0 for k in range(3)]
        selB = [1.0 if (s <= 2 and k == 2) else 0.0 for k in range(3)]
        small = np.zeros((128, NS), np.float32)

        def put(name, arr):
            o, w = SM[name]
            small[:, o:o + w] = np.asarray(arr, np.float32).reshape(-1, w) if np.ndim(arr) > 1 else np.tile(np.asarray(arr, np.float32), (128, 1))
        put("cT", _pp(c[b], 8))
        put("adab", _pp(f("ada_b")[0], 48))
        put("finb", _pp(f("final_ada_b"), 16))
        put("gmix", _pp(f("norm_mix_g")[0], 8))
        put("gffn", _pp(f("norm_ffn_g")[0], 8))
        put("gfin", _pp(f("final_norm_g"), 8))
        put("mown", np.concatenate([np.full(16, 1.0 if s > 0 else 0.0), np.full(16, 1.0 if s < 3 else 0.0)]))
        put("m3", m3.reshape(-1))
        put("flg", np.array(keep + selF + selB))
        put("dw", f("conf_dw_w")[0].T.reshape(8, 128, 31).transpose(1, 0, 2).reshape(128, 248))
        put("dwb", _pp(f("conf_dw_b")[0], 8))
        put("lng", _pp(f("conf_ln_g")[0], 8))
        put("lnb", _pp(f("conf_ln_b")[0], 8))
        put("cw", conv_w.T.reshape(24, 128, 5).transpose(1, 0, 2).reshape(128, 120))
        put("cb", _pp(conv_b, 24))
        put("cw3", cw3.reshape(128, 300))
        put("pidx", np.arange(128, dtype=np.float32).reshape(128, 1))
        put("sng", _pp(f("ssm_norm_g")[0], 16))
        rows = np.zeros((1, NR), np.float32)

        def putr(name, arr):
            o, w = RW[name]
            rows[0, o:o + w] = np.asarray(arr, np.float32).reshape(-1)
        putr("cob", f("conf_out_b")[0])
        putr("rb", f("router_b")[0])
        putr("dtb", np.concatenate(dtb))
        putr("alog", np.concatenate(alog))
        putr("ssd", f("ssm_d")[0])
        putr("dtb3", dtb3)
        putr("alog3", alog3)
        putr("bstart", np.arange(NBLK, dtype=np.float32) * BS)
        putr("thr16", np.arange(16, dtype=np.float32) * BS)
        m = dict(shared)
        m.update(xo=xo, xs3=np.ascontiguousarray(xs3), small=small, rows=rows, wdt3=wdt3)
        maps.append(m)
    return maps


_NC_CACHE = {}


def kernel(**inputs):
    if "nc" not in _NC_CACHE:
        _NC_CACHE["nc"] = build()
    nc = _NC_CACHE["nc"]
    maps = make_in_maps(inputs)
    res = run_bass_kernel_spmd(nc, maps, core_ids=list(range(8)))
    out = np.zeros((2, 4 * T, D), np.float32)
    for j in range(8):
        out[j // 4, (j % 4) * T:(j % 4 + 1) * T] = res.results[j]["out"]
    return out
```

```python
import numpy as np
from contextlib import ExitStack
import concourse.bass as bass
import concourse.mybir as mybir
from concourse.bass_utils import run_bass_kernel_spmd

F32 = mybir.dt.float32
BF16 = mybir.dt.bfloat16
I32 = mybir.dt.int32
AF = mybir.ActivationFunctionType
ALU = mybir.AluOpType
AX = mybir.AxisListType

ENGS = ("pe", "act", "dve", "pool", "sp")
NDMA = 12

D = 1024
T = 2048
HALO = 16
TH = T + 2 * HALO
TS = T + 4
NCH = 16
EPS = 1e-6
NEXP = 32
C_A, C_G, C_Z, C_XS, C_B, C_C, C_DTF, C_DTB, C_GC, C_GS = 0, 1024, 2048, 4096, 6144, 6656, 7168, 7200, 7232, 8256


class Tag:
    __slots__ = ("name", "w", "r")

    def __init__(self, name=""):
        self.name = name
        self.w = None
        self.r = {}


class Prog:
    def __init__(self):
        self.ops = {e: [] for e in ENGS}
        self.cnt = {e: 0 for e in ENGS}
        self.seen = {e: {} for e in ENGS}
        self.dma_i = {"sp": 0, "pool": 0, "act": 0}
        self.dma_uses = {}
        self.fence_toks = {}

    def fence(self):
        f = {}
        for e in ENGS:
            if e != "sp" and self.cnt[e] > 0:
                f[("e", e)] = self.cnt[e]
        for k, u in self.dma_uses.items():
            f[k] = 16 * u
        self.fence_toks = f

    def _deps(self, eng, reads, writes):
        deps = dict(self.fence_toks)

        def add(k, v):
            if deps.get(k, 0) < v:
                deps[k] = v
        for t in reads:
            if t.w is not None:
                add(*t.w)
        for t in writes:
            if t.w is not None:
                add(*t.w)
            for k, v in t.r.items():
                add(k, v)
        waits = []
        for k, v in deps.items():
            if k == ("e", "pe") and eng == "pe":
                continue
            if self.seen[eng].get(k, 0) >= v:
                continue
            self.seen[eng][k] = v
            waits.append((k, v))
        return waits

    def _mark(self, tok, reads, writes):
        for t in reads:
            if t.r.get(tok[0], 0) < tok[1]:
                t.r[tok[0]] = tok[1]
        for t in writes:
            t.w = tok
            t.r = {}

    def op(self, eng, fn, reads=(), writes=()):
        waits = self._deps(eng, reads, writes)
        self.cnt[eng] += 1
        tok = (("e", eng), self.cnt[eng])
        self.ops[eng].append((waits, fn, (("e", eng), 1)))
        self._mark(tok, reads, writes)

    def dma(self, q, fn, reads=(), writes=()):
        waits = self._deps(q, reads, writes)
        i = self.dma_i[q]
        self.dma_i[q] += 1
        key = ("d", q, i % NDMA)
        uses = self.dma_uses.get(key, 0)
        if uses > 0 and self.seen[q].get(key, 0) < 16 * uses:
            waits.append((key, 16 * uses))
            self.seen[q][key] = 16 * uses
        self.dma_uses[key] = uses + 1
        tok = (key, 16 * (uses + 1))
        self.ops[q].append((waits, fn, (key, 16)))
        self._mark(tok, reads, writes)

    def finalize(self, nc, stack):
        keys = [("e", e) for e in ENGS if e != "sp"]
        for q in ("sp", "pool", "act"):
            for s in range(NDMA):
                keys.append(("d", q, s))
        sems = {k: stack.enter_context(nc.semaphore("s_" + "_".join(str(x) for x in k))) for k in keys}
        fin = [(k, 16 * u) for k, u in self.dma_uses.items()]
        fin += [(("e", e), self.cnt[e]) for e in ENGS if e != "sp" and self.cnt[e] > 0]
        ops = self.ops

        def replay(name, eng, final=False):
            for waits, fn, inc in ops[name]:
                for k, v in waits:
                    eng.wait_ge(sems[k], v)
                fn(eng).then_inc(sems[inc[0]], inc[1])
            if final:
                for k, v in fin:
                    eng.wait_ge(sems[k], v)

        with nc.Block() as block:
            @block.tensor
            def _(e):
                replay("pe", e)

            @block.scalar
            def _(e):
                replay("act", e)

            @block.vector
            def _(e):
                replay("dve", e)

            @block.gpsimd
            def _(e):
                replay("pool", e)

            @block.sync
            def _(e):
                replay("sp", e, final=True)


def _layout(items):
    off, o = {}, 0
    for n, w in items:
        off[n] = (o, w)
        o += w
    return off, o


SM, NS = _layout([("cT", 8), ("adab", 48), ("finb", 16), ("gmix", 8), ("gffn", 8), ("gfin", 8), ("mown", 32),
                  ("m3", 12), ("flg", 9), ("dw", 248), ("dwb", 8), ("lng", 8), ("lnb", 8), ("cw", 120), ("cb", 24),
                  ("cw3", 300), ("sng", 16), ("pidx", 1)])
BS = 256
NBLK = 8192 // BS + 32
RW, NR = _layout([("cob", 1024), ("rb", 32), ("dtb", 64), ("alog", 64), ("ssd", 32), ("dtb3", 96), ("alog3", 96), ("bstart", NBLK), ("thr16", 16)])


def build(upto=99, debug=False):
    nc = bass.Bass("TRN2", target_bir_lowering=False)
    P = Prog()

    def din(name, shape, dt=F32):
        return nc.dram_tensor(name, list(shape), dt, kind="ExternalInput").ap()

    def dscr(name, shape, dt):
        return nc.dram_tensor(name, list(shape), dt, kind="ExternalOutput" if debug else "Internal").ap()

    dbg_outs = {}

    def dbg(name, ap_sb, shape, dt, reads):
        if not debug:
            return
        d = nc.dram_tensor("dbg_" + name, list(shape), dt, kind="ExternalOutput").ap()
        dbg_outs[name] = d
        P.dma("sp", lambda e: e.dma_start(out=d, in_=ap_sb), reads, [Tag("dbgd")])

    xo = din("xo", [TH, D])
    xs3 = din("xs3", [3, TS, D])
    small_d = din("small", [128, NS])
    rows_d = din("rows", [1, NR])
    ada_w = din("ada_w", [1, D, 6 * D])
    fada_w = din("final_ada_w", [D, 2 * D])
    w_in = din("w_in", [1, D, 9280])
    wdt3 = din("wdt3", [3, D, 32])
    conf_out_w = din("conf_out_w", [1, D, D])
    ssm_out_w = din("ssm_out_w", [1, 2 * D, D])
    w_o = din("w_o", [1, D, D])
    router_w = din("router_w", [1, D, NEXP])
    w_gu = din("w_gu", [1, NEXP, D, 2 * D])
    w_down = din("w_down", [1, NEXP, D, D])
    b_down = din("b_down", [1, NEXP, D])
    bgu_tab = din("bgu_tab", [NEXP * 128, 16])
    out_d = nc.dram_tensor("out", [T, D], F32, kind="ExternalOutput").ap()

    xs_tok = [dscr(f"xs_tok{i}", [T, 2048], BF16) for i in range(4)]
    b_tok = [dscr(f"b_tok{i}", [T, 512], BF16) for i in range(4)]
    bT_d = dscr("bT", [512, T], BF16)
    cT_d = dscr("cTd", [512, T], BF16)
    hF_d = dscr("hF", [NCH, 128, 2048], BF16)
    hB_d = dscr("hB", [NCH, 128, 2048], BF16)
    yconf_d = dscr("yconf", [T, D], BF16)
    ysn_d = dscr("ysn", [T, 2048], BF16)
    x1_d = dscr("x1", [T, D], F32)
    vbuf_d = dscr("vbuf", [NBLK * BS, D], BF16)
    ybuf_d = dscr("ybuf", [NBLK * BS, D], F32)
    wgu_bf = nc.dram_tensor("wgu_bf", [NEXP * 128, 8 * 2 * D], BF16, kind="Internal").ap()
    wdn_bf = nc.dram_tensor("wdn_bf", [NEXP * 128, 8 * D], BF16, kind="Internal").ap()

    with ExitStack() as st:
        def sb(name, shape, dt):
            return st.enter_context(nc.sbuf_tensor("sb_" + name, list(shape), dt))

        tags = {}

        def tg(name):
            if name not in tags:
                tags[name] = Tag(name)
            return tags[name]

        banks = [st.enter_context(nc.psum_tensor(f"ps{i}", [128, 512], F32)) for i in range(8)]
        bank_i = [0]

        bank_rng = [0, 8]

        def psum():
            lo, hi = bank_rng
            i = lo + bank_i[0] % (hi - lo)
            bank_i[0] += 1
            return banks[i], tg(f"ps{i}")

        small = sb("small", [128, NS], F32)
        rows = sb("rows", [128, NR], F32)
        identf = sb("identf", [128, 128], F32)
        ident = sb("ident", [128, 128], BF16)
        onesf = sb("onesf", [128, 128], F32)
        onesb = sb("onesb", [128, 128], BF16)
        tri_f = sb("tri_f", [128, 128], F32)
        tri_b = sb("tri_b", [128, 128], F32)
        ustr_f = sb("ustr_f", [128, 128], F32)
        ustr_b = sb("ustr_b", [128, 128], F32)
        mk_f = sb("mk_f", [128, 128], BF16)
        mk_b = sb("mk_b", [128, 128], BF16)
        adaP = sb("adaP", [128, 64], F32)
        modP = sb("modP", [128, 24], F32)
        rowsC = sb("rowsC", [128, 6, D], F32)
        uT = sb("uT", [128, 8, TH], BF16)
        wbuf = [sb(f"wbuf{i}", [128, 8, 512], BF16) for i in range(3)]
        wbuf_i = [0]
        ARENA = 101 * 1024
        arena = sb("arena", [128, ARENA // 4], F32)
        ar_off = [0]

        def carve(shape, dt):
            n = int(np.prod(shape[1:]))
            nb = n * (4 if dt == F32 else 2)
            nb = (nb + 63) // 64 * 64
            o = ar_off[0]
            assert o + nb <= ARENA, (o, nb, ARENA)
            ar_off[0] = o + nb
            ap = arena[0:shape[0], o // 4:(o + nb) // 4]
            if dt != F32:
                ap = ap.bitcast(dt)
            ap = ap[:, 0:n]
            if len(shape) == 3:
                ap = ap.rearrange("p (a b) -> p a b", a=shape[1])
            elif len(shape) == 4:
                ap = ap.rearrange("p (a b c) -> p a b c", a=shape[1], b=shape[2])
            return ap

        def new_phase():
            ar_off[0] = 0
            P.fence()

        def S(name, i=0, n=1):
            o, w = SM[name]
            return small[:, o + i:o + i + n]

        def Rr(name, i=0, n=None):
            o, w = RW[name]
            n = w - i if n is None else n
            return rows[:, o + i:o + i + n]

        def mm(out, lhsT, rhs, start, stop, reads, writes, skip=False):
            P.op("pe", lambda e: e.matmul(out, lhsT=lhsT, rhs=rhs, start=start, stop=stop, skip_group_check=skip), reads, writes)

        def tr(out, in_, idt, reads, writes):
            P.op("pe", lambda e: e.transpose(out=out, in_=in_, identity=idt), reads, writes)

        def act(out, in_, func, reads, writes, bias=None, scale=None, accum=None):
            kw = {}
            if bias is not None:
                kw["bias"] = bias
            if scale is not None:
                kw["scale"] = scale
            if accum is not None:
                kw["accum_out"] = accum
            P.op("act", lambda e: e.activation(out=out, in_=in_, func=func, **kw), reads, writes)

        def tt(eng, out, in0, in1, op, reads, writes):
            P.op(eng, lambda e: e.tensor_tensor(out=out, in0=in0, in1=in1, op=op), reads, writes)

        def ts(eng, out, in0, s1, s2, op0, op1, reads, writes):
            if op1 is None:
                P.op(eng, lambda e: e.tensor_scalar(out=out, in0=in0, scalar1=s1, scalar2=None, op0=op0), reads, writes)
            else:
                P.op(eng, lambda e: e.tensor_scalar(out=out, in0=in0, scalar1=s1, scalar2=s2, op0=op0, op1=op1), reads, writes)

        def stt(eng, out, in0, sc, in1, op0, op1, reads, writes):
            P.op(eng, lambda e: e.scalar_tensor_tensor(out=out, in0=in0, scalar=sc, in1=in1, op0=op0, op1=op1), reads, writes)

        def cp(eng, out, in_, reads, writes):
            if eng == "act":
                P.op("act", lambda e: e.activation(out=out, in_=in_, func=AF.Copy), reads, writes)
            else:
                P.op(eng, lambda e: e.tensor_copy(out=out, in_=in_), reads, writes)

        def dma(q, out, in_, reads, writes):
            P.dma(q, lambda e: e.dma_start(out=out, in_=in_), reads, writes)

        def load_w(src, ncols=512):
            i = wbuf_i[0] % 3
            wbuf_i[0] += 1
            t = tg(f"wbuf{i}")
            dma("pool", wbuf[i][:, :, 0:ncols], src.rearrange("(kc p) n -> p kc n", p=128), [], [t])
            return wbuf[i], t

        tC = tg("const")
        dma("sp", small[:], small_d, [], [tC])
        dma("sp", rows[:], rows_d.partition_broadcast(128), [], [tC])
        P.op("pool", lambda e: e.memset(identf[:], 0.0), [], [tC])
        P.op("pool", lambda e: e.affine_select(out=identf[:], in_=identf[:], pattern=[[-1, 128]], compare_op=ALU.not_equal,
                                               fill=1.0, base=0, channel_multiplier=1), [tC], [tC])
        cp("pool", ident[:], identf[:], [tC], [tC])
        P.op("pool", lambda e: e.memset(onesf[:], 1.0), [], [tC])
        cp("pool", onesb[:], onesf[:], [tC], [tC])
        P.op("pool", lambda e: e.affine_select(out=tri_f[:], in_=onesf[:], pattern=[[1, 128]], compare_op=ALU.is_ge,
                                               fill=0.0, base=0, channel_multiplier=-1), [tC], [tC])
        P.op("pool", lambda e: e.affine_select(out=tri_b[:], in_=onesf[:], pattern=[[-1, 128]], compare_op=ALU.is_ge,
                                               fill=0.0, base=0, channel_multiplier=1), [tC], [tC])
        P.op("pool", lambda e: e.affine_select(out=ustr_f[:], in_=onesf[:], pattern=[[-1, 128]], compare_op=ALU.is_gt,
                                               fill=0.0, base=0, channel_multiplier=1), [tC], [tC])
        P.op("pool", lambda e: e.affine_select(out=ustr_b[:], in_=onesf[:], pattern=[[1, 128]], compare_op=ALU.is_gt,
                                               fill=0.0, base=0, channel_multiplier=-1), [tC], [tC])
        cp("pool", mk_f[:], tri_f[:], [tC], [tC])
        cp("pool", mk_b[:], tri_b[:], [tC], [tC])

        new_phase()
        cact = carve([128, 8], BF16)
        wA = [carve([128, 8, 512], BF16) for _ in range(3)]
        tA = tg("adaP")
        act(cact, S("cT", 0, 8), AF.Silu, [tC], [tg("cact")])
        pa, pat = psum()
        for blk in range(16):
            src = ada_w[0][:, blk * 512:(blk + 1) * 512] if blk < 12 else fada_w[:, (blk - 12) * 512:(blk - 11) * 512]
            wt = tg(f"wA{blk % 3}")
            dma("pool", wA[blk % 3], src.rearrange("(kc p) n -> p kc n", p=128), [], [wt])
            for cc in range(4):
                col = blk * 4 + cc
                for kc in range(8):
                    mm(pa[:, col:col + 1], wA[blk % 3][:, kc, cc * 128:(cc + 1) * 128], cact[:, kc:kc + 1], kc == 0, kc == 7,
                       [wt, tg("cact")], [pat])
        tt("dve", adaP[:, 0:48], pa[:, 0:48], S("adab", 0, 48), ALU.add, [pat, tC], [tA])
        tt("dve", adaP[:, 48:64], pa[:, 48:64], S("finb", 0, 16), ALU.add, [pat, tC], [tA])
        for i, (gname, c0) in enumerate((("gmix", 8), ("gffn", 32), ("gfin", 56))):
            stt("dve", modP[:, i * 8:(i + 1) * 8], adaP[:, c0:c0 + 8], 1.0, S(gname, 0, 8), ALU.add, ALU.mult, [tA, tC], [tg("modP")])
        A1, A2, Af = modP[:, 0:8], modP[:, 8:16], modP[:, 16:24]
        sh1, sh2 = adaP[:, 0:8], adaP[:, 24:32]
        dg = carve([128, 128], F32)
        for ri, vec in enumerate((adaP[:, 16:24], adaP[:, 40:48], Af, adaP[:, 48:56], A2, sh2)):
            for half in range(2):
                pr, prt = psum()
                for q in range(4):
                    kc = half * 4 + q
                    ts("dve", dg, identf[:], vec[:, kc:kc + 1], None, ALU.mult, None, [tC, tA, tg("modP")], [tg("dg")])
                    mm(pr[:, q * 128:(q + 1) * 128], onesf[:], dg, True, True, [tC, tg("dg")], [prt])
                cp("act", rowsC[:, ri, half * 512:(half + 1) * 512], pr[:, :], [prt], [tg("rowsC")])
        G1r, G2r, Afr, SHfr, A2r, SH2r = (rowsC[:, i, :] for i in range(6))
        if upto <= 0:
            dbg("adaP", adaP[:], [128, 64], F32, [tA])
            dbg("rowsC", rowsC[:], [128, 6, D], F32, [tg("rowsC")])
            dbg("rows", rows[:], [128, NR], F32, [tC])
            dbg("tri", tri_f[:], [128, 128], F32, [tC])
            P.finalize(nc, st)
            return nc

        def norm_T(src, n_tok, A_pp, sh_pp, dstT, dst_tag, bufs, want_f32=None):
            xt_l, junk, xn_l, st_l = bufs
            nt = (n_tok + 127) // 128

            def s1(t):
                r = min(128, n_tok - t * 128)
                xt, sq = xt_l[t % 2], st_l[t % 2]
                txt, tsq = tg(f"nT_xt{t % 2}"), tg(f"nT_sq{t % 2}")
                dma("sp", xt[0:r, :], src[t * 128:t * 128 + r, :], [], [txt])
                act(junk[0:r, :], xt[0:r, :], AF.Square, [txt], [tg("nT_junk"), tsq], accum=sq[0:r, 0:1])
                ts("dve", sq[0:r, 1:2], sq[0:r, 0:1], 1.0 / D, EPS, ALU.mult, ALU.add, [tsq], [tsq])
                act(sq[0:r, 2:3], sq[0:r, 1:2], AF.Sqrt, [tsq], [tsq])
                P.op("dve", lambda e, sq=sq, r=r: e.reciprocal(out=sq[0:r, 3:4], in_=sq[0:r, 2:3]), [tsq], [tsq])

            def s2(t):
                r = min(128, n_tok - t * 128)
                xt, xn, sq = xt_l[t % 2], xn_l[t % 2], st_l[t % 2]
                txt, txn, tsq = tg(f"nT_xt{t % 2}"), tg(f"nT_xn{t % 2}"), tg(f"nT_sq{t % 2}")
                act(xn[0:r, :], xt[0:r, :], AF.Copy, [txt, tsq], [txn], scale=sq[0:r, 3:4])
                pb, pbt = psum()
                pbb = pb[:].bitcast(BF16)
                for kc in range(8):
                    tr(pbb[:, kc * 128:kc * 128 + r], xn[0:r, kc * 128:(kc + 1) * 128], ident[0:r, 0:r], [txn, tC], [pbt])
                for kc in range(8):
                    ts("dve", dstT[:, kc, t * 128:t * 128 + r], pbb[:, kc * 128:kc * 128 + r], A_pp[:, kc:kc + 1], sh_pp[:, kc:kc + 1],
                       ALU.mult, ALU.add, [pbt, tA, tg("modP")], [dst_tag])

            s1(0)
            for t in range(nt):
                if t + 1 < nt:
                    s1(t + 1)
                s2(t)

        def blocks(W):
            out, t0 = [], 0
            while t0 < W:
                n = min(512, W - t0)
                out.append((t0, n))
                t0 += n
            return out

        def front(seq, srcT, src_tag, W, off, nchunk, col0, cw_name, cw_base, mL, mR, nm, bufs, hook=None):
            raw_l, cv_l, dg5, stage_l = bufs
            wstate = {}

            def stA(ch):
                if hook is not None:
                    hook()
                if ch % 4 == 0:
                    wstate["w"] = load_w(w_in[0][:, col0 + ch * 128:col0 + ch * 128 + 512])
                wtile, wt = wstate["w"]
                raw = raw_l[ch % 2]
                traw = tg(f"fr_raw{ch % 2}")
                for bi, (t0, n) in enumerate(blocks(W)):
                    pb, pbt = psum()
                    for k in range(8):
                        mm(pb[:, 0:n], wtile[:, k, (ch % 4) * 128:(ch % 4 + 1) * 128], srcT[:, k, t0:t0 + n], k == 0, k == 7,
                           [wt, src_tag], [pbt])
                    cp("act" if bi % 2 == 0 else "dve", raw[:, t0:t0 + n], pb[:, 0:n], [pbt], [traw])
                tt("dve", raw[:, 0:nm], raw[:, 0:nm], mL, ALU.mult, [traw, tC], [traw])
                tt("dve", raw[:, W - nm:W], raw[:, W - nm:W], mR, ALU.mult, [traw, tC], [traw])

            def stB(ch):
                raw = raw_l[ch % 2]
                traw = tg(f"fr_raw{ch % 2}")
                tdg = tg("fr_dg")
                for k in range(5):
                    ts("dve", dg5[:, k, :], ident[:], S(cw_name, cw_base + ch * 5 + k, 1), None, ALU.mult, None, [tC], [tdg])
                cv = cv_l[ch % 2]
                tcv = tg(f"fr_cv{ch % 2}")
                for tb in range(4):
                    pb, pbt = psum()
                    for k in range(5):
                        s0 = off - 2 + k + tb * 512
                        mm(pb[:, :], dg5[:, k, :], raw[:, s0:s0 + 512], k == 0, k == 4, [tdg, traw], [pbt])
                    act(cv[:, tb * 512:(tb + 1) * 512], pb[:, :], AF.Silu, [pbt, tC], [tcv], bias=S("cb", ch, 1))

            def stC(ch):
                cv = cv_l[ch % 2]
                tcv = tg(f"fr_cv{ch % 2}")
                if ch < 20:
                    stg = stage_l[ch % 2]
                    tstg = tg(f"fr_stg{ch % 2}")
                    for q in range(4):
                        pb, pbt = psum()
                        pbb = pb[:].bitcast(BF16)
                        for i in range(4):
                            tl = q * 4 + i
                            tr(pbb[:, i * 128:(i + 1) * 128], cv[:, tl * 128:(tl + 1) * 128], ident[:], [tcv, tC], [pbt])
                        cp("dve" if q % 2 == 0 else "act", stg[:, q * 4:(q + 1) * 4, :], pbb[:, 0:512].rearrange("p (a b) -> p a b", a=4),
                           [pbt], [tstg])
                    if ch < 16:
                        dst = xs_tok[seq].rearrange("(t p) c -> p t c", p=128)[:, :, ch * 128:(ch + 1) * 128]
                    else:
                        dst = b_tok[seq].rearrange("(t p) c -> p t c", p=128)[:, :, (ch - 16) * 128:(ch - 15) * 128]
                    dma("sp", dst, stg[:, :, :], [tstg], [tg(f"d_tok{seq}")])
                if seq == 3 and ch >= 16:
                    dstd = bT_d if ch < 20 else cT_d
                    c4 = (ch - 16) % 4
                    dma("sp", dstd[c4 * 128:(c4 + 1) * 128, :], cv[:, :], [tcv], [tg("d_bc")])

            for step in range(nchunk + 2):
                if step < nchunk:
                    stA(step)
                if 0 <= step - 1 < nchunk:
                    stB(step - 1)
                if 0 <= step - 2 < nchunk:
                    stC(step - 2)

        def dt_pass(srcT, src_tag, off, wdt_tile, wdt_tag, ncol, bias_row, dst, dst_tag, tmp):
            for t in range(NCH):
                pb, pbt = psum()
                for k in range(8):
                    mm(pb[:, 0:ncol], srcT[:, k, off + t * 128:off + (t + 1) * 128], wdt_tile[:, k, 0:ncol], k == 0, k == 7,
                       [src_tag, wdt_tag], [pbt])
                tt("dve", tmp[:, 0:ncol], pb[:, 0:ncol], bias_row, ALU.add, [pbt, tC], [tg("dt_tmp")])
                act(tmp[:, 0:ncol], tmp[:, 0:ncol], AF.Exp, [tg("dt_tmp")], [tg("dt_tmp")])
                act(dst[:, t, 0:ncol], tmp[:, 0:ncol], AF.Ln, [tg("dt_tmp")], [dst_tag], bias=1.0)

        new_phase()
        nb = ([carve([128, D], F32) for _ in range(2)], carve([128, D], BF16), [carve([128, D], BF16) for _ in range(2)],
              [carve([128, 4], F32) for _ in range(2)])
        tuT = tg("uT")
        norm_T(xo, TH, A1, sh1, uT, tuT, nb)
        dbg("adaP", adaP[:], [128, 64], F32, [tA])
        dbg("rowsC", rowsC[:], [128, 6, D], F32, [tg("rowsC")])
        dbg("uT", uT[:], [128, 8, TH], BF16, [tuT])
        if upto <= 1:
            P.finalize(nc, st)
            return nc

        pre_jobs = []
        for e in range(NEXP):
            for blk in range(4):
                pre_jobs.append((w_gu[0][e][:, blk * 512:(blk + 1) * 512],
                                 wgu_bf[e * 128:(e + 1) * 128, :].rearrange("p (kc n) -> p kc n", kc=8)[:, :, blk * 512:(blk + 1) * 512]))
            for blk in range(2):
                pre_jobs.append((w_down[0][e][:, blk * 512:(blk + 1) * 512],
                                 wdn_bf[e * 128:(e + 1) * 128, :].rearrange("p (kc n) -> p kc n", kc=8)[:, :, blk * 512:(blk + 1) * 512]))
        pre_i = [0]
        pre_pending = [None]
        pstage = []

        def prepass_step(n=1):
            for _ in range(n):
                if pre_pending[0] is not None:
                    buf, tag, dst = pre_pending[0]
                    dma("sp", dst, buf, [tag], [tg("d_wbf")])
                    pre_pending[0] = None
                if pre_i[0] < len(pre_jobs):
                    src, dst = pre_jobs[pre_i[0]]
                    buf, tname = pstage[pre_i[0] % len(pstage)]
                    pre_i[0] += 1
                    tag = tg(tname)
                    dma("pool", buf, src.rearrange("(kc p) n -> p kc n", p=128), [], [tag])
                    pre_pending[0] = (buf, tag, dst)

        def prepass_flush_pending():
            if pre_pending[0] is not None:
                buf, tag, dst = pre_pending[0]
                dma("sp", dst, buf, [tag], [tg("d_wbf")])
                pre_pending[0] = None

        new_phase()
        pstage[:] = [(carve([128, 8, 512], BF16), f"pstage{i}") for i in range(2)]
        hcT = carve([128, 8, T], BF16)
        sg_l = [carve([128, TH], BF16) for _ in range(2)]
        h_l = [carve([128, TH], BF16) for _ in range(2)]
        dg31 = carve([128, 31, 128], BF16)
        sqb = carve([128, 8, 512], BF16)
        lnA = carve([128, 512], F32)
        lnB = carve([128, 512], F32)
        lnC = carve([128, 512], F32)
        lnT = [carve([128, 512], F32) for _ in range(2)]
        ycs = [carve([128, D], BF16) for _ in range(2)]
        for j in range(8):
            prepass_step(5)
            if j % 4 == 0:
                wg_t, wg_tag = load_w(w_in[0][:, C_G + j * 128:C_G + j * 128 + 512])
                wa_t, wa_tag = load_w(w_in[0][:, C_A + j * 128:C_A + j * 128 + 512])
            sg, h = sg_l[j % 2], h_l[j % 2]
            tsg, th = tg(f"cf_sg{j % 2}"), tg(f"cf_h{j % 2}")
            for (t0, n) in blocks(TH):
                pb, pbt = psum()
                for k in range(8):
                    mm(pb[:, 0:n], wg_t[:, k, (j % 4) * 128:(j % 4 + 1) * 128], uT[:, k, t0:t0 + n], k == 0, k == 7, [wg_tag, tuT], [pbt])
                act(sg[:, t0:t0 + n], pb[:, 0:n], AF.Sigmoid, [pbt], [tsg])
            for (t0, n) in blocks(TH):
                pb, pbt = psum()
                for k in range(8):
                    mm(pb[:, 0:n], wa_t[:, k, (j % 4) * 128:(j % 4 + 1) * 128], uT[:, k, t0:t0 + n], k == 0, k == 7, [wa_tag, tuT], [pbt])
                tt("dve", h[:, t0:t0 + n], pb[:, 0:n], sg[:, t0:t0 + n], ALU.mult, [pbt, tsg], [th])
            tt("dve", h[:, 0:16], h[:, 0:16], S("mown", 0, 16), ALU.mult, [th, tC], [th])
            tt("dve", h[:, TH - 16:TH], h[:, TH - 16:TH], S("mown", 16, 16), ALU.mult, [th, tC], [th])
            tdg = tg("cf_dg")
            for k in range(31):
                ts("dve", dg31[:, k, :], ident[:], S("dw", j * 31 + k, 1), None, ALU.mult, None, [tC], [tdg])
            for tb in range(4):
                pb, pbt = psum()
                for k in range(31):
                    s0 = HALO - 15 + k + tb * 512
                    mm(pb[:, :], dg31[:, k, :], h[:, s0:s0 + 512], k == 0, k == 30, [tdg, th], [pbt])
                act(hcT[:, j, tb * 512:(tb + 1) * 512], pb[:, :], AF.Identity, [pbt, tC], [tg(f"hcT{tb}")], bias=S("dwb", j, 1))
        for tb in range(4):
            thc = tg(f"hcT{tb}")
            sl = slice(tb * 512, (tb + 1) * 512)
            tt("dve", sqb[:, :, :], hcT[:, :, sl], hcT[:, :, sl], ALU.mult, [thc], [tg("sqb")])
            p1, p1t = psum()
            for j in range(8):
                mm(p1[:, :], onesb[:], hcT[:, j, sl], j == 0, j == 7, [tC, thc], [p1t])
            p2, p2t = psum()
            for j in range(8):
                mm(p2[:, :], onesb[:], sqb[:, j, :], j == 0, j == 7, [tC, tg("sqb")], [p2t])
            tln = tg("ln")
            ts("dve", lnA, p1[:, :], 1.0 / D, None, ALU.mult, None, [p1t], [tln])
            tt("dve", lnB, lnA, lnA, ALU.mult, [tln], [tln])
            stt("dve", lnB, p2[:, :], 1.0 / D, lnB, ALU.mult, ALU.subtract, [p2t, tln], [tln])
            ts("dve", lnB, lnB, EPS, None, ALU.add, None, [tln], [tln])
            act(lnB, lnB, AF.Sqrt, [tln], [tln])
            P.op("dve", lambda e: e.reciprocal(out=lnB, in_=lnB), [tln], [tln])
            tt("dve", lnC, lnA, lnB, ALU.mult, [tln], [tln])
            for j in range(8):
                lt = lnT[j % 2]
                tlt = tg(f"lnT{j % 2}")
                tt("dve", lt, hcT[:, j, sl], lnB, ALU.mult, [thc, tln], [tlt])
                tt("dve", lt, lt, lnC, ALU.subtract, [tlt, tln], [tlt])
                act(hcT[:, j, sl], lt, AF.Silu, [tlt, tC], [thc], bias=S("lnb", j, 1), scale=S("lng", j, 1))
        prepass_flush_pending()
        wco_h = [load_w(conf_out_w[0][:, h_ * 512:(h_ + 1) * 512]) for h_ in range(2)]
        for t in range(NCH):
            yc = ycs[t % 2]
            tyc = tg(f"ycs{t % 2}")
            for half in range(2):
                pb, pbt = psum()
                for k in range(8):
                    mm(pb[:, :], hcT[:, k, t * 128:(t + 1) * 128], wco_h[half][0][:, k, :], k == 0, k == 7,
                       [tg(f"hcT{t // 4}"), wco_h[half][1]], [pbt])
                tt("dve", yc[:, half * 512:(half + 1) * 512], pb[:, :], Rr("cob", half * 512, 512), ALU.add, [pbt, tC], [tyc])
            dma("sp", yconf_d[t * 128:(t + 1) * 128, :], yc[:, :], [tyc], [tg("d_yconf")])
        if upto <= 2:
            P.finalize(nc, st)
            return nc

        new_phase()
        pstage[:] = [(carve([128, 8, 512], BF16), f"pstage{i}") for i in range(2)]
        fb = ([carve([128, TH], BF16) for _ in range(2)], [carve([128, T], BF16) for _ in range(2)], carve([128, 5, 128], BF16),
              [carve([128, 16, 128], BF16) for _ in range(2)])
        dt_own = sb("dt_own", [128, NCH, 64], F32)
        dt_sl = sb("dt_sl", [128, 3, NCH, 32], F32)
        dt_tmp = sb("dt_tmp", [128, 64], F32)
        wdt_t = carve([128, 8, 64], BF16)
        twdt = tg("wdt")
        dma("pool", wdt_t, w_in[0][:, C_DTF:C_DTF + 64].rearrange("(kc p) n -> p kc n", p=128), [], [twdt])
        front(3, uT, tuT, TH, HALO, 24, C_XS, "cw", 0, S("mown", 0, 16), S("mown", 16, 16), 16, fb, hook=prepass_step)
        dt_pass(uT, tuT, HALO, wdt_t, twdt, 64, Rr("dtb"), dt_own, tg("dt_own"), dt_tmp)
        if upto <= 3:
            dbg("dt_own", dt_own[:], [128, NCH, 64], F32, [tg("dt_own")])
            P.finalize(nc, st)
            return nc
        uTs = carve([128, 8, TS], BF16)
        nb3 = ([carve([128, D], F32) for _ in range(2)], carve([128, D], BF16), [carve([128, D], BF16) for _ in range(2)],
               [carve([128, 4], F32) for _ in range(2)])
        wdt_s = carve([128, 8, 32], BF16)
        for k in range(3):
            tus = tg("uTs")
            norm_T(xs3[k], TS, A1, sh1, uTs, tus, nb3)
            dma("pool", wdt_s, wdt3[k].rearrange("(kc p) n -> p kc n", p=128), [], [tg("wdts")])
            front(k, uTs, tus, TS, 2, 20, C_XS, "cw3", k * 100, S("m3", k * 4, 2), S("m3", k * 4 + 2, 2), 2, fb, hook=prepass_step)
            dt_pass(uTs, tus, 2, wdt_s, tg("wdts"), 32, Rr("dtb3", k * 32, 32), dt_sl[:, k], tg("dt_sl"), dt_tmp)

        prepass_flush_pending()
        new_phase()
        pstage[:] = [(wbuf[i][:, :, :], f"wbuf{i}") for i in range(3)]
        Arow = carve([128, 64], F32)
        Arow3 = carve([128, 96], F32)
        act(Arow, Rr("alog"), AF.Exp, [tC], [tg("Arow")])
        ts("dve", Arow, Arow, -1.0, None, ALU.mult, None, [tg("Arow")], [tg("Arow")])
        act(Arow3, Rr("alog3"), AF.Exp, [tC], [tg("Arow")])
        ts("dve", Arow3, Arow3, -1.0, None, ALU.mult, None, [tg("Arow")], [tg("Arow")])
        xs_c = [carve([128, 2048], BF16) for _ in range(2)]
        b_c = [carve([128, 512], BF16) for _ in range(2)]
        xdtd = [carve([128, 2048], BF16) for _ in range(2)]
        sm_l = [carve([128, 6, 32], F32) for _ in range(2)]
        Rst = carve([128, 2048], F32)
        hFs = carve([128, 2048], F32)
        hBs = carve([128, 2048], F32)
        hbf = [carve([128, 2048], BF16) for _ in range(2)]
        cs_i = [0]

        def cs_front(seq, c, dt_ap, A_ap, tri):
            i = cs_i[0] % 2
            cs_i[0] += 1
            if cs_i[0] % 2 == 0:
                prepass_step(1)
            xc, bc, xd, smt = xs_c[i], b_c[i], xdtd[i], sm_l[i]
            txc, tsm, txd = tg(f"cs_x{i}"), tg(f"cs_sm{i}"), tg(f"cs_xd{i}")
            dma("sp", xc, xs_tok[seq][c * 128:(c + 1) * 128, :], [tg(f"d_tok{seq}")], [txc])
            dma("sp", bc, b_tok[seq][c * 128:(c + 1) * 128, :], [tg(f"d_tok{seq}")], [txc])
            a_t, tot, dec, cdb, sc = smt[:, 0, :], smt[:, 1, :], smt[:, 2, :], smt[:, 3, :], smt[:, 4, :]
            tt("dve", a_t, dt_ap, A_ap, ALU.mult, [tg("dt_own"), tg("dt_sl"), tg("Arow")], [tsm])
            pa_, pat_ = psum()
            mm(pa_[:, 0:32], tri[:], a_t, True, True, [tC, tsm], [pat_])
            mm(pa_[:, 32:64], onesf[:], a_t, True, True, [tC, tsm], [pat_])
            cp("act", tot, pa_[:, 32:64], [pat_], [tsm])
            tt("dve", dec, tot, pa_[:, 0:32], ALU.subtract, [pat_, tsm], [tsm])
            act(dec, dec, AF.Exp, [tsm], [tsm])
            act(cdb, tot, AF.Exp, [tsm], [tsm])
            tt("dve", sc, dec, dt_ap, ALU.mult, [tsm, tg("dt_own"), tg("dt_sl")], [tsm])
            tt("dve", xd.rearrange("p (h d) -> p h d", h=32), xc.rearrange("p (h d) -> p h d", h=32),
               sc.unsqueeze(2).to_broadcast([128, 32, 64]), ALU.mult, [txc, tsm], [txd])
            return bc, xd, cdb, txc, txd, tsm

        def cs_back(ctx, Racc, tR):
            bc, xd, cdb, txc, txd, tsm = ctx
            for g in range(4):
                pb, pbt = psum()
                mm(pb[:, :], bc[:, g * 128:(g + 1) * 128], xd[:, g * 512:(g + 1) * 512], True, True, [txc, txd], [pbt])
                Rg = Racc[:, g * 512:(g + 1) * 512]
                tt("dve", Rg.rearrange("p (h d) -> p h d", h=8), Rg.rearrange("p (h d) -> p h d", h=8),
                   cdb[:, g * 8:(g + 1) * 8].unsqueeze(2).to_broadcast([128, 8, 64]), ALU.mult, [tR, tsm], [tR])
                tt("dve", Rg, Rg, pb[:, :], ALU.add, [tR, pbt], [tR])

        tRs, tHF, tHB = tg("Rst"), tg("hFs"), tg("hBs")
        P.op("pool", lambda e: e.memset(Rst, 0.0), [], [tRs])
        P.op("pool", lambda e: e.memset(hFs, 0.0), [], [tHF])
        P.op("pool", lambda e: e.memset(hBs, 0.0), [], [tHB])
        items = []
        for k in range(3):
            for c in range(NCH):
                pre = (lambda k=k: ts("dve", Rst, Rst, S("flg", k, 1), None, ALU.mult, None, [tRs, tC], [tRs])) if c == 0 else None

                def post(k=k):
                    stt("dve", hFs, Rst, S("flg", 3 + k, 1), hFs, ALU.mult, ALU.add, [tRs, tHF, tC], [tHF])
                    stt("dve", hBs, Rst, S("flg", 6 + k, 1), hBs, ALU.mult, ALU.add, [tRs, tHB, tC], [tHB])
                items.append(((k, c, dt_sl[:, k, c, :], Arow3[:, k * 32:(k + 1) * 32], tri_f), Rst, tRs, pre, post if c == NCH - 1 else None))
        for c in range(NCH):
            def pre(c=c):
                hb_ = hbf[c % 2]
                cp("act", hb_, hFs, [tHF], [tg(f"hbf{c % 2}")])
                dma("sp", hF_d[c], hb_, [tg(f"hbf{c % 2}")], [tg("d_hF")])
            items.append(((3, c, dt_own[:, c, 0:32], Arow[:, 0:32], tri_f), hFs, tHF, pre, None))
        for c in range(NCH - 1, -1, -1):
            def pre(c=c):
                hb_ = hbf[c % 2]
                cp("act", hb_, hBs, [tHB], [tg(f"hbf{c % 2}")])
                dma("sp", hB_d[c], hb_, [tg(f"hbf{c % 2}")], [tg("d_hB")])
            items.append(((3, c, dt_own[:, c, 32:64], Arow[:, 32:64], tri_b), hBs, tHB, pre, None))

        def run_back(it, ctx):
            _, Racc, tR, pre, post = it
            if pre is not None:
                pre()
            cs_back(ctx, Racc, tR)
            if post is not None:
                post()
        prev = None
        for it in items:
            ctx = cs_front(*it[0])
            if prev is not None:
                run_back(*prev)
            prev = (it, ctx)
        run_back(*prev)
        if upto <= 4:
            P.finalize(nc, st)
            return nc

        new_phase()
        Arow = carve([128, 64], F32)
        act(Arow, Rr("alog"), AF.Exp, [tC], [tg("Arow")])
        ts("dve", Arow, Arow, -1.0, None, ALU.mult, None, [tg("Arow")], [tg("Arow")])
        wz = carve([128, 8, 2048], BF16)
        twz = tg("wz")
        for q in range(4):
            dma("pool", wz[:, :, q * 512:(q + 1) * 512], w_in[0][:, C_Z + q * 512:C_Z + (q + 1) * 512].rearrange("(kc p) n -> p kc n", p=128),
                [], [twz])
        xsc_l = [carve([128, 2048], BF16) for _ in range(2)]
        bTc_l = [carve([128, 4, 128], BF16) for _ in range(2)]
        cTc_l = [carve([128, 4, 128], BF16) for _ in range(2)]
        hst = [carve([128, 2048], BF16) for _ in range(2)]
        xdt = [carve([128, 2048], BF16) for _ in range(2)]
        rsegp = [carve([128, 4, 128], F32) for _ in range(3)]
        eseg = [carve([128, 512], BF16) for _ in range(2)]
        MTl = [carve([128, 4, 128], BF16) for _ in range(2)]
        CBm = [carve([128, 4, 128], BF16) for _ in range(2)]
        yo = carve([128, 2048], F32)
        ytmp_l = [carve([128, 512], F32) for _ in range(2)]
        szl = [carve([128, 512], BF16) for _ in range(4)]
        ysl = [carve([128, 2048], BF16) for _ in range(2)]
        sm5 = carve([128, 8, 32], F32)
        ss5 = carve([128, 8], F32)
        jk5 = carve([128, 512], BF16)
        tris = (tri_f, tri_b)
        ustrs = (ustr_f, ustr_b)
        mks = (mk_f, mk_b)
        hds = (hF_d, hB_d)
        rs_i = 0
        yt_i = 0
        for c in range(NCH):
            prepass_step(2)
            bank_rng[:] = [4, 8]
            cb = c % 2
            xsc, bTc, cTc = xsc_l[cb], bTc_l[cb], cTc_l[cb]
            tx, tbc, tyo = tg(f"p5_x{cb}"), tg(f"p5_bc{cb}"), tg("p5_yo")
            dma("sp", xsc, xs_tok[3][c * 128:(c + 1) * 128, :], [tg("d_tok3")], [tx])
            dma("sp", bTc, bT_d.rearrange("(g n) t -> n g t", n=128)[:, :, c * 128:(c + 1) * 128], [tg("d_bc")], [tbc])
            dma("sp", cTc, cT_d.rearrange("(g n) t -> n g t", n=128)[:, :, c * 128:(c + 1) * 128], [tg("d_bc")], [tbc])
            for d in range(2):
                dma("sp", hst[d], hds[d][c], [tg("d_hF"), tg("d_hB")], [tg(f"p5_h{d}")])
            for g in range(4):
                sl = slice(g * 512, (g + 1) * 512)
                pz, pzt = psum()
                for k in range(8):
                    mm(pz[:, :], uT[:, k, HALO + c * 128:HALO + (c + 1) * 128], wz[:, k, sl], k == 0, k == 7, [tuT, twz], [pzt])
                act(szl[g], pz[:, :], AF.Silu, [pzt], [tg(f"p5_sz{g}")])
            pcb, pcbt = psum()
            for g in range(4):
                mm(pcb[:, g * 128:(g + 1) * 128], bTc[:, g, :], cTc[:, g, :], True, True, [tbc], [pcbt])
            for d in range(2):
                tt("dve", CBm[d], pcb[:, :].rearrange("p (g i) -> p g i", g=4), mks[d][:, :].unsqueeze(1).to_broadcast([128, 4, 128]),
                   ALU.mult, [pcbt, tC], [tg(f"p5_cbm{d}")])
            for d in range(2):
                tsm = tg(f"p5_sm{d}")
                dt_ap = dt_own[:, c, d * 32:(d + 1) * 32]
                a_t, e_t = sm5[:, d * 4 + 0, :], sm5[:, d * 4 + 1, :]
                tt("dve", a_t, dt_ap, Arow[:, d * 32:(d + 1) * 32], ALU.mult, [tg("dt_own"), tg("Arow")], [tsm])
                pa_, pat_ = psum()
                mm(pa_[:, 0:32], tris[d][:], a_t, True, True, [tC, tsm], [pat_])
                act(e_t, pa_[:, 0:32], AF.Exp, [pat_], [tsm])
                tt("dve" if d == 0 else "pool", xdt[d].rearrange("p (h d) -> p h d", h=32), xsc.rearrange("p (h d) -> p h d", h=32),
                   dt_ap.unsqueeze(2).to_broadcast([128, 32, 64]), ALU.mult, [tx, tg("dt_own")], [tg(f"p5_xdt{d}")])
            tt("pool", yo.rearrange("p (h d) -> p h d", h=32), xsc.rearrange("p (h d) -> p h d", h=32),
               Rr("ssd").unsqueeze(2).to_broadcast([128, 32, 64]), ALU.mult, [tx, tC], [tyo])
            for d in range(2):
                tsm = tg(f"p5_sm{d}")
                txd = tg(f"p5_xdt{d}")
                a_t, e_t = sm5[:, d * 4 + 0, :], sm5[:, d * 4 + 1, :]

                def seg(hq, d=d, a_t=a_t, tsm=tsm):
                    nonlocal rs_i
                    rsp, trs = rsegp[rs_i % 3], tg(f"p5_rs{rs_i % 3}")
                    rs_i += 1
                    tt("dve", rsp, tris[d][:, :].unsqueeze(1).to_broadcast([128, 4, 128]),
                       a_t[:, hq * 4:(hq + 1) * 4].unsqueeze(2).to_broadcast([128, 4, 128]), ALU.mult, [tC, tsm], [trs])
                    pseg, psegt = psum()
                    mm(pseg[:, :], ustrs[d][:], rsp.rearrange("p a b -> p (a b)"), True, True, [tC, trs], [psegt])
                    return pseg, psegt
                nxt = seg(0)
                for hq in range(8):
                    g = hq // 2
                    pseg, psegt = nxt
                    if hq + 1 < 8:
                        nxt = seg(hq + 1)
                    es, tes = eseg[hq % 2], tg(f"p5_es{hq % 2}")
                    act(es, pseg[:, :], AF.Exp, [psegt], [tes])
                    MT, tmt = MTl[hq % 2], tg(f"p5_mt{hq % 2}")
                    tt("dve", MT, es.rearrange("p (a b) -> p a b", a=4), CBm[d][:, g, :].unsqueeze(1).to_broadcast([128, 4, 128]), ALU.mult,
                       [tes, tg(f"p5_cbm{d}")], [tmt])
                    for hh in range(4):
                        h = hq * 4 + hh
                        mm(banks[g][:, (h % 8) * 64:(h % 8 + 1) * 64], MT[:, hh, :], xdt[d][:, h * 64:(h + 1) * 64], d == 0 and h % 8 == 0,
                           d == 1 and h % 8 == 7, [tmt, txd], [tg(f"ps{g}")], skip=True)
                for g in range(4):
                    po, pot = psum()
                    mm(po[:, :], cTc[:, g, :], hst[d][:, g * 512:(g + 1) * 512], True, True, [tbc, tg(f"p5_h{d}")], [pot])
                    ytmp, tyt = ytmp_l[yt_i % 2], tg(f"p5_ytmp{yt_i % 2}")
                    yt_i += 1
                    tt("dve", ytmp.rearrange("p (h d) -> p h d", h=8), po[:, :].rearrange("p (h d) -> p h d", h=8),
                       e_t[:, g * 8:(g + 1) * 8].unsqueeze(2).to_broadcast([128, 8, 64]), ALU.mult, [pot, tsm], [tyt])
                    tt("dve", yo[:, g * 512:(g + 1) * 512], yo[:, g * 512:(g + 1) * 512], ytmp, ALU.add, [tyt, tyo], [tyo])
            ysn_t, tys = ysl[c % 2], tg(f"p5_ys{c % 2}")
            for g in range(4):
                sl = slice(g * 512, (g + 1) * 512)
                tt("dve", yo[:, sl], yo[:, sl], banks[g][:, :], ALU.add, [tyo, tg(f"ps{g}")], [tyo])
                tt("dve", yo[:, sl], yo[:, sl], szl[g], ALU.mult, [tyo, tg(f"p5_sz{g}")], [tyo])
                act(jk5, yo[:, sl], AF.Square, [tyo], [tg("p5_jk"), tg("p5_ss")], accum=ss5[:, g:g + 1])
            tss = tg("p5_ss")
            ts("dve", ss5[:, 4:8], ss5[:, 0:4], 1.0 / 512, EPS, ALU.mult, ALU.add, [tss], [tss])
            act(ss5[:, 4:8], ss5[:, 4:8], AF.Sqrt, [tss], [tss])
            P.op("dve", lambda e: e.reciprocal(out=ss5[:, 4:8], in_=ss5[:, 4:8]), [tss], [tss])
            for g in range(4):
                sl = slice(g * 512, (g + 1) * 512)
                if g % 2 == 0:
                    ts("dve", ysn_t[:, sl], yo[:, sl], ss5[:, 4 + g:5 + g], None, ALU.mult, None, [tyo, tss], [tys])
                else:
                    act(ysn_t[:, sl], yo[:, sl], AF.Copy, [tyo, tss], [tys], scale=ss5[:, 4 + g:5 + g])
            dma("sp", ysn_d[c * 128:(c + 1) * 128, :], ysn_t, [tys], [tg("d_ysn")])
        bank_rng[:] = [0, 8]
        while pre_i[0] < len(pre_jobs) or pre_pending[0] is not None:
            prepass_step(1)
        if upto <= 5:
            P.finalize(nc, st)
            return nc

        new_phase()
        wgs = carve([128, 8, 2048], BF16)
        wso = carve([128, 16, D], BF16)
        tw6 = tg("w6")
        wo_h = [load_w(w_o[0][:, q * 512:(q + 1) * 512]) for q in range(2)]
        for q in range(4):
            dma("pool", wgs[:, :, q * 512:(q + 1) * 512], w_in[0][:, C_GC + q * 512:C_GC + (q + 1) * 512].rearrange("(kc p) n -> p kc n", p=128),
                [], [tw6])
        for q in range(2):
            dma("pool", wso[:, :, q * 512:(q + 1) * 512], ssm_out_w[0][:, q * 512:(q + 1) * 512].rearrange("(kc p) n -> p kc n", p=128), [], [tw6])
        for kc in range(16):
            ts("dve", wso[:, kc, :], wso[:, kc, :], S("sng", kc, 1), None, ALU.mult, None, [tw6, tC], [tw6])
        ysn_tl = [carve([128, 2048], BF16) for _ in range(2)]
        yc_tl = [carve([128, D], BF16) for _ in range(2)]
        x_tl = [carve([128, D], F32) for _ in range(2)]
        ysnT = carve([128, 16, 128], BF16)
        sgt = carve([128, 2048], BF16)
        m1 = carve([128, D], BF16)
        mg = carve([128, D], BF16)
        mT = carve([128, 8, 128], BF16)
        tmp6 = carve([128, 512], F32)
        for t in range(NCH):
            tin, tys_, tsg6, tm1, tmg, tmT, ttmp = tg(f"p6_in{t % 2}"), tg("p6_ysnT"), tg("p6_sg"), tg("p6_m1"), tg("p6_mg"), tg("p6_mT"), tg("p6_tmp")
            ysn_t, yc_t, x_t = ysn_tl[t % 2], yc_tl[t % 2], x_tl[t % 2]
            tpx = tg(f"p6_x{t % 2}")
            dma("sp", ysn_t, ysn_d[t * 128:(t + 1) * 128, :], [tg("d_ysn")], [tin])
            dma("sp", yc_t, yconf_d[t * 128:(t + 1) * 128, :], [tg("d_yconf")], [tin])
            dma("sp", x_t, xo[HALO + t * 128:HALO + (t + 1) * 128, :], [], [tpx])
            for q in range(2):
                pb, pbt = psum()
                pbb = pb[:].bitcast(BF16)
                for i in range(8):
                    tr(pbb[:, i * 128:(i + 1) * 128], ysn_t[:, (q * 8 + i) * 128:(q * 8 + i + 1) * 128], ident[:], [tin, tC], [pbt])
                cp("dve" if q == 0 else "act", ysnT[:, q * 8:(q + 1) * 8, :], pbb.rearrange("p (a b) -> p a b", a=8), [pbt], [tys_])
            for q in range(4):
                pb, pbt = psum()
                for k in range(8):
                    mm(pb[:, :], uT[:, k, HALO + t * 128:HALO + (t + 1) * 128], wgs[:, k, q * 512:(q + 1) * 512], k == 0, k == 7, [tuT, tw6], [pbt])
                act(sgt[:, q * 512:(q + 1) * 512], pb[:, :], AF.Sigmoid, [pbt], [tsg6])
            tt("dve", m1, sgt[:, 0:D], yc_t, ALU.mult, [tsg6, tin], [tm1])
            for half in range(2):
                sl = slice(half * 512, (half + 1) * 512)
                pb, pbt = psum()
                for k in range(16):
                    mm(pb[:, :], ysnT[:, k, :], wso[:, k, sl], k == 0, k == 15, [tys_, tw6], [pbt])
                tt("dve", tmp6, pb[:, :], sgt[:, D + half * 512:D + (half + 1) * 512], ALU.mult, [pbt, tsg6], [ttmp])
                tt("dve", mg[:, sl], tmp6, m1[:, sl], ALU.add, [ttmp, tm1], [tmg])
            pb, pbt = psum()
            pbb = pb[:].bitcast(BF16)
            for i in range(8):
                tr(pbb[:, i * 128:(i + 1) * 128], mg[:, i * 128:(i + 1) * 128], ident[:], [tmg, tC], [pbt])
            cp("act", mT, pbb.rearrange("p (a b) -> p a b", a=8), [pbt], [tmT])
            for half in range(2):
                sl = slice(half * 512, (half + 1) * 512)
                pb, pbt = psum()
                for k in range(8):
                    mm(pb[:, :], mT[:, k, :], wo_h[half][0][:, k, :], k == 0, k == 7, [tmT, wo_h[half][1]], [pbt])
                tt("dve", tmp6, pb[:, :], G1r[:, sl], ALU.mult, [pbt, tg("rowsC")], [ttmp])
                tt("dve", x_t[:, sl], tmp6, x_t[:, sl], ALU.add, [ttmp, tpx], [tpx])
            dma("sp", x1_d[t * 128:(t + 1) * 128, :], x_t, [tpx], [tg("d_x1")])
        if upto <= 6:
            P.finalize(nc, st)
            return nc

        new_phase()
        xt_l = [carve([128, D], F32) for _ in range(2)]
        junk = carve([128, D], BF16)
        xn = carve([128, D], F32)
        vtok_all = carve([128, NCH, D], BF16)
        vTt_l = [carve([128, 8, 128], BF16) for _ in range(2)]
        st_l = [carve([128, 4], F32) for _ in range(2)]
        rwb = carve([128, 8, NEXP], BF16)
        lg_all = carve([128, NCH, 32], F32)
        wr_all = carve([128, NCH, 32], F32)
        sl_all = carve([128, NCH, 32], F32)
        m8_all = carve([128, NCH, 8], F32)
        rt = carve([128, 8, 32], F32)
        base = carve([128, 32], F32)
        big = carve([128, NBLK, 32], F32)
        bexp = carve([128, NBLK], F32)
        idxw_f = carve([128, NBLK], F32)
        dma("pool", rwb, router_w[0].rearrange("(kc p) n -> p kc n", p=128), [], [tg("rwb")])
        wk_all = dt_own[:, :, 0:4]
        idx_f = dt_own[:, :, 4:8]
        idx_i = sb("idx_i", [128, NCH * 4], I32)
        idxw_i = sb("idxw_i", [128, NBLK], I32)
        bexp_i = sb("bexp_i", [128, NBLK], I32)
        twk, trt, tbase, trA = tg("wk_all"), tg("rt"), tg("base"), tg("routeA")
        P.op("pool", lambda e: e.memset(base, 0.0), [], [tbase])
        xn_l = [xn, carve([128, D], F32)]
        rt_l = [rt, carve([128, 8, 32], F32)]

        def rA1(t):
            xt, sq, vTt, xn_ = xt_l[t % 2], st_l[t % 2], vTt_l[t % 2], xn_l[t % 2]
            txt, txn, tsq, tvTt = tg(f"m_xt{t % 2}"), tg(f"m_xn{t % 2}"), tg(f"m_sq{t % 2}"), tg(f"m_vTt{t % 2}")
            tvt = tg(f"vtok{t}")
            dma("sp", xt, x1_d[t * 128:(t + 1) * 128, :], [tg("d_x1")], [txt])
            act(junk, xt, AF.Square, [txt], [tg("m_junk"), tsq], accum=sq[:, 0:1])
            ts("dve", sq[:, 1:2], sq[:, 0:1], 1.0 / D, EPS, ALU.mult, ALU.add, [tsq], [tsq])
            act(sq[:, 2:3], sq[:, 1:2], AF.Sqrt, [tsq], [tsq])
            P.op("dve", lambda e, sq=sq: e.reciprocal(out=sq[:, 3:4], in_=sq[:, 2:3]), [tsq], [tsq])
            stt("dve", xn_, xt, sq[:, 3:4], A2r, ALU.mult, ALU.mult, [txt, tsq, tg("rowsC")], [txn])
            tt("dve", vtok_all[:, t, :], xn_, SH2r, ALU.add, [txn, tg("rowsC")], [tvt])
            pb, pbt = psum()
            pbb = pb[:].bitcast(BF16)
            for kc in range(8):
                tr(pbb[:, kc * 128:(kc + 1) * 128], vtok_all[:, t, kc * 128:(kc + 1) * 128], ident[:], [tvt, tC], [pbt])
            cp("act", vTt, pbb.rearrange("p (a b) -> p a b", a=8), [pbt], [tvTt])
            pl, plt = psum()
            for k in range(8):
                mm(pl[:, 0:32], vTt[:, k, :], rwb[:, k, :], k == 0, k == 7, [tvTt, tg("rwb")], [plt])
            return pl, plt

        def rA2(t, pl, plt):
            rt_ = rt_l[t % 2]
            trt_, trA_ = tg(f"rt{t % 2}"), tg(f"routeA{t}")
            lg, m8, wr = lg_all[:, t, :], m8_all[:, t, :], wr_all[:, t, :]
            ex, msk = rt_[:, 0, :], rt_[:, 1, :]
            sc1 = rt_[:, 2, 0:4]
            tt("dve", lg, pl[:, 0:32], Rr("rb"), ALU.add, [plt, tC], [trA_])
            P.op("dve", lambda e, m8=m8, lg=lg: e.max(out=m8, in_=lg), [trA_], [trA_])
            ts("dve", sc1[:, 0:1], m8[:, 0:1], -1.0, None, ALU.mult, None, [trA_], [trt_])
            ts("dve", msk, lg, m8[:, 3:4], None, ALU.is_ge, None, [trA_], [trt_])
            act(ex, lg, AF.Exp, [trA_, trt_], [trt_], bias=sc1[:, 0:1])
            tt("dve", ex, ex, msk, ALU.mult, [trt_], [trt_])
            P.op("dve", lambda e, ex=ex, sc1=sc1: e.tensor_reduce(out=sc1[:, 1:2], in_=ex, axis=AX.X, op=ALU.add), [trt_], [trt_])
            P.op("dve", lambda e, sc1=sc1: e.reciprocal(out=sc1[:, 2:3], in_=sc1[:, 1:2]), [trt_], [trt_])
            ts("dve", wr, ex, sc1[:, 2:3], None, ALU.mult, None, [trt_], [trA_])
            pp, ppt = psum()
            mm(pp[:, 0:32], ustr_b[:], msk, True, True, [tC, trt_], [ppt])
            mm(pp[:, 32:64], onesf[:], msk, True, True, [tC, trt_], [ppt])
            tt("dve", sl_all[:, t, :], pp[:, 0:32], base, ALU.add, [ppt, tbase], [trA_])
            tt("dve", base, base, pp[:, 32:64], ALU.add, [ppt, tbase], [tbase])

        cur = rA1(0)
        for t in range(NCH):
            nxt = rA1(t + 1) if t + 1 < NCH else None
            rA2(t, *cur)
            cur = nxt
        trt = tg("rt0")
        tpb = tg("padblk")
        cnt3 = big[:, 0:32, 0:16]
        tt("dve", cnt3, base.unsqueeze(2).to_broadcast([128, 32, 16]), Rr("thr16").unsqueeze(1).to_broadcast([128, 32, 16]), ALU.is_gt,
           [tbase, tC], [tpb])
        padded, pend, ptmp, pstart = rt[:, 3, :], rt[:, 4, :], rt[:, 5, :], rt[:, 6, :]
        P.op("dve", lambda e: e.tensor_reduce(out=padded, in_=cnt3, axis=AX.X, op=ALU.add), [tpb], [trt])
        ts("dve", padded, padded, float(BS), None, ALU.mult, None, [trt], [trt])
        cp("dve", pend, padded, [trt], [trt])
        for sh in (1, 2, 4, 8, 16):
            cp("dve", ptmp, pend, [trt], [trt])
            tt("dve", pend[:, sh:32], ptmp[:, sh:32], ptmp[:, 0:32 - sh], ALU.add, [trt], [trt])
        tt("dve", pstart, pend, padded, ALU.subtract, [trt], [trt])
        tt("dve", big, pend.unsqueeze(1).to_broadcast([128, NBLK, 32]), Rr("bstart").unsqueeze(2).to_broadcast([128, NBLK, 32]), ALU.is_le,
           [trt, tC, tpb], [tpb])
        P.op("dve", lambda e: e.tensor_reduce(out=bexp, in_=big, axis=AX.X, op=ALU.add), [tpb], [tpb])
        ts("dve", bexp, bexp, 31.0, None, ALU.min, None, [tpb], [tpb])
        ts("dve", idxw_f, bexp, 128.0, S("pidx", 0, 1), ALU.mult, ALU.add, [tpb, tC], [tpb])
        cp("dve", idxw_i[:, :], idxw_f, [tpb], [tg("idxw")])
        cp("dve", bexp_i[:, :], bexp, [tpb], [tg("idxw")])
        allA = [tg(f"routeA{t}") for t in range(NCH)]
        oh4 = carve([128, NCH, 4, 32], F32)
        tmp4 = carve([128, NCH, 4, 32], F32)
        slot_all = carve([128, NCH, 32], F32)
        t4 = tg("rc4")
        shp = [128, NCH, 4, 32]
        tt("dve", slot_all, sl_all, pstart.unsqueeze(1).to_broadcast([128, NCH, 32]), ALU.add, allA + [trt], [t4])
        tt("dve", oh4, lg_all.unsqueeze(2).to_broadcast(shp), m8_all[:, :, 0:4].unsqueeze(3).to_broadcast(shp), ALU.is_equal, allA, [t4])
        tt("dve", tmp4, oh4, slot_all.unsqueeze(2).to_broadcast(shp), ALU.mult, [t4], [t4])
        P.op("dve", lambda e: e.tensor_reduce(out=idx_f, in_=tmp4, axis=AX.X, op=ALU.add), [t4], [twk])
        tt("dve", tmp4, oh4, wr_all.unsqueeze(2).to_broadcast(shp), ALU.mult, [t4, twk] + allA, [t4])
        P.op("dve", lambda e: e.tensor_reduce(out=wk_all, in_=tmp4, axis=AX.X, op=ALU.add), [t4], [twk])
        cp("dve", idx_i[:, :].rearrange("p (t k) -> p t k", k=4), idx_f, [twk], [twk])
        for t in range(NCH):
            for k in range(4):
                P.dma("pool", lambda e, t=t, k=k: e.indirect_dma_start(
                    out=vbuf_d[:, :], out_offset=bass.IndirectOffsetOnAxis(ap=idx_i[:, t * 4 + k:t * 4 + k + 1], axis=0),
                    in_=vtok_all[:, t, :], in_offset=None, bounds_check=None), [tg(f"vtok{t}"), twk], [tg("d_vbuf")])
        dbg("wk", dt_own[:], [128, NCH, 64], F32, [twk])
        dbg("bexp", bexp, [128, NBLK], F32, [tpb])
        new_phase()
        wg_l = [uT[:].rearrange("p a b -> p (a b)")[:, 0:16 * D].rearrange("p (a b) -> p a b", a=8), carve([128, 8, 2 * D], BF16)]
        wd_l = [carve([128, 8, D], BF16) for _ in range(2)]
        bg_l = [carve([128, 16], F32) for _ in range(2)]
        bd_l = [carve([128, D], F32) for _ in range(2)]
        NT = BS // 128
        xin = carve([128, NT, D], BF16)
        actT = carve([128, 8, BS], BF16)
        gcb = [carve([128, BS], BF16) for _ in range(2)]
        sgb = [carve([128, BS], BF16) for _ in range(2)]
        ucb = [carve([128, BS], BF16) for _ in range(2)]
        yst = [carve([128, D], F32) for _ in range(2)]
        xT_l = [carve([128, 8, BS], BF16) for _ in range(2)]
        ei = 0
        yi = 0

        def blk_bufs(i):
            b2 = i % 2
            return (wg_l[b2], wd_l[b2], bg_l[b2], bd_l[b2], xT_l[b2],
                    (tuT if b2 == 0 else tg("wg1")), tg(f"wd{b2}"), tg(f"bg{b2}"), tg(f"bd{b2}"), tg(f"xT{b2}"))

        def blk_load(i):
            wg, wd, bg, bd, xTb, twg, twd, tbg, tbd, txT = blk_bufs(i)
            iw = idxw_i[:, i:i + 1]
            P.dma("pool", lambda e, wg=wg, iw=iw: e.indirect_dma_start(
                out=wg.rearrange("p a b -> p (a b)"), out_offset=None, in_=wgu_bf[:, :], in_offset=bass.IndirectOffsetOnAxis(ap=iw, axis=0),
                bounds_check=None), [tg("d_wbf"), tg("idxw")], [twg])
            P.dma("pool", lambda e, wd=wd, iw=iw: e.indirect_dma_start(
                out=wd.rearrange("p a b -> p (a b)"), out_offset=None, in_=wdn_bf[:, :], in_offset=bass.IndirectOffsetOnAxis(ap=iw, axis=0),
                bounds_check=None), [tg("d_wbf"), tg("idxw")], [twd])
            P.dma("pool", lambda e, bg=bg, iw=iw: e.indirect_dma_start(
                out=bg[:, :], out_offset=None, in_=bgu_tab[:, :], in_offset=bass.IndirectOffsetOnAxis(ap=iw, axis=0),
                bounds_check=None), [tg("idxw")], [tbg])
            P.dma("pool", lambda e, bd=bd, i=i: e.indirect_dma_start(
                out=bd[:, :], out_offset=None, in_=b_down[0], in_offset=bass.IndirectOffsetOnAxis(ap=bexp_i[:, i:i + 1], axis=0),
                bounds_check=None), [tg("idxw")], [tbd])
            dma("sp", xin, vbuf_d[i * BS:(i + 1) * BS, :].rearrange("(a p) d -> p a d", p=128), [tg("d_vbuf")], [tg("xin")])

        def blk_prep(i):
            wg, wd, bg, bd, xTb, twg, twd, tbg, tbd, txT = blk_bufs(i)
            for a in range(NT):
                pb, pbt = psum()
                pbb = pb[:].bitcast(BF16)
                for kc in range(8):
                    tr(pbb[:, kc * 128:(kc + 1) * 128], xin[:, a, kc * 128:(kc + 1) * 128], ident[:], [tg("xin"), tC], [pbt])
                cp("act", xTb[:, :, a * 128:(a + 1) * 128], pbb.rearrange("p (a b) -> p a b", a=8), [pbt], [txT])

        def blk_gu(i):
            nonlocal ei
            wg, wd, bg, bd, xTb, twg, twd, tbg, tbd, txT = blk_bufs(i)
            for j in range(8):
                pg, pgt = psum()
                for k in range(8):
                    mm(pg[:, 0:BS], wg[:, k, j * 128:(j + 1) * 128], xTb[:, k, :], k == 0, k == 7, [twg, txT], [pgt])
                for k in range(8):
                    mm(pg[:, BS:2 * BS], wg[:, k, D + j * 128:D + (j + 1) * 128], xTb[:, k, :], k == 0, k == 7, [twg, txT], [pgt], skip=True)
                i2 = ei % 2
                ei += 1
                g_, s_, u_ = gcb[i2], sgb[i2], ucb[i2]
                tg_, ts_, tu_ = tg(f"m_g{i2}"), tg(f"m_s{i2}"), tg(f"m_u{i2}")
                ts("dve", g_, pg[:, 0:BS], bg[:, j:j + 1], 7.0, ALU.add, ALU.min, [pgt, tbg], [tg_])
                act(s_, g_, AF.Sigmoid, [tg_], [ts_], scale=1.702)
                tt("dve", s_, g_, s_, ALU.mult, [tg_, ts_], [ts_])
                ts("dve", u_, pg[:, BS:2 * BS], bg[:, 8 + j:9 + j], 7.0, ALU.add, ALU.min, [pgt, tbg], [tu_])
                ts("dve", u_, u_, -7.0, 1.0, ALU.max, ALU.add, [tu_], [tu_])
                tt("dve", actT[:, j, :], u_, s_, ALU.mult, [tu_, ts_], [tg("actT")])

        def blk_down(i):
            nonlocal yi
            wg, wd, bg, bd, xTb, twg, twd, tbg, tbd, txT = blk_bufs(i)
            for a in range(NT):
                ys_, tys2 = yst[yi % 2], tg(f"yst{yi % 2}")
                yi += 1
                for half in range(2):
                    sl = slice(half * 512, (half + 1) * 512)
                    pb, pbt = psum()
                    for k in range(8):
                        mm(pb[:, :], actT[:, k, a * 128:(a + 1) * 128], wd[:, k, sl], k == 0, k == 7, [tg("actT"), twd], [pbt])
                    tt("dve", ys_[:, sl], pb[:, :], bd[:, sl], ALU.add, [pbt, tbd], [tys2])
                dma("sp", ybuf_d[i * BS + a * 128:i * BS + (a + 1) * 128, :], ys_, [tys2], [tg("d_ybuf")])

        blk_load(0)
        blk_prep(0)
        for i in range(NBLK):
            if i + 1 < NBLK:
                blk_load(i + 1)
            blk_gu(i)
            if i + 1 < NBLK:
                blk_prep(i + 1)
            blk_down(i)
        new_phase()
        yk_l = [[carve([128, D], F32) for _ in range(4)] for _ in range(2)]
        x1l = [carve([128, D], F32) for _ in range(2)]
        o8 = [carve([128, D], F32) for _ in range(2)]
        j8 = carve([128, D], BF16)
        s8 = [carve([128, 4], F32) for _ in range(2)]
        for t in range(NCH):
            xt_, ot, sq, yks = x1l[t % 2], o8[t % 2], s8[t % 2], yk_l[t % 2]
            txt, tot_, tsq = tg(f"p8_x{t % 2}"), tg(f"p8_o{t % 2}"), tg(f"p8_s{t % 2}")
            dma("sp", xt_, x1_d[t * 128:(t + 1) * 128, :], [tg("d_x1")], [txt])
            for k in range(4):
                P.dma("pool", lambda e, t=t, k=k, yk=yks[k]: e.indirect_dma_start(
                    out=yk[:, :], out_offset=None, in_=ybuf_d[:, :], in_offset=bass.IndirectOffsetOnAxis(ap=idx_i[:, t * 4 + k:t * 4 + k + 1], axis=0),
                    bounds_check=None), [tg("d_ybuf"), twk], [tg(f"p8_yk{t % 2}_{k}")])
            ts("dve", ot, yks[0], wk_all[:, t, 0:1], None, ALU.mult, None, [tg(f"p8_yk{t % 2}_0"), twk], [tot_])
            for k in range(1, 4):
                stt("dve", ot, yks[k], wk_all[:, t, k:k + 1], ot, ALU.mult, ALU.add, [tg(f"p8_yk{t % 2}_{k}"), twk, tot_], [tot_])
            tt("dve", ot, ot, G2r, ALU.mult, [tot_, tg("rowsC")], [tot_])
            tt("dve", xt_, xt_, ot, ALU.add, [txt, tot_], [txt])
            act(j8, xt_, AF.Square, [txt], [tg("p8_j"), tsq], accum=sq[:, 0:1])
            ts("dve", sq[:, 1:2], sq[:, 0:1], 1.0 / D, EPS, ALU.mult, ALU.add, [tsq], [tsq])
            act(sq[:, 2:3], sq[:, 1:2], AF.Sqrt, [tsq], [tsq])
            P.op("dve", lambda e, sq=sq: e.reciprocal(out=sq[:, 3:4], in_=sq[:, 2:3]), [tsq], [tsq])
            stt("dve", ot, xt_, sq[:, 3:4], Afr, ALU.mult, ALU.mult, [txt, tsq, tg("rowsC")], [tot_])
            tt("dve", ot, ot, SHfr, ALU.add, [tot_, tg("rowsC")], [tot_])
            dma("sp", out_d[t * 128:(t + 1) * 128, :], ot, [tot_], [tg("d_out")])
        P.finalize(nc, st)
    return nc


def _pp(v, n):
    return np.ascontiguousarray(np.asarray(v, np.float32).reshape(n, 128).T)


def make_in_maps(inp):
    f = lambda k: np.asarray(inp[k], np.float32)
    x, c, w_in = f("x"), f("c"), f("w_in")
    conv_w, conv_b = f("ssm_conv_w")[0], f("ssm_conv_b")[0]
    shared = {k: np.ascontiguousarray(f(k)) for k in ("ada_w", "final_ada_w", "w_in", "conf_out_w", "ssm_out_w", "w_o",
                                                       "router_w", "w_gu", "w_down", "b_down")}
    shared["bgu_tab"] = np.ascontiguousarray(f("b_gu")[0].reshape(32, 16, 128).transpose(0, 2, 1).reshape(32 * 128, 16))
    dtb = (f("dt_bias_f")[0], f("dt_bias_b")[0])
    alog = (f("a_log_f")[0], f("a_log_b")[0])
    wdt = (w_in[0][:, C_DTF:C_DTF + 32], w_in[0][:, C_DTB:C_DTB + 32])
    maps = []
    for j in range(8):
        b, s = j // 4, j % 4
        xb = x[b]
        L = xb.shape[0]

        def rows_of(lo, hi):
            o = np.zeros((hi - lo, D), np.float32)
            a, e = max(lo, 0), min(hi, L)
            o[a - lo:e - lo] = xb[a:e]
            return o
        xo = rows_of(T * s - HALO, T * s + T + HALO)
        slots = [(q, 0) for q in range(s)] + [(q, 1) for q in range(3, s, -1)]
        xs3 = np.zeros((3, TS, D), np.float32)
        m3 = np.zeros((3, 4), np.float32)
        wdt3 = np.zeros((3, D, 32), np.float32)
        dtb3 = np.zeros((3, 32), np.float32)
        alog3 = np.zeros((3, 32), np.float32)
        cw3 = np.zeros((128, 3, 20, 5), np.float32)
        for k, (q, d) in enumerate(slots):
            r = rows_of(T * q - 2, T * q + T + 2)
            v = np.array([T * q - 2 >= 0, T * q - 1 >= 0, T * q + T < L, T * q + T + 1 < L], np.float32)
            cw = conv_w[:, :2560]
            if d == 1:
                r, v, cw = r[::-1], v[::-1], cw[::-1]
            xs3[k], m3[k] = r, v
            wdt3[k], dtb3[k], alog3[k] = wdt[d], dtb[d], alog[d]
            cw3[:, k] = cw.T.reshape(20, 128, 5).transpose(1, 0, 2)
        keep = [0.0 if (k == 0 or k == s) else 1.0 for k in range(3)]
        selF = [1.0 if (s >= 1 and k == s - 1) else 0.0 for k in range(3)]
        selB = [1.0 if (s <= 2 and k == 2) else 0.0 for k in range(3)]
        small = np.zeros((128, NS), np.float32)

        def put(name, arr):
            o, w = SM[name]
            small[:, o:o + w] = np.asarray(arr, np.float32).reshape(-1, w) if np.ndim(arr) > 1 else np.tile(np.asarray(arr, np.float32), (128, 1))
        put("cT", _pp(c[b], 8))
        put("adab", _pp(f("ada_b")[0], 48))
        put("finb", _pp(f("final_ada_b"), 16))
        put("gmix", _pp(f("norm_mix_g")[0], 8))
        put("gffn", _pp(f("norm_ffn_g")[0], 8))
        put("gfin", _pp(f("final_norm_g"), 8))
        put("mown", np.concatenate([np.full(16, 1.0 if s > 0 else 0.0), np.full(16, 1.0 if s < 3 else 0.0)]))
        put("m3", m3.reshape(-1))
        put("flg", np.array(keep + selF + selB))
        put("dw", f("conf_dw_w")[0].T.reshape(8, 128, 31).transpose(1, 0, 2).reshape(128, 248))
        put("dwb", _pp(f("conf_dw_b")[0], 8))
        put("lng", _pp(f("conf_ln_g")[0], 8))
        put("lnb", _pp(f("conf_ln_b")[0], 8))
        put("cw", conv_w.T.reshape(24, 128, 5).transpose(1, 0, 2).reshape(128, 120))
        put("cb", _pp(conv_b, 24))
        put("cw3", cw3.reshape(128, 300))
        put("pidx", np.arange(128, dtype=np.float32).reshape(128, 1))
        put("sng", _pp(f("ssm_norm_g")[0], 16))
        rows = np.zeros((1, NR), np.float32)

        def putr(name, arr):
            o, w = RW[name]
            rows[0, o:o + w] = np.asarray(arr, np.float32).reshape(-1)
        putr("cob", f("conf_out_b")[0])
        putr("rb", f("router_b")[0])
        putr("dtb", np.concatenate(dtb))
        putr("alog", np.concatenate(alog))
        putr("ssd", f("ssm_d")[0])
        putr("dtb3", dtb3)
        putr("alog3", alog3)
        putr("bstart", np.arange(NBLK, dtype=np.float32) * BS)
        putr("thr16", np.arange(16, dtype=np.float32) * BS)
        m = dict(shared)
        m.update(xo=xo, xs3=np.ascontiguousarray(xs3), small=small, rows=rows, wdt3=wdt3)
        maps.append(m)
    return maps


_NC_CACHE = {}


def kernel(**inputs):
    if "nc" not in _NC_CACHE:
        _NC_CACHE["nc"] = build()
    nc = _NC_CACHE["nc"]
    maps = make_in_maps(inputs)
    res = run_bass_kernel_spmd(nc, maps, core_ids=list(range(8)))
    out = np.zeros((2, 4 * T, D), np.float32)
    for j in range(8):
        out[j // 4, (j % 4) * T:(j % 4 + 1) * T] = res.results[j]["out"]
    return out
```
